# Optimizing a Trainium2 kernel written in Bass

```python
import math
import jax, jax.numpy as jnp
from jax import lax
import numpy as np

D_MODEL = 4096
BATCH = 4
SEQ = 2048
DEPTH = 2

N_BRANCHES = 3
BRANCH_WIDTH = 1024
GLA_HEADS = 4
GLA_DK = 128
GLA_DV = 256
GLA_RANK = 16
GLA_TAU = 16.0
GLA_CHUNK = 64
DSA_HEADS = 8
DSA_DH = 128
IDX_HEADS = 32
IDX_DIM = 64
TOPK_MAX = 256
SB_HEADS = 8
SB_DH = 128
Q_BLOCK = 128
N_BUCKETS = 32
MAX_DISTANCE = 128
D_FF = 4 * D_MODEL
EPS = 1e-6

IN_WIDTHS = (
    GLA_HEADS * GLA_DK,
    GLA_HEADS * GLA_DK,
    GLA_HEADS * GLA_DV,
    GLA_HEADS * GLA_DV,
    GLA_RANK,
    DSA_HEADS * DSA_DH,
    DSA_DH,
    DSA_DH,
    IDX_HEADS * IDX_DIM,
    IDX_DIM,
    IDX_HEADS,
    SB_HEADS * SB_DH,
    SB_HEADS * SB_DH,
    SB_HEADS * SB_DH,
    N_BRANCHES * D_MODEL,
)
IN_COLS = sum(IN_WIDTHS)

kernel_name = "hybrid_gla_dsa_stickbreaking_gated"


def rmsnorm(x, g):
    xf = x.astype(jnp.float32)
    y = xf * lax.rsqrt(jnp.mean(xf * xf, axis=-1, keepdims=True) + EPS)
    return (y * g.astype(jnp.float32)).astype(x.dtype)


def split_projection(proj):
    points, acc = [], 0
    for w in IN_WIDTHS[:-1]:
        acc += w
        points.append(acc)
    return jnp.split(proj, points, axis=-1)


def rel_bucket(dist):
    max_exact = N_BUCKETS // 2
    d = jnp.maximum(dist, 1).astype(jnp.float32)
    large = max_exact + (jnp.log(d / max_exact) / math.log(MAX_DISTANCE / max_exact)
                         * (N_BUCKETS - max_exact)).astype(jnp.int32)
    large = jnp.minimum(large, N_BUCKETS - 1)
    return jnp.where(dist < max_exact, dist, large)


def gla_branch(q, k, v, g_out, a_low, gate_up, gate_bias, head_gain):
    B, T, _ = q.shape
    C = GLA_CHUNK
    N = T // C
    f32 = jnp.float32
    log_a = jax.nn.log_sigmoid((a_low @ gate_up + gate_bias).astype(f32)) / GLA_TAU

    def heads(t, d):
        return t.astype(f32).reshape(B, N, C, GLA_HEADS, d).transpose(1, 0, 3, 2, 4)

    qc = heads(q, GLA_DK) * (GLA_DK ** -0.5)
    kc = heads(k, GLA_DK)
    vc = heads(v, GLA_DV)
    gc = heads(log_a, GLA_DK)
    causal = jnp.tril(jnp.ones((C, C), dtype=bool))

    def step(S, inp):
        qi, ki, vi, gi = inp
        b = jnp.cumsum(gi, axis=-2)
        o_inter = jnp.einsum('bhcd,bhde->bhce', qi * jnp.exp(b), S)
        diff = b[:, :, :, None, :] - b[:, :, None, :, :]
        decay = jnp.exp(jnp.where(causal[:, :, None], diff, -jnp.inf))
        A = jnp.einsum('bhid,bhjd,bhijd->bhij', qi, ki, decay)
        o = o_inter + jnp.einsum('bhij,bhje->bhie', A, vi)
        b_last = b[:, :, -1:, :]
        S = jnp.exp(b_last[:, :, 0, :, None]) * S + jnp.einsum(
            'bhcd,bhce->bhde', ki * jnp.exp(b_last - b), vi)
        return S, o

    S0 = jnp.zeros((B, GLA_HEADS, GLA_DK, GLA_DV), f32)
    _, o = lax.scan(step, S0, (qc, kc, vc, gc))
    o = o.transpose(1, 0, 3, 2, 4).reshape(B, T, GLA_HEADS, GLA_DV)
    o = rmsnorm(o, head_gain).reshape(B, T, GLA_HEADS * GLA_DV)
    o = o * jax.nn.silu(g_out.astype(f32))
    return o.astype(q.dtype)


def dsa_branch(q, k, v, q_idx, k_idx, w_idx, rel_bias):
    B, T, _ = q.shape
    f32 = jnp.float32
    topk = min(TOPK_MAX, T // 4)
    nb = T // Q_BLOCK
    q = q.reshape(B, T, DSA_HEADS, DSA_DH)
    q_idx = q_idx.reshape(B, T, IDX_HEADS, IDX_DIM)
    w_idx = w_idx * (IDX_HEADS ** -0.5)
    key_pos = jnp.arange(T, dtype=jnp.int32)
    gather = jax.vmap(lambda kv, ii: kv[ii])

    def block(start):
        qb = lax.dynamic_slice_in_dim(q, start, Q_BLOCK, axis=1)
        qib = lax.dynamic_slice_in_dim(q_idx, start, Q_BLOCK, axis=1)
        wb = lax.dynamic_slice_in_dim(w_idx, start, Q_BLOCK, axis=1)
        qpos = start + jnp.arange(Q_BLOCK, dtype=jnp.int32)
        s_idx = jnp.einsum('bqhd,bsd->bqhs', qib, k_idx) * (IDX_DIM ** -0.5)
        I = jnp.einsum('bqh,bqhs->bqs', wb, jax.nn.relu(s_idx)).astype(f32)
        visible = key_pos[None, :] <= qpos[:, None]
        I = jnp.where(visible[None], I, -jnp.inf)
        _, sel = lax.top_k(I, topk)
        k_sel = gather(k, sel)
        v_sel = gather(v, sel)
        dist = qpos[None, :, None] - sel
        bias = rel_bias[rel_bucket(jnp.maximum(dist, 0))]
        logits = (jnp.einsum('bqhd,bqkd->bqhk', qb, k_sel).astype(f32) * (DSA_DH ** -0.5)
                  + bias.astype(f32).transpose(0, 1, 3, 2))
        logits = jnp.where((dist >= 0)[:, :, None, :], logits, -jnp.inf)
        p = jax.nn.softmax(logits, axis=-1)
        o = jnp.einsum('bqhk,bqkd->bqhd', p.astype(v.dtype), v_sel)
        return o.reshape(B, Q_BLOCK, DSA_HEADS * DSA_DH)

    out = lax.map(block, jnp.arange(nb, dtype=jnp.int32) * Q_BLOCK)
    return out.transpose(1, 0, 2, 3).reshape(B, T, DSA_HEADS * DSA_DH)


def stickbreaking_branch(q, k, v):
    B, T, _ = q.shape
    f32 = jnp.float32
    nb = T // Q_BLOCK
    q = q.reshape(B, T, SB_HEADS, SB_DH)
    k = k.reshape(B, T, SB_HEADS, SB_DH)
    v = v.reshape(B, T, SB_HEADS, SB_DH)
    key_pos = jnp.arange(T, dtype=jnp.int32)

    def block(start):
        qb = lax.dynamic_slice_in_dim(q, start, Q_BLOCK, axis=1)
        qpos = start + jnp.arange(Q_BLOCK, dtype=jnp.int32)
        z = jnp.einsum('bqhd,bshd->bhqs', qb, k).astype(f32) * (SB_DH ** -0.5)
        strict = key_pos[None, :] < qpos[:, None]
        log_beta = jax.nn.log_sigmoid(z)
        log_1m = jnp.where(strict, jax.nn.log_sigmoid(-z), 0.0)
        after = lax.cumsum(log_1m, axis=3, reverse=True) - log_1m
        w = jnp.where(strict, jnp.exp(log_beta + after), 0.0)
        o = jnp.einsum('bhqs,bshd->bqhd', w.astype(v.dtype), v)
        return o.reshape(B, Q_BLOCK, SB_HEADS * SB_DH)

    out = lax.map(block, jnp.arange(nb, dtype=jnp.int32) * Q_BLOCK)
    return out.transpose(1, 0, 2, 3).reshape(B, T, SB_HEADS * SB_DH)


def mixer_block(h, w_in, gla_gate_up, gla_gate_bias, gla_head_gain, rel_bias, w_branch, w_out):
    B, T, _ = h.shape
    (gq, gk, gv, gg, ga, dq, dk, dv, iq, ik, iw, sq, sk, sv, gates) = split_projection(h @ w_in)
    o_a = gla_branch(gq, gk, gv, gg, ga, gla_gate_up, gla_gate_bias, gla_head_gain)
    o_b = dsa_branch(dq, dk, dv, iq, ik, iw, rel_bias)
    o_c = stickbreaking_branch(sq, sk, sv)
    gates = jax.nn.sigmoid(gates.reshape(B, T, N_BRANCHES, D_MODEL))
    merged = (gates[:, :, 0] * (o_a @ w_branch[0])
              + gates[:, :, 1] * (o_b @ w_branch[1])
              + gates[:, :, 2] * (o_c @ w_branch[2]))
    return merged @ w_out


def sq_relu_mlp(h, w_up, w_down):
    return jnp.square(jax.nn.relu(h @ w_up)) @ w_down


def setup_inputs(seed: int = 0) -> dict:
    key = jax.random.key(seed)
    ks = jax.random.split(key, 16)
    f32 = jnp.float32

    def nrm(k, shape, fan_in):
        return jax.random.normal(k, shape, f32) * (fan_in ** -0.5)

    def gain(k, shape):
        return 1.0 + 0.05 * jax.random.normal(k, shape, f32)

    return {
        "x": jax.random.normal(ks[0], (BATCH, SEQ, D_MODEL), f32),
        "rel_bias": 0.5 * jax.random.normal(ks[1], (N_BUCKETS, DSA_HEADS), f32),
        "norm_mix_pre": gain(ks[2], (DEPTH, D_MODEL)),
        "norm_mix_post": gain(ks[3], (DEPTH, D_MODEL)),
        "norm_mlp_pre": gain(ks[4], (DEPTH, D_MODEL)),
        "norm_mlp_post": gain(ks[5], (DEPTH, D_MODEL)),
        "w_in": nrm(ks[6], (DEPTH, D_MODEL, IN_COLS), D_MODEL),
        "gla_gate_up": nrm(ks[7], (DEPTH, GLA_RANK, GLA_HEADS * GLA_DK), GLA_RANK),
        "gla_gate_bias": 0.1 * jax.random.normal(ks[8], (DEPTH, GLA_HEADS * GLA_DK), f32),
        "gla_head_gain": gain(ks[9], (DEPTH, GLA_DV)),
        "w_branch": nrm(ks[10], (DEPTH, N_BRANCHES, BRANCH_WIDTH, D_MODEL), BRANCH_WIDTH),
        "w_out": nrm(ks[11], (DEPTH, D_MODEL, D_MODEL), D_MODEL),
        "w_mlp_up": nrm(ks[12], (DEPTH, D_MODEL, D_FF), D_MODEL),
        "w_mlp_down": nrm(ks[13], (DEPTH, D_FF, D_MODEL), D_FF),
    }


def reference(x, rel_bias, norm_mix_pre, norm_mix_post, norm_mlp_pre, norm_mlp_post,
              w_in, gla_gate_up, gla_gate_bias, gla_head_gain, w_branch, w_out,
              w_mlp_up, w_mlp_down):
    for l in range(DEPTH):
        h = mixer_block(rmsnorm(x, norm_mix_pre[l]), w_in[l], gla_gate_up[l], gla_gate_bias[l],
                        gla_head_gain[l], rel_bias, w_branch[l], w_out[l])
        x = x + rmsnorm(h, norm_mix_post[l])
        h = sq_relu_mlp(rmsnorm(x, norm_mlp_pre[l]), w_mlp_up[l], w_mlp_down[l])
        x = x + rmsnorm(h, norm_mlp_post[l])
    return x
```

```python
import numpy as np
from contextlib import ExitStack
import concourse.bass as bass
import concourse.mybir as mybir
from concourse.bass_utils import run_bass_kernel_spmd
import ml_dtypes

F32 = mybir.dt.float32
BF16 = mybir.dt.bfloat16
AF = mybir.ActivationFunctionType
ALU = mybir.AluOpType
AX = mybir.AxisListType
SEM_WRAP = 30000

T = 1024
D = 4096
KC = D // 128
DFF = 16384
EPS = 1e-6
NDSEM = 56
ARENA_COLS = 100 * 1024

SEG = {}
_o = 0
for _n, _w in [("gq", 512), ("gk", 512), ("gv", 1024), ("gg", 1024), ("ga", 16), ("dq", 1024),
               ("dk", 128), ("dv", 128), ("iq", 2048), ("ik", 64), ("iw", 32), ("sq", 1024),
               ("sk", 1024), ("sv", 1024), ("gates", 12288)]:
    SEG[_n] = (_o, _w)
    _o += _w
IN_COLS = _o


class Sem:
    __slots__ = ("h", "idx", "count")

    def __init__(self, h, idx):
        self.h = h
        self.idx = idx
        self.count = 0


class Buf:
    __slots__ = ("name", "w", "r")

    def __init__(self, name=""):
        self.name = name
        self.w = None
        self.r = {}


class Eng:
    def __init__(self, S, name, eng, nsems):
        self.name = name
        self.eng = eng
        self.sems = [S.new_sem(f"{name}{i}") for i in range(nsems)]
        self.count = 0
        self.known = {}
        self.pending = False
        self.is_pe = name == "pe"

    def tag_next(self):
        c = self.count
        return (self.sems[c // SEM_WRAP], c % SEM_WRAP + 1)

    def tag_last(self):
        c = self.count - 1
        if c < 0:
            return None
        return (self.sems[c // SEM_WRAP], c % SEM_WRAP + 1)


class Sched:
    def __init__(self, nc, stack):
        self.nc = nc
        self.stack = stack
        self.nsem = 0
        self.pe = Eng(self, "pe", nc.tensor, 8)
        self.act = Eng(self, "act", nc.scalar, 4)
        self.dve = Eng(self, "dve", nc.vector, 6)
        self.pool = Eng(self, "pool", nc.gpsimd, 3)
        self.sp = Eng(self, "sp", nc.sync, 1)
        self.engs = [self.pe, self.act, self.dve, self.pool, self.sp]
        self.dsems = [self.new_sem(f"dma{i}") for i in range(NDSEM)]
        self.dsem_i = 0
        self.xsems = [self.new_sem("cc")]
        self.arena = stack.enter_context(nc.sbuf_tensor("arena", [128, ARENA_COLS], BF16))
        self.aoff = 0
        self.ps = []
        self.psb = []
        for i in range(8):
            t = stack.enter_context(nc.psum_tensor(f"psum{i}", [128, 512], F32))
            self.ps.append(t)
            self.psb.append(Buf(f"ps{i}"))

    def new_sem(self, name):
        h = self.stack.enter_context(self.nc.semaphore(name))
        s = Sem(h, self.nsem)
        self.nsem += 1
        return s

    def alloc(self, cols, dtype=BF16):
        n = cols * (2 if dtype == F32 else 1)
        n = (n + 15) // 16 * 16
        assert self.aoff + n <= ARENA_COLS, (self.aoff, n)
        v = self.arena[:, self.aoff:self.aoff + n]
        self.aoff += n
        if dtype == F32:
            v = v.bitcast(F32)
        if v.shape[1] != cols:
            v = v[:, 0:cols]
        return v, Buf()

    def dsem(self):
        s = self.dsems[self.dsem_i]
        self.dsem_i += 1
        assert self.dsem_i <= NDSEM
        return s

    def _wait(self, E, reads, writes):
        need = {}

        def add(tag):
            s, v = tag
            if need.get(s.idx, (None, 0))[1] < v:
                need[s.idx] = (s, v)

        for b in reads:
            if b.w is not None:
                add(b.w)
        for b in writes:
            if b.w is not None:
                add(b.w)
            for t in b.r.values():
                add(t)
        for idx, (s, v) in need.items():
            if E.is_pe and s in E.sems:
                continue
            if E.known.get(idx, 0) >= v:
                continue
            E.eng.wait_ge(s.h, v)
            E.known[idx] = v

    def _record(self, tag, reads, writes):
        s, v = tag
        for b in reads:
            old = b.r.get(s.idx)
            if old is None or old[1] < v:
                b.r[s.idx] = tag
        for b in writes:
            b.w = tag
            b.r = {}

    def op(self, E, fn, reads=(), writes=(), signal=True):
        self._wait(E, reads, writes)
        ins = fn(E.eng)
        tag = E.tag_next()
        if signal:
            ins.then_inc(tag[0].h, 1)
            E.count += 1
            E.pending = False
        else:
            E.pending = True
        self._record(tag, reads, writes)
        return ins

    def dma(self, Q, out, in_, sem, reads=(), writes=(), **kw):
        self._wait(Q, reads, writes)
        ins = Q.eng.dma_start(out=out, in_=in_, **kw)
        sem.count += 16
        ins.then_inc(sem.h, 16)
        self._record((sem, sem.count), reads, writes)
        return ins

    def barrier(self, skip_x=False):
        tags = []
        for E in self.engs:
            assert not E.pending, E.name
            t = E.tag_last()
            if t is not None:
                tags.append(t)
        for s in self.dsems + ([] if skip_x else self.xsems):
            if s.count > 0:
                tags.append((s, s.count))
        for E in self.engs:
            for (s, v) in tags:
                if s in E.sems:
                    continue
                if E.known.get(s.idx, 0) >= v:
                    continue
                E.eng.wait_ge(s.h, v)
                E.known[s.idx] = v
        self.aoff = 0
        self.dsem_i = 0
        for b in self.psb:
            b.w = None
            b.r = {}

    def finish(self):
        self.barrier()


def mm(S, out, lhsT, rhs, start, stop, reads, writes, sig=True):
    S.op(S.pe, lambda e: e.matmul(out, lhsT=lhsT, rhs=rhs, start=start, stop=stop),
         reads=reads, writes=writes, signal=(stop or sig))


class Consts:
    pass


def setup_consts(S, nc, io):
    C = Consts()
    C.ones, b0 = S.alloc(128)
    C.ident, b1 = S.alloc(128)
    C.ustrict, b2 = S.alloc(128)
    C.flag, b3 = S.alloc(1, F32)
    C.onesf, b4 = S.alloc(128)
    C.gains, b5 = S.alloc(io["gains"].shape[1], F32)
    sems = [S.dsem() for _ in range(5)]
    S.dma(S.sp, C.ones, io["c_ones"], sems[0], writes=[b0])
    S.dma(S.sp, C.ident, io["c_ident"], sems[1], writes=[b1])
    S.dma(S.sp, C.ustrict, io["c_ustrict"], sems[2], writes=[b2])
    S.dma(S.sp, C.flag, io["flag"], sems[3], writes=[b3])
    S.dma(S.sp, C.gains, io["gains"], sems[4], writes=[b5])
    S.op(S.dve, lambda e: e.tensor_scalar(out=C.onesf, in0=C.ones, scalar1=C.flag[:, 0:1], scalar2=None,
                                          op0=ALU.mult), reads=[b0, b3], writes=[b4])
    S.barrier()
    S.abase = S.aoff = (sum([128, 128, 128, 16, 128]) + io["gains"].shape[1] * 2 + 15) // 16 * 16
    return C


def phase_begin(S):
    S.barrier()
    S.aoff = S.abase


def phase_norm(S, C, xT, gcol, hT, hB):
    xs = []
    for i in range(3):
        v, b = S.alloc(T, F32)
        xs.append((v, b, S.dsem()))
    sq = [S.alloc(T) for _ in range(2)]
    rstd, rb = S.alloc(T, F32)
    ss = [S.ps[0], S.ps[1]]
    ssb = [S.psb[0], S.psb[1]]
    for c in range(KC):
        v, b, sem = xs[c % 3]
        S.dma(S.sp, v, xT[c * 128:(c + 1) * 128, :], sem, writes=[b])
        q, qb = sq[c % 2]
        S.op(S.act, lambda e: e.activation(out=q, in_=v, func=AF.Square), reads=[b], writes=[qb])
        for t in range(2):
            mm(S, ss[t][:, :], C.ones, q[:, t * 512:(t + 1) * 512], c == 0, c == KC - 1, [qb], [ssb[t]])
    for t in range(2):
        sl = slice(t * 512, (t + 1) * 512)
        S.op(S.dve, lambda e: e.tensor_scalar(out=rstd[:, sl], in0=ss[t][:, :], scalar1=1.0 / D, scalar2=EPS,
                                              op0=ALU.mult, op1=ALU.add), reads=[ssb[t]], writes=[rb])
    S.op(S.act, lambda e: e.activation(out=rstd, in_=rstd, func=AF.Sqrt), reads=[rb], writes=[rb])
    S.op(S.dve, lambda e: e.reciprocal(out=rstd, in_=rstd), reads=[rb], writes=[rb])
    for c in range(KC):
        v, b, sem = xs[c % 3]
        S.dma(S.sp, v, xT[c * 128:(c + 1) * 128, :], sem, writes=[b])
        S.op(S.dve, lambda e: e.scalar_tensor_tensor(out=hT[:, c * T:(c + 1) * T], in0=v,
                                                     scalar=C.gains[:, gcol + c:gcol + c + 1], in1=rstd,
                                                     op0=ALU.mult, op1=ALU.mult), reads=[b, rb], writes=[hB])


class Slabs:
    def __init__(self, S, nk, wmax, nbuf=2):
        self.S = S
        self.nk = nk
        self.wmax = wmax
        self.bufs = []
        for i in range(nbuf):
            v, b = S.alloc(nk * wmax)
            self.bufs.append((v, b, S.dsem()))
        self.i = 0

    def load(self, W, k0, c0, w, kstep=8):
        S = self.S
        v, b, sem = self.bufs[self.i % len(self.bufs)]
        self.i += 1
        view = v[:, 0:self.nk * w].rearrange("p (k c) -> p k c", k=self.nk)
        for ks in range(0, self.nk, kstep):
            ke = min(self.nk, ks + kstep)
            src = W[(k0 + ks) * 128:(k0 + ke) * 128, c0:c0 + w].rearrange("(k p) c -> p k c", p=128)
            S.dma(S.pool, view[:, ks:ke, :], src, sem, writes=[b])
        return view, b


def phase_proj_fm(S, C, hT, hB, W, segs, nk=KC):
    slabs = Slabs(S, nk, 512)
    stg = []
    for i in range(3):
        v, b = S.alloc(T)
        stg.append((v, b, S.dsem()))
    stgf = []
    for i in range(2):
        v, b = S.alloc(T, F32)
        stgf.append((v, b, S.dsem()))
    relu_tmp = [S.alloc(512, F32) for _ in range(2)]
    work = []
    for (c0, width, dst, epi) in segs:
        for s0 in range(0, width, 512):
            work.append((c0 + s0, min(512, width - s0), dst, s0, epi))
    pi = 0
    si = 0
    nxt = slabs.load(W, 0, work[0][0], work[0][1])
    for wi, (c0, w, dst, r0, epi) in enumerate(work):
        view, wb = nxt
        if wi + 1 < len(work):
            nxt = slabs.load(W, 0, work[wi + 1][0], work[wi + 1][1])
        for n0 in range(0, w, 128):
            m = min(128, w - n0)
            banks = [(pi * 2) % 8, (pi * 2 + 1) % 8]
            pi += 1
            for k in range(nk):
                for t in range(2):
                    mm(S, S.ps[banks[t]][0:m, :], view[:, k, n0:n0 + m], hT[:, k * T + t * 512:k * T + (t + 1) * 512],
                       k == 0, k == nk - 1, [wb, hB], [S.psb[banks[t]]], sig=False)
            if epi == "f32":
                v, b, sem = stgf[si % 2]
            else:
                v, b, sem = stg[si % 3]
            si += 1
            for t in range(2):
                o = v[0:m, t * 512:(t + 1) * 512]
                p = S.ps[banks[t]][0:m, :]
                pb = S.psb[banks[t]]
                if epi == "copy" or epi == "f32":
                    if t == 0:
                        S.op(S.act, lambda e: e.copy(out=o, in_=p), reads=[pb], writes=[b])
                    else:
                        S.op(S.dve, lambda e: e.tensor_copy(out=o, in_=p), reads=[pb], writes=[b])
                elif epi == "sigmoid":
                    S.op(S.act, lambda e: e.activation(out=o, in_=p, func=AF.Sigmoid), reads=[pb], writes=[b])
                elif epi == "relu2":
                    rt, rtb = relu_tmp[(si + t) % 2]
                    S.op(S.act, lambda e: e.activation(out=rt[0:m, :], in_=p, func=AF.Relu), reads=[pb], writes=[rtb])
                    S.op(S.dve, lambda e: e.tensor_tensor(out=o, in0=rt[0:m, :], in1=rt[0:m, :], op=ALU.mult),
                         reads=[rtb], writes=[b])
                else:
                    raise ValueError(epi)
            S.dma(S.sp, dst[r0 + n0:r0 + n0 + m, :], v[0:m, :], sem, reads=[b])


def make_gates_co(S, C, hT, hB, W, c0, width, dst, banks=(6, 7), sw=256):
    slabs = Slabs(S, KC, sw)
    stg = []
    for i in range(3):
        v, b = S.alloc(T)
        stg.append((v, b, S.dsem()))

    def gen():
        work = [(c0 + s0, min(sw, width - s0), s0) for s0 in range(0, width, sw)]
        nxt = slabs.load(W, 0, work[0][0], work[0][1])
        si = 0
        for wi, (cc, w, r0) in enumerate(work):
            view, wb = nxt
            if wi + 1 < len(work):
                nxt = slabs.load(W, 0, work[wi + 1][0], work[wi + 1][1])
            for n0 in range(0, w, 128):
                for k in range(KC):
                    for t in range(2):
                        mm(S, S.ps[banks[t]][:, :], view[:, k, n0:n0 + 128], hT[:, k * T + t * 512:k * T + (t + 1) * 512],
                           k == 0, k == KC - 1, [wb, hB], [S.psb[banks[t]]], sig=False)
                    yield
                v, b, sem = stg[si % 3]
                si += 1
                for t in range(2):
                    S.op(S.act, lambda e: e.activation(out=v[:, t * 512:(t + 1) * 512], in_=S.ps[banks[t]][:, :],
                                                       func=AF.Sigmoid), reads=[S.psb[banks[t]]], writes=[b])
                S.dma(S.sp, dst[r0 + n0:r0 + n0 + 128, :], v, sem, reads=[b])
                yield
    return gen()


def phase_proj_tm(S, C, hT, hB, W, segs):
    slabs = Slabs(S, KC, 512)
    stg = []
    for i in range(3):
        v, b = S.alloc(512)
        stg.append((v, b, S.dsem()))
    work = []
    for (c0, width, dst) in segs:
        for s0 in range(0, width, 512):
            work.append((c0 + s0, min(512, width - s0), dst, s0))
    pi = 0
    si = 0
    nxt = slabs.load(W, 0, work[0][0], work[0][1])
    for wi, (c0, w, dst, r0) in enumerate(work):
        view, wb = nxt
        if wi + 1 < len(work):
            nxt = slabs.load(W, 0, work[wi + 1][0], work[wi + 1][1])
        for tb in range(T // 128):
            bank = pi % 8
            pi += 1
            for k in range(KC):
                mm(S, S.ps[bank][:, 0:w], hT[:, k * T + tb * 128:k * T + (tb + 1) * 128], view[:, k, :],
                   k == 0, k == KC - 1, [wb, hB], [S.psb[bank]], sig=False)
            v, b, sem = stg[si % 3]
            si += 1
            if tb % 2 == 0:
                S.op(S.act, lambda e: e.copy(out=v[:, 0:w], in_=S.ps[bank][:, 0:w]), reads=[S.psb[bank]], writes=[b])
            else:
                S.op(S.dve, lambda e: e.tensor_copy(out=v[:, 0:w], in_=S.ps[bank][:, 0:w]), reads=[S.psb[bank]],
                     writes=[b])
            S.dma(S.sp, dst[tb * 128:(tb + 1) * 128, r0:r0 + w], v[:, 0:w], sem, reads=[b])


def phase_linear_resid(S, C, inT, W, K, yT, x_src, x_dst, gcol):
    N = D
    kch = K // 128
    FG = 16
    nfg = kch // FG
    NG = 3
    wsl = []
    for i in range(2):
        v, b = S.alloc(FG * NG * 128)
        wsl.append((v, b, S.dsem()))
    usl = []
    for i in range(2):
        v, b = S.alloc(FG * T)
        usl.append((v, b, S.dsem()))
    ystg = []
    for i in range(2):
        v, b = S.alloc(T, F32)
        ystg.append((v, b, S.dsem()))
    sq = [S.alloc(T) for _ in range(2)]
    ss = [S.ps[6], S.ps[7]]
    ssb = [S.psb[6], S.psb[7]]
    groups = []
    n = 0
    nch = N // 128
    while n < nch:
        g = min(NG, nch - n)
        groups.append((n, g))
        n += g
    li = 0
    yi = 0
    first_ss = True
    for (n0, g) in groups:
        for fg in range(nfg):
            wv, wb, wsem = wsl[li % 2]
            uv, ub, usem = usl[li % 2]
            li += 1
            wview = wv[:, 0:FG * g * 128].rearrange("p (k c) -> p k c", k=FG)
            for ks in range(0, FG, 8):
                src = W[(fg * FG + ks) * 128:(fg * FG + ks + 8) * 128, n0 * 128:(n0 + g) * 128].rearrange(
                    "(k p) c -> p k c", p=128)
                S.dma(S.pool, wview[:, ks:ks + 8, :], src, wsem, writes=[wb])
            uview = uv.rearrange("p (k t) -> p k t", k=FG)
            for ks in range(0, FG, 8):
                src = inT[(fg * FG + ks) * 128:(fg * FG + ks + 8) * 128, :].rearrange("(k p) t -> p k t", p=128)
                S.dma(S.sp, uview[:, ks:ks + 8, :], src, usem, writes=[ub])
            for j in range(g):
                for k in range(FG):
                    for t in range(2):
                        bank = j * 2 + t
                        mm(S, S.ps[bank][:, :], wview[:, k, j * 128:(j + 1) * 128], uview[:, k, t * 512:(t + 1) * 512],
                           fg == 0 and k == 0, fg == nfg - 1 and k == FG - 1, [wb, ub], [S.psb[bank]],
                           sig=(k == FG - 1 and j == g - 1 and t == 1))
        for j in range(g):
            v, b, sem = ystg[yi % 2]
            q, qb = sq[yi % 2]
            yi += 1
            for t in range(2):
                bank = j * 2 + t
                sl = slice(t * 512, (t + 1) * 512)
                S.op(S.act, lambda e: e.copy(out=v[:, sl], in_=S.ps[bank][:, :]), reads=[S.psb[bank]], writes=[b])
                S.op(S.dve, lambda e: e.tensor_tensor(out=q[:, sl], in0=S.ps[bank][:, :], in1=v[:, sl], op=ALU.mult),
                     reads=[S.psb[bank], b], writes=[qb])
            last = (n0 + j == nch - 1)
            for t in range(2):
                mm(S, ss[t][:, :], C.ones, q[:, t * 512:(t + 1) * 512], first_ss, last, [qb], [ssb[t]])
            first_ss = False
            S.dma(S.sp, yT[(n0 + j) * 128:(n0 + j + 1) * 128, :], v, sem, reads=[b])
    rstd, rb = S.alloc(T, F32)
    for t in range(2):
        sl = slice(t * 512, (t + 1) * 512)
        S.op(S.dve, lambda e: e.tensor_scalar(out=rstd[:, sl], in0=ss[t][:, :], scalar1=1.0 / D, scalar2=EPS,
                                              op0=ALU.mult, op1=ALU.add), reads=[ssb[t]], writes=[rb])
    S.op(S.act, lambda e: e.activation(out=rstd, in_=rstd, func=AF.Sqrt), reads=[rb], writes=[rb])
    S.op(S.dve, lambda e: e.reciprocal(out=rstd, in_=rstd), reads=[rb], writes=[rb])
    ydone = Buf()
    for (v, b, sem) in ystg:
        ydone.w = (sem, sem.count) if ydone.w is None else ydone.w
    ysrc = []
    for i in range(2):
        v, b = S.alloc(T, F32)
        ysrc.append((v, b, S.dsem()))
    xsrc = []
    for i in range(2):
        v, b = S.alloc(T, F32)
        xsrc.append((v, b, S.dsem()))
    xo = []
    for i in range(2):
        v, b = S.alloc(T, F32)
        xo.append((v, b, S.dsem()))
    ystore_bufs = [b for (_, b, _) in ystg]
    for c in range(KC):
        yv, yb, ysem = ysrc[c % 2]
        xv, xb, xsem = xsrc[c % 2]
        ov, ob, osem = xo[c % 2]
        S.dma(S.sp, yv, yT[c * 128:(c + 1) * 128, :], ysem, reads=[], writes=[yb] + (ystore_bufs if c == 0 else []))
        S.dma(S.sp, xv, x_src[c * 128:(c + 1) * 128, :], xsem, writes=[xb])
        S.op(S.dve, lambda e: e.scalar_tensor_tensor(out=yv, in0=yv, scalar=C.gains[:, gcol + c:gcol + c + 1], in1=rstd,
                                                     op0=ALU.mult, op1=ALU.mult), reads=[yb, rb], writes=[yb])
        S.op(S.pool, lambda e: e.tensor_tensor(out=ov, in0=yv, in1=xv, op=ALU.add), reads=[yb, xb], writes=[ob])
        S.dma(S.sp, x_dst[c * 128:(c + 1) * 128, :], ov, osem, reads=[ob])


def phase_merge(S, C, oT, gatesT, Wb, mT):
    o_sb, ob = S.alloc(24 * T)
    osem = S.dsem()
    oview = o_sb.rearrange("p (k t) -> p k t", k=24)
    for i in range(3):
        S.dma(S.sp, oview[:, i * 8:(i + 1) * 8, :], oT[i * 1024:(i + 1) * 1024, :].rearrange("(k p) t -> p k t", p=128),
              osem, writes=[ob])
    slabs = Slabs(S, 8, 512, nbuf=6)
    gt = []
    for i in range(2):
        v, b = S.alloc(3 * T)
        gt.append((v, b, S.dsem()))
    acc = [S.alloc(512, F32) for _ in range(2)]
    tmp = [S.alloc(512, F32) for _ in range(4)]
    mst = []
    for i in range(2):
        v, b = S.alloc(T)
        mst.append((v, b, S.dsem()))
    pi = 0
    ci = 0
    for c0 in range(0, D, 512):
        wv = [slabs.load(Wb[i], 0, c0, 512) for i in range(3)]
        for n in range(4):
            cc = c0 // 128 + n
            gv, gb, gsem = gt[cc % 2]
            gview = gv.rearrange("p (i t) -> p i t", i=3)
            src = gatesT.rearrange("(i c) t -> c i t", i=3)[cc * 128:(cc + 1) * 128, :, :]
            S.dma(S.sp, gview, src, gsem, writes=[gb])
            mv, mb, msem = mst[cc % 2]
            for t in range(2):
                banks = [(pi * 3 + i) % 6 for i in range(3)]
                pi += 1
                for i in range(3):
                    for k in range(8):
                        mm(S, S.ps[banks[i]][:, :], wv[i][0][:, k, n * 128:(n + 1) * 128],
                           oview[:, i * 8 + k, t * 512:(t + 1) * 512], k == 0, k == 7, [wv[i][1], ob], [S.psb[banks[i]]],
                           sig=False)
                a, ab = acc[ci % 2]
                t1, t1b = tmp[(2 * ci) % 4]
                t2, t2b = tmp[(2 * ci + 1) % 4]
                ci += 1
                sl = slice(t * 512, (t + 1) * 512)
                S.op(S.dve, lambda e: e.tensor_tensor(out=a, in0=S.ps[banks[0]][:, :], in1=gview[:, 0, sl], op=ALU.mult),
                     reads=[S.psb[banks[0]], gb], writes=[ab])
                S.op(S.dve, lambda e: e.tensor_tensor(out=t1, in0=S.ps[banks[1]][:, :], in1=gview[:, 1, sl], op=ALU.mult),
                     reads=[S.psb[banks[1]], gb], writes=[t1b])
                S.op(S.dve, lambda e: e.tensor_tensor(out=t2, in0=S.ps[banks[2]][:, :], in1=gview[:, 2, sl], op=ALU.mult),
                     reads=[S.psb[banks[2]], gb], writes=[t2b])
                S.op(S.pool, lambda e: e.tensor_tensor(out=a, in0=a, in1=t1, op=ALU.add), reads=[ab, t1b], writes=[ab])
                S.op(S.pool, lambda e: e.tensor_tensor(out=mv[:, sl], in0=a, in1=t2, op=ALU.add), reads=[ab, t2b],
                     writes=[mb])
            S.dma(S.sp, mT[cc * 128:(cc + 1) * 128, :], mv, msem, reads=[mb])


GC_PER_LAYER = 134


def gcols(l):
    b = l * GC_PER_LAYER
    return dict(mix_pre=b, mix_post=b + 32, mlp_pre=b + 64, mlp_post=b + 96, gla_gain=b + 128, gla_bias=b + 130)


def phase_gla_local(S, C, io, qkT, gaT, gvTM, gate_up, gc, qgT, oaloc, sfin):
    c_dk = 128 ** -0.5
    rmask, rmb = S.alloc(T, F32)
    onesr, orb = S.alloc(T, F32)
    cz, czb = S.alloc(64, F32)
    gu, gub = S.alloc(512, F32)
    ga, gab = S.alloc(T, F32)
    nb, nbb = S.alloc(4, F32)
    sems = [S.dsem() for _ in range(5)]
    S.dma(S.sp, rmask, io["c_resetmask"], sems[0], writes=[rmb])
    S.dma(S.sp, onesr, io["c_onesrow"], sems[1], writes=[orb])
    S.dma(S.sp, cz[0:64, :], io["c_causal64"], sems[2], writes=[czb])
    S.dma(S.sp, gu[0:16, :], gate_up, sems[3], writes=[gub])
    S.dma(S.sp, ga[0:16, :], gaT, sems[4], writes=[gab])
    S.op(S.dve, lambda e: e.tensor_scalar(out=nb, in0=C.gains[:, gc["gla_bias"]:gc["gla_bias"] + 4], scalar1=-1.0,
                                          scalar2=None, op0=ALU.mult), writes=[nbb])
    ld = []
    for i in range(2):
        q, qb = S.alloc(T)
        k, kb = S.alloc(T)
        v, vb = S.alloc(16 * 256)
        ld.append((q, qb, k, kb, v, vb, S.dsem(), S.dsem(), S.dsem()))
    e1, e1b = S.alloc(T, F32)
    sp, spb = S.alloc(T, F32)
    bs, bsb = S.alloc(T, F32)
    bg, bgb = S.alloc(T, F32)
    ex, exb = S.alloc(T, F32)
    dl, dlb = S.alloc(T, F32)
    edec, edb = S.alloc(16, F32)
    qe, qeb = S.alloc(T)
    ke, keb = S.alloc(T)
    kd, kdb = S.alloc(T)
    qg, qgb = S.alloc(T)
    qgsem = S.dsem()
    atm = [S.alloc(64) for _ in range(2)]
    kdt = [S.alloc(128) for _ in range(2)]
    Sst, Sb = S.alloc(256, F32)
    Sbf, Sbfb = S.alloc(256)
    ost = []
    for i in range(2):
        v, b = S.alloc(512, F32)
        ost.append((v, b, S.dsem()))
    sfsem = S.dsem()
    oi = 0
    for h in range(4):
        q, qb, k, kb, v, vb, s0, s1, s2 = ld[h % 2]
        S.dma(S.sp, q, qkT[h * 128:(h + 1) * 128, :], s0, writes=[qb])
        S.dma(S.sp, k, qkT[512 + h * 128:512 + (h + 1) * 128, :], s1, writes=[kb])
        vview = v[0:64, :].rearrange("p (n e) -> p n e", n=16)
        S.dma(S.sp, vview, gvTM[:, h * 256:(h + 1) * 256].rearrange("(n j) e -> j n e", j=64), s2, writes=[vb])
        for t in range(2):
            mm(S, S.ps[t][:, :], gu[0:16, h * 128:(h + 1) * 128], ga[0:16, t * 512:(t + 1) * 512], True, True,
               [gub, gab], [S.psb[t]])
            S.op(S.act, lambda e: e.activation(out=e1[:, t * 512:(t + 1) * 512], in_=S.ps[t][:, :], func=AF.Exp,
                                               scale=-1.0, bias=nb[:, h:h + 1]), reads=[S.psb[t], nbb], writes=[e1b])
        S.op(S.act, lambda e: e.activation(out=sp, in_=e1, func=AF.Ln, bias=1.0), reads=[e1b], writes=[spb])
        S.op(S.dve, lambda e: e.tensor_tensor_scan(out=bs, data0=rmask, data1=sp, initial=0.0, op0=ALU.mult,
                                                   op1=ALU.add), reads=[rmb, spb], writes=[bsb])
        S.op(S.dve, lambda e: e.tensor_tensor_scan(out=bg, data0=onesr, data1=sp, initial=0.0, op0=ALU.mult,
                                                   op1=ALU.add), reads=[orb, spb], writes=[bgb])
        bs3 = bs.rearrange("p (n j) -> p n j", j=64)
        S.op(S.act, lambda e: e.activation(out=ex, in_=bs, func=AF.Exp, scale=-1.0 / 16), reads=[bsb], writes=[exb])
        S.op(S.dve, lambda e: e.scalar_tensor_tensor(out=qe, in0=q, scalar=c_dk, in1=ex, op0=ALU.mult, op1=ALU.mult),
             reads=[qb, exb], writes=[qeb])
        S.op(S.act, lambda e: e.activation(out=ex, in_=bs, func=AF.Exp, scale=1.0 / 16), reads=[bsb], writes=[exb])
        S.op(S.dve, lambda e: e.tensor_tensor(out=ke, in0=k, in1=ex, op=ALU.mult), reads=[kb, exb], writes=[keb])
        S.op(S.dve, lambda e: e.tensor_tensor(out=dl.rearrange("p (n j) -> p n j", j=64), in0=bs3,
                                              in1=bs3[:, :, 63:64].to_broadcast([128, 16, 64]), op=ALU.subtract),
             reads=[bsb], writes=[dlb])
        S.op(S.act, lambda e: e.activation(out=ex, in_=dl, func=AF.Exp, scale=1.0 / 16), reads=[dlb], writes=[exb])
        S.op(S.dve, lambda e: e.tensor_tensor(out=kd, in0=k, in1=ex, op=ALU.mult), reads=[kb, exb], writes=[kdb])
        S.op(S.act, lambda e: e.activation(out=edec, in_=bs3[:, :, 63], func=AF.Exp, scale=-1.0 / 16), reads=[bsb],
             writes=[edb])
        S.op(S.act, lambda e: e.activation(out=ex, in_=bg, func=AF.Exp, scale=-1.0 / 16), reads=[bgb], writes=[exb])
        S.op(S.dve, lambda e: e.scalar_tensor_tensor(out=qg, in0=q, scalar=c_dk, in1=ex, op0=ALU.mult, op1=ALU.mult),
             reads=[qb, exb], writes=[qgb])
        S.dma(S.sp, qgT[h * 128:(h + 1) * 128, :], qg, qgsem, reads=[qgb])
        for n in range(16):
            cs = slice(n * 64, (n + 1) * 64)
            mm(S, S.ps[2][0:64, 0:64], ke[:, cs], qe[:, cs], True, True, [keb, qeb], [S.psb[2]])
            am, amb = atm[n % 2]
            S.op(S.dve, lambda e: e.tensor_tensor(out=am[0:64, :], in0=S.ps[2][0:64, 0:64], in1=cz[0:64, :], op=ALU.mult),
                 reads=[S.psb[2], czb], writes=[amb])
            pt = S.ps[3][:, :].bitcast(BF16)
            S.op(S.pe, lambda e: e.transpose(out=pt[0:64, 0:128], in_=kd[:, cs], identity=C.ident), reads=[kdb],
                 writes=[S.psb[3]])
            kt, ktb = kdt[n % 2]
            S.op(S.act, lambda e: e.copy(out=kt[0:64, :], in_=pt[0:64, 0:128]), reads=[S.psb[3]], writes=[ktb])
            for eh in range(2):
                ob_ = S.psb[4 + eh]
                oc = S.ps[4 + eh][:, (n % 8) * 64:(n % 8 + 1) * 64]
                mm(S, oc, vview[:, n, eh * 128:(eh + 1) * 128], am[0:64, :], True, n == 0, [vb, amb], [ob_])
                if n > 0:
                    mm(S, oc, Sbf[:, eh * 128:(eh + 1) * 128], qe[:, cs], False, True, [Sbfb, qeb], [ob_])
            mm(S, S.ps[6][:, 0:256], kt[0:64, :], vview[:, n, :], True, True, [ktb, vb], [S.psb[6]])
            if n == 0:
                S.op(S.dve, lambda e: e.tensor_copy(out=Sst, in_=S.ps[6][:, 0:256]), reads=[S.psb[6]], writes=[Sb])
            else:
                S.op(S.dve, lambda e: e.scalar_tensor_tensor(out=Sst, in0=Sst, scalar=edec[:, n:n + 1],
                                                             in1=S.ps[6][:, 0:256], op0=ALU.mult, op1=ALU.add),
                     reads=[Sb, edb, S.psb[6]], writes=[Sb])
            if n < 15:
                S.op(S.act, lambda e: e.copy(out=Sbf, in_=Sst), reads=[Sb], writes=[Sbfb])
            if n % 8 == 7:
                tt = n // 8
                for eh in range(2):
                    ov, ob2, osem = ost[oi % 2]
                    oi += 1
                    S.op(S.act, lambda e: e.copy(out=ov, in_=S.ps[4 + eh][:, :]), reads=[S.psb[4 + eh]], writes=[ob2])
                    S.dma(S.sp, oaloc[h * 256 + eh * 128:h * 256 + (eh + 1) * 128, tt * 512:(tt + 1) * 512], ov, osem,
                          reads=[ob2])
        S.dma(S.sp, sfin[h * 128:(h + 1) * 128, :], Sst, sfsem, reads=[Sb])


def phase_gla_fin(S, C, sfin_o, qgT, oaloc, ggT, gc, oT):
    c256 = 1.0 / 256
    ld = []
    for i in range(2):
        si, sib = S.alloc(256, F32)
        qg, qgb = S.alloc(T)
        ol, olb = S.alloc(2 * T, F32)
        gg, ggb = S.alloc(2 * T)
        ld.append((si, sib, qg, qgb, ol, olb, gg, ggb, S.dsem(), S.dsem(), S.dsem(), S.dsem()))
    sbf, sbfb = S.alloc(256)
    sq, sqb = S.alloc(2 * T)
    rstd, rb = S.alloc(T, F32)
    sg, sgb = S.alloc(2 * T, F32)
    outs = []
    for i in range(2):
        v, b = S.alloc(2 * T)
        outs.append((v, b, S.dsem()))
    for h in range(4):
        si, sib, qg, qgb, ol, olb, gg, ggb, s0, s1, s2, s3 = ld[h % 2]
        S.dma(S.sp, si, sfin_o[h * 128:(h + 1) * 128, :], s0, writes=[sib])
        S.dma(S.sp, qg, qgT[h * 128:(h + 1) * 128, :], s1, writes=[qgb])
        olv = ol.rearrange("p (e t) -> p e t", e=2)
        S.dma(S.sp, olv, oaloc[h * 256:(h + 1) * 256, :].rearrange("(e p) t -> p e t", p=128), s2, writes=[olb])
        ggv = gg.rearrange("p (e t) -> p e t", e=2)
        S.dma(S.sp, ggv, ggT[h * 256:(h + 1) * 256, :].rearrange("(e p) t -> p e t", p=128), s3, writes=[ggb])
        S.op(S.dve, lambda e: e.tensor_scalar(out=sbf, in0=si, scalar1=C.flag[:, 0:1], scalar2=None, op0=ALU.mult),
             reads=[sib], writes=[sbfb])
        for eh in range(2):
            for t in range(2):
                bank = eh * 2 + t
                mm(S, S.ps[bank][:, :], sbf[:, eh * 128:(eh + 1) * 128], qg[:, t * 512:(t + 1) * 512], True, True,
                   [sbfb, qgb], [S.psb[bank]])
                sl = slice(t * 512, (t + 1) * 512)
                S.op(S.dve, lambda e: e.tensor_tensor(out=olv[:, eh, sl], in0=S.ps[bank][:, :], in1=olv[:, eh, sl],
                                                      op=ALU.add), reads=[S.psb[bank], olb], writes=[olb])
        S.op(S.act, lambda e: e.activation(out=sq, in_=ol, func=AF.Square), reads=[olb], writes=[sqb])
        sqv = sq.rearrange("p (e t) -> p e t", e=2)
        for t in range(2):
            for eh in range(2):
                mm(S, S.ps[4 + t][:, :], C.ones, sqv[:, eh, t * 512:(t + 1) * 512], eh == 0, eh == 1, [sqb], [S.psb[4 + t]])
            S.op(S.dve, lambda e: e.tensor_scalar(out=rstd[:, t * 512:(t + 1) * 512], in0=S.ps[4 + t][:, :], scalar1=c256,
                                                  scalar2=EPS, op0=ALU.mult, op1=ALU.add), reads=[S.psb[4 + t]],
                 writes=[rb])
        S.op(S.act, lambda e: e.activation(out=rstd, in_=rstd, func=AF.Sqrt), reads=[rb], writes=[rb])
        S.op(S.dve, lambda e: e.reciprocal(out=rstd, in_=rstd), reads=[rb], writes=[rb])
        S.op(S.act, lambda e: e.activation(out=sg, in_=gg, func=AF.Silu), reads=[ggb], writes=[sgb])
        sgv = sg.rearrange("p (e t) -> p e t", e=2)
        ov, ob2, osem = outs[h % 2]
        ovv = ov.rearrange("p (e t) -> p e t", e=2)
        for eh in range(2):
            S.op(S.dve, lambda e: e.scalar_tensor_tensor(out=olv[:, eh, :], in0=olv[:, eh, :],
                                                         scalar=C.gains[:, gc["gla_gain"] + eh:gc["gla_gain"] + eh + 1],
                                                         in1=rstd, op0=ALU.mult, op1=ALU.mult), reads=[olb, rb],
                 writes=[olb])
            S.op(S.pool, lambda e: e.tensor_tensor(out=ovv[:, eh, :], in0=olv[:, eh, :], in1=sgv[:, eh, :], op=ALU.mult),
                 reads=[olb, sgb], writes=[ob2])
        S.dma(S.sp, oT[h * 256:(h + 1) * 256, :].rearrange("(e p) t -> p e t", p=128), ovv, osem, reads=[ob2])


def phase_sb(S, C, io, sqT, skT, svTM, skT_o, svTM_o, oT):
    c = 128 ** -0.5
    msk, mskb = S.alloc(4 * 512)
    msem = S.dsem()
    S.dma(S.sp, msk.rearrange("p (r t) -> p r t", r=4), io["c_sbmask"], msem, writes=[mskb])
    mview = msk.rearrange("p (r t) -> p r t", r=4)
    ld = []
    for i in range(2):
        q, qb = S.alloc(T)
        k, kb = S.alloc(T)
        ko, kob = S.alloc(T)
        v, vb = S.alloc(T)
        vo, vob = S.alloc(T)
        ld.append((q, qb, k, kb, ko, kob, v, vb, vo, vob, [S.dsem() for _ in range(5)]))
    NB = 10
    ZB = [0, 1, 4]
    AB = [2, 3, 5]
    e1 = [S.alloc(512, F32) for _ in range(NB)]
    sp = [S.alloc(512, F32) for _ in range(NB)]
    mt = [S.alloc(512) for _ in range(NB)]
    ms = [S.alloc(512) for _ in range(5)]
    tt_ = [S.alloc(512, F32) for _ in range(NB)]
    wt = [S.alloc(512) for _ in range(NB)]
    ost = []
    for i in range(2):
        v_, b_ = S.alloc(512)
        ost.append((v_, b_, S.dsem()))
    descs = []
    units = []
    for h in range(8):
        for tt in range(2):
            blocks = [("own", g) for g in range(4 * tt + 3, -1, -1)] + [("oth", g) for g in range(7, -1, -1)]
            u = len(units)
            units.append((h, tt, len(blocks)))
            for bidx, (kind, g) in enumerate(blocks):
                descs.append((u, h, tt, bidx, len(blocks), kind, g))
    N = len(descs)

    def views(h):
        q, qb, k, kb, ko, kob, v, vb, vo, vob, sems = ld[h % 2]
        return (q, qb, k, kb, ko, kob, v.rearrange("p (b d) -> p b d", b=8), vb,
                vo.rearrange("p (b d) -> p b d", b=8), vob, vo, sems)

    def load_head(h):
        q, qb, k, kb, ko, kob, vv, vb, vov, vob, vo, sems = views(h)
        S.dma(S.sp, q, sqT[h * 128:(h + 1) * 128, :], sems[0], writes=[qb])
        S.dma(S.sp, k, skT[h * 128:(h + 1) * 128, :], sems[1], writes=[kb])
        S.dma(S.sp, ko, skT_o[h * 128:(h + 1) * 128, :], sems[2], writes=[kob])
        S.dma(S.sp, vv, svTM[:, h * 128:(h + 1) * 128].rearrange("(b p) d -> p b d", p=128), sems[3], writes=[vb])
        S.dma(S.sp, vov, svTM_o[:, h * 128:(h + 1) * 128].rearrange("(b p) d -> p b d", p=128), sems[4], writes=[vob])

    def info(gi):
        u, h, tt, bidx, nblk, kind, g = descs[gi]
        masked = kind == "own" and g >= 4 * tt
        return u, h, tt, bidx, nblk, kind, g, masked, g - 4 * tt, gi % NB, gi % 3

    def stage1(gi):
        u, h, tt, bidx, nblk, kind, g, masked, r, si, zi = info(gi)
        q, qb, k, kb, ko, kob, vv, vb, vov, vob, vo, sems = views(h)
        if tt == 0 and bidx == 0:
            S.op(S.act, lambda e: e.mul(out=vo, in_=vo, mul=C.flag[:, 0:1]), reads=[vob], writes=[vob])
            if h + 1 < 8:
                load_head(h + 1)
        tsl = slice(tt * 512, (tt + 1) * 512)
        kk, kkb = (k, kb) if kind == "own" else (ko, kob)
        e_, eb_ = e1[si]
        s_, sb_ = sp[si]
        m_, mb_ = mt[si]
        zb = ZB[zi]
        mm(S, S.ps[zb][:, :], kk[:, g * 128:(g + 1) * 128], q[:, tsl], True, True, [kkb, qb], [S.psb[zb]])
        S.op(S.act, lambda e: e.activation(out=e_, in_=S.ps[zb][:, :], func=AF.Exp, scale=-c), reads=[S.psb[zb]],
             writes=[eb_])
        S.op(S.act, lambda e: e.activation(out=s_, in_=e_, func=AF.Ln, bias=1.0), reads=[eb_], writes=[sb_])
        S.op(S.dve, lambda e: e.scalar_tensor_tensor(out=m_, in0=S.ps[zb][:, :], scalar=c, in1=s_, op0=ALU.mult,
                                                     op1=ALU.add), reads=[S.psb[zb], sb_], writes=[mb_])
        if masked:
            S.op(S.pool, lambda e: e.tensor_tensor(out=m_, in0=m_, in1=mview[:, r, :], op=ALU.mult),
                 reads=[mb_, mskb], writes=[mb_])
        if bidx < nblk - 1:
            nm = ms[gi % 5]
            if bidx == 0:
                S.op(S.pool, lambda e: e.tensor_copy(out=nm[0], in_=m_), reads=[mb_], writes=[nm[1]])
            else:
                pm = ms[(gi - 1) % 5]
                S.op(S.pool, lambda e: e.tensor_tensor(out=nm[0], in0=pm[0], in1=m_, op=ALU.add),
                     reads=[pm[1], mb_], writes=[nm[1]])

    def stage2a(gi):
        u, h, tt, bidx, nblk, kind, g, masked, r, si, zi = info(gi)
        ab = AB[zi]
        s_, sb_ = sp[si]
        m_, mb_ = mt[si]
        t_, tb_ = tt_[si]
        mm(S, S.ps[ab][:, :], C.ustrict, m_, True, bidx == 0, [mb_], [S.psb[ab]])
        if bidx > 0:
            pm = ms[(gi - 1) % 5]
            mm(S, S.ps[ab][:, :], C.ones, pm[0], False, True, [pm[1]], [S.psb[ab]])
        S.op(S.dve, lambda e: e.tensor_tensor(out=t_, in0=S.ps[ab][:, :], in1=s_, op=ALU.add),
             reads=[S.psb[ab], sb_], writes=[tb_])

    def stage2b(gi):
        u, h, tt, bidx, nblk, kind, g, masked, r, si, zi = info(gi)
        t_, tb_ = tt_[si]
        w_, wb_ = wt[si]
        S.op(S.act, lambda e: e.activation(out=w_, in_=t_, func=AF.Exp, scale=-1.0), reads=[tb_], writes=[wb_])
        if masked:
            S.op(S.pool, lambda e: e.tensor_tensor(out=w_, in0=w_, in1=mview[:, r, :], op=ALU.mult),
                 reads=[wb_, mskb], writes=[wb_])

    def stage3(gi):
        u, h, tt, bidx, nblk, kind, g, masked, r, si, zi = info(gi)
        q, qb, k, kb, ko, kob, vv, vb, vov, vob, vo, sems = views(h)
        vsrc, vsb = (vv, vb) if kind == "own" else (vov, vob)
        w_, wb_ = wt[si]
        obank = 6 + (u % 2)
        mm(S, S.ps[obank][:, :], vsrc[:, g, :], w_, bidx == 0, bidx == nblk - 1, [vsb, wb_], [S.psb[obank]])
        if bidx == nblk - 1:
            ov, ob2, osem = ost[u % 2]
            S.op(S.act, lambda e: e.copy(out=ov, in_=S.ps[obank][:, :]), reads=[S.psb[obank]], writes=[ob2])
            S.dma(S.sp, oT[2048 + h * 128:2048 + (h + 1) * 128, tt * 512:(tt + 1) * 512], ov, osem, reads=[ob2])

    load_head(0)
    for i in range(N + 7):
        if i < N:
            stage1(i)
        if 0 <= i - 1 < N:
            stage2a(i - 1)
        if 0 <= i - 5 < N:
            stage2b(i - 5)
        if 0 <= i - 7 < N:
            stage3(i - 7)


BIG = 1.0e30
OHW = 1152
NBIS = 26
TOPK_MODE = "bisect"


def phase_dsa(S, C, io, dqT, dkT, dvTM, iqT, ikT, iwTM, dkT_o, dvTM_o, ikT_o, rel_bias, gvec, oT, nc, co=None,
              low_mark=None):
    c = 128 ** -0.5
    wconst = (64 ** -0.5) * (32 ** -0.5)

    def tick(n):
        if co is not None:
            for _ in range(n):
                next(co, None)

    dsa_lo = S.aoff
    rb31, rb31b = S.alloc(8, F32)
    rbsem = S.dsem()
    S.dma(S.sp, rb31, rel_bias[31:32, :].partition_broadcast(128), rbsem, writes=[rb31b])
    iq, iqb = S.alloc(16 * T)
    iqv = iq.rearrange("p (a t) -> p a t", a=16)
    ik, ikb = S.alloc(2 * T)
    iw, iwb = S.alloc(8 * 32)
    wsc, wscb = S.alloc(8 * 32, F32)
    wscv = wsc.rearrange("p (b h) -> p b h", b=8)
    nbig, nbigb = S.alloc(1, F32)
    selT, selTb = S.alloc(16 * T)
    selTv = selT.rearrange("p (b t) -> p b t", b=16)
    s2 = [S.dsem() for _ in range(3)]
    for half in range(2):
        S.dma(S.sp, iqv[half * 64:(half + 1) * 64, :, :],
              iqT[half * 1024:(half + 1) * 1024, :].rearrange("(a d) t -> d a t", d=64), s2[0], writes=[iqb])
        S.dma(S.sp, ik[half * 64:(half + 1) * 64, 0:T], ikT_o, s2[1], writes=[ikb])
        S.dma(S.sp, ik[half * 64:(half + 1) * 64, T:2 * T], ikT, s2[1], writes=[ikb])
    S.dma(S.sp, iw.rearrange("p (b h) -> p b h", b=8), iwTM.rearrange("(b p) h -> p b h", p=128), s2[2], writes=[iwb])
    S.op(S.dve, lambda e: e.tensor_scalar(out=wsc, in0=iw, scalar1=wconst, scalar2=None, op0=ALU.mult), reads=[iwb],
         writes=[wscb])
    S.op(S.dve, lambda e: e.tensor_scalar(out=nbig, in0=C.flag, scalar1=-1.0, scalar2=BIG, op0=ALU.add, op1=ALU.mult),
         writes=[nbigb])
    S.op(S.pool, lambda e: e.memset(selT, 0.0), writes=[selTb])
    Is = [S.alloc(2 * T, F32) for _ in range(1)]
    dgm, dgmb = S.alloc(128, F32)
    dgn, dgnb = S.alloc(128, F32)
    dgsem = S.dsem()
    S.dma(S.sp, dgm, io["c_diagm"], dgsem, writes=[dgmb])
    S.dma(S.sp, dgn, io["c_diagn"], dgsem, writes=[dgnb])
    I2, I2b = S.alloc(2 * T, F32)
    Dt, Dtb = S.alloc(NBIS, F32)
    p2, p2b = S.alloc(NBIS, F32)
    p2sem = S.dsem()
    S.dma(S.sp, p2, io["c_pow2"], p2sem, writes=[p2b])
    rts = [S.alloc(512, F32) for _ in range(2)]
    m8, m8b = S.alloc(8, F32)
    thr, thrb = S.alloc(1, F32)
    sel, selb = S.alloc(2 * T)
    pi = 0
    ri_ = 0
    for j in range(8):
        L = T + (j + 1) * 128
        I, Ib = Is[0]
        S.op(S.dve, lambda e: e.memset(I[:, 0:L], 0.0), writes=[Ib])
        S.op(S.dve, lambda e: e.tensor_scalar(out=I[:, 0:T], in0=I[:, 0:T], scalar1=nbig[:, 0:1], scalar2=None,
                                              op0=ALU.add), reads=[nbigb, Ib], writes=[Ib])
        for hh in range(32):
            half, a = hh // 16, hh % 16
            chunks = []
            for c0 in range(0, L, 512):
                w = min(512, L - c0)
                bank = pi % 4
                pi += 1
                mm(S, S.ps[bank][:, 0:w], iqv[half * 64:(half + 1) * 64, a, j * 128:(j + 1) * 128],
                   ik[half * 64:(half + 1) * 64, c0:c0 + w], True, True, [iqb, ikb], [S.psb[bank]])
                chunks.append((c0, w, bank))
            tick(len(chunks))
            for (c0, w, bank) in chunks:
                rt, rtb = rts[ri_ % 2]
                ri_ += 1
                S.op(S.act, lambda e: e.activation(out=rt[:, 0:w], in_=S.ps[bank][:, 0:w], func=AF.Relu),
                     reads=[S.psb[bank]], writes=[rtb])
                S.op(S.dve, lambda e: e.scalar_tensor_tensor(out=I[:, c0:c0 + w], in0=rt[:, 0:w],
                                                             scalar=wscv[:, j, hh:hh + 1], in1=I[:, c0:c0 + w],
                                                             op0=ALU.mult, op1=ALU.add), reads=[rtb, wscb, Ib],
                     writes=[Ib])
            tick(len(chunks))
        dg = I[:, T + j * 128:T + (j + 1) * 128]
        S.op(S.dve, lambda e: e.tensor_tensor(out=dg, in0=dg, in1=dgm, op=ALU.mult), reads=[Ib, dgmb], writes=[Ib])
        S.op(S.dve, lambda e: e.tensor_tensor(out=dg, in0=dg, in1=dgn, op=ALU.add), reads=[Ib, dgnb], writes=[Ib])
        if True:
            W1 = I2[:, 0:L]
            S.op(S.dve, lambda e: e.scalar_tensor_tensor(out=W1, in0=I[:, 0:L], scalar=-BIG / 2, in1=I[:, 0:L],
                                                         op0=ALU.is_ge, op1=ALU.mult), reads=[Ib], writes=[I2b])
            S.op(S.dve, lambda e: e.tensor_reduce(out=thr, in_=W1, axis=AX.X, op=ALU.min), reads=[I2b], writes=[thrb])
            S.op(S.dve, lambda e: e.reduce_max(out=m8[:, 1:2], in_=I[:, 0:L], axis=AX.X), reads=[Ib], writes=[m8b])
            S.op(S.dve, lambda e: e.tensor_tensor(out=m8[:, 2:3], in0=m8[:, 1:2], in1=thr, op=ALU.subtract),
                 reads=[m8b, thrb], writes=[m8b])
            S.op(S.dve, lambda e: e.tensor_scalar(out=Dt, in0=p2, scalar1=m8[:, 2:3], scalar2=None, op0=ALU.mult),
                 reads=[m8b, p2b], writes=[Dtb])
            for it in range(NBIS):
                S.op(S.dve, lambda e: e.tensor_tensor(out=m8[:, 3:4], in0=Dt[:, it:it + 1], in1=thr, op=ALU.add),
                     reads=[Dtb, thrb], writes=[m8b])
                S.op(S.dve, lambda e: e.tensor_scalar(out=sel[:, 0:L], in0=I[:, 0:L], scalar1=m8[:, 3:4], scalar2=None,
                                                      op0=ALU.is_ge, op1=ALU.add, accum_out=m8[:, 4:5]),
                     reads=[Ib, m8b], writes=[selb, m8b])
                S.op(S.dve, lambda e: e.tensor_scalar(out=m8[:, 5:6], in0=m8[:, 4:5], scalar1=255.5,
                                                      scalar2=Dt[:, it:it + 1], op0=ALU.is_ge, op1=ALU.mult),
                     reads=[m8b, Dtb], writes=[m8b])
                S.op(S.dve, lambda e: e.tensor_tensor(out=thr, in0=thr, in1=m8[:, 5:6], op=ALU.add),
                     reads=[thrb, m8b], writes=[thrb])
                tick(6)
        S.op(S.dve, lambda e: e.tensor_scalar(out=sel[:, 0:L], in0=I[:, 0:L], scalar1=thr[:, 0:1], scalar2=None,
                                              op0=ALU.is_ge), reads=[Ib, thrb], writes=[selb])
        nblk = 8 + j + 1
        for b0 in range(0, nblk, 4):
            nb_ = min(4, nblk - b0)
            bank = 4 + (b0 // 4) % 2
            pt = S.ps[bank][:, :].bitcast(BF16)
            for bb in range(nb_):
                S.op(S.pe, lambda e: e.transpose(out=pt[:, bb * 128:(bb + 1) * 128],
                                                 in_=sel[:, (b0 + bb) * 128:(b0 + bb + 1) * 128], identity=C.ident),
                     reads=[selb], writes=[S.psb[bank]])
            S.op(S.act, lambda e: e.copy(out=selTv[:, b0:b0 + nb_, j * 128:(j + 1) * 128],
                                         in_=pt[:, 0:nb_ * 128].rearrange("p (b t) -> p b t", b=nb_)),
                 reads=[S.psb[bank]], writes=[selTb])
    if co is not None:
        for _ in co:
            pass
    hi_mark = S.aoff
    if low_mark is not None:
        S.barrier()
        S.aoff = low_mark
    rbs, rbsb = S.alloc(8, F32)
    oh, ohb = S.alloc(OHW, F32)
    gvs, gvsb = S.alloc(OHW)
    eb, ebb = S.alloc(8 * 5 * 512)
    ebv = eb.rearrange("p (h r t) -> p h r t", h=8, r=5)
    sems = [S.dsem() for _ in range(5)]
    S.dma(S.sp, rbs[0:32, :], rel_bias, sems[0], writes=[rbsb])
    S.dma(S.sp, oh[0:32, :], io["c_oh"], sems[1], writes=[ohb])
    S.op(S.act, lambda e: e.activation(out=rbs[0:32, :], in_=rbs[0:32, :], func=AF.Exp), reads=[rbsb], writes=[rbsb])
    for i in range(3):
        mm(S, S.ps[i][0:8, 0:384], rbs[0:32, 0:8], oh[0:32, i * 384:(i + 1) * 384], True, True, [rbsb, ohb], [S.psb[i]])
        S.op(S.act, lambda e: e.copy(out=gvs[0:8, i * 384:(i + 1) * 384], in_=S.ps[i][0:8, 0:384]), reads=[S.psb[i]],
             writes=[gvsb])
    gvb = Buf()
    S.dma(S.sp, gvec, gvs[0:8, :], sems[3], reads=[gvsb], writes=[gvb])
    aid, aidb = S.alloc(128)
    S.dma(S.sp, aid, io["c_antiident"], sems[4], writes=[aidb])
    hk = []
    for i in range(3):
        v_, b_ = S.alloc(512)
        hk.append((v_, b_, S.dsem()))
    ti = 0
    for h in range(8):
        for ri in range(5):
            r = ri - 1
            src = bass.AP(gvec.tensor, h * OHW + 385 - 128 * r, [[1, 128], [1, 512]])
            hv, hb, hsem = hk[ti % 3]
            bank = 4 + (ti % 4)
            ti += 1
            S.dma(S.sp, hv, src, hsem, reads=[gvb], writes=[hb])
            mm(S, S.ps[bank][:, :], aid, hv, True, True, [aidb, hb], [S.psb[bank]])
            S.op(S.act, lambda e: e.copy(out=ebv[:, h, ri, :], in_=S.ps[bank][:, :]), reads=[S.psb[bank]], writes=[ebb])
    dk, dkb = S.alloc(2 * T)
    dv, dvb = S.alloc(2 * T)
    dvv = dv.rearrange("p (b d) -> p b d", b=16)
    s3 = [S.dsem() for _ in range(2)]
    S.dma(S.sp, dk[:, 0:T], dkT_o, s3[0], writes=[dkb])
    S.dma(S.sp, dk[:, T:2 * T], dkT, s3[0], writes=[dkb])
    S.dma(S.sp, dvv[:, 0:8, :], dvTM_o.rearrange("(b p) d -> p b d", p=128), s3[1], writes=[dvb])
    S.dma(S.sp, dvv[:, 8:16, :], dvTM.rearrange("(b p) d -> p b d", p=128), s3[1], writes=[dvb])
    S.op(S.act, lambda e: e.mul(out=dv[:, 0:T], in_=dv[:, 0:T], mul=C.flag[:, 0:1]), reads=[dvb], writes=[dvb])
    qs = []
    for i in range(2):
        v, b = S.alloc(T)
        qs.append((v, b, S.dsem()))
    pts = [S.alloc(512) for _ in range(8)]
    rec, recb = S.alloc(512, F32)
    ost = []
    for i in range(2):
        v_, b_ = S.alloc(512)
        ost.append((v_, b_, S.dsem()))
    if low_mark is not None:
        assert S.aoff <= dsa_lo, (S.aoff, dsa_lo)
        S.aoff = hi_mark
    bi = 0
    oi = 0
    for h in range(8):
        q, qb, qsem = qs[h % 2]
        S.dma(S.sp, q, dqT[h * 128:(h + 1) * 128, :], qsem, writes=[qb])
        for tt in range(2):
            tsl = slice(tt * 512, (tt + 1) * 512)
            blocks = [("oth", g) for g in range(8)] + [("own", g) for g in range(4 * tt + 4)]
            nblk = len(blocks)
            ob_ = 2 + (oi % 2)
            db_ = 4 + (oi % 2)
            base = bi
            bi += nblk

            def stage1(bidx):
                kind, g = blocks[bidx]
                bix = g if kind == "oth" else 8 + g
                r = g - 4 * tt if kind == "own" else g - 8 - 4 * tt
                near = r >= -1
                lb = (base + bidx) % 2
                p_, pb_ = pts[(base + bidx) % 8]
                mm(S, S.ps[lb][:, :], dk[:, bix * 128:(bix + 1) * 128], q[:, tsl], True, True, [dkb, qb], [S.psb[lb]])
                if near:
                    S.op(S.act, lambda e: e.activation(out=p_, in_=S.ps[lb][:, :], func=AF.Exp, scale=c),
                         reads=[S.psb[lb]], writes=[pb_])
                    S.op(S.pool, lambda e: e.tensor_tensor(out=p_, in0=p_, in1=ebv[:, h, r + 1, :], op=ALU.mult),
                         reads=[pb_, ebb], writes=[pb_])
                else:
                    S.op(S.act, lambda e: e.activation(out=p_, in_=S.ps[lb][:, :], func=AF.Exp, scale=c,
                                                       bias=rb31[:, h:h + 1]), reads=[S.psb[lb], rb31b], writes=[pb_])
                S.op(S.dve, lambda e: e.tensor_tensor(out=p_, in0=p_, in1=selTv[:, bix, tsl], op=ALU.mult),
                     reads=[pb_, selTb], writes=[pb_])

            def stage2(bidx):
                kind, g = blocks[bidx]
                bix = g if kind == "oth" else 8 + g
                p_, pb_ = pts[(base + bidx) % 8]
                mm(S, S.ps[ob_][:, :], dvv[:, bix, :], p_, bidx == 0, bidx == nblk - 1, [dvb, pb_], [S.psb[ob_]])
                mm(S, S.ps[db_][:, :], C.onesf if kind == "oth" else C.ones, p_, bidx == 0, bidx == nblk - 1, [pb_],
                   [S.psb[db_]])

            for i in range(nblk + 4):
                if i < nblk:
                    stage1(i)
                if 0 <= i - 4 < nblk:
                    stage2(i - 4)
            ov, ob2, osem = ost[oi % 2]
            oi += 1
            S.op(S.dve, lambda e: e.reciprocal(out=rec, in_=S.ps[db_][:, :]), reads=[S.psb[db_]], writes=[recb])
            S.op(S.dve, lambda e: e.tensor_tensor(out=ov, in0=S.ps[ob_][:, :], in1=rec, op=ALU.mult),
                 reads=[S.psb[ob_], recb], writes=[ob2])
            S.dma(S.sp, oT[1024 + h * 128:1024 + (h + 1) * 128, tsl], ov, osem, reads=[ob2])


R_FM = 6144
FM_ROWS = dict(gq=0, gk=512, gg=1024, dq=2048, iq=3072, sq=5120)
TM_COLS = dict(gv=0, iw=1024)
TM_W = 1056
PAIRS = [[0, 1], [2, 3], [4, 5], [6, 7]]

CONST_SPECS = dict(
    c_ones=([128, 128], BF16), c_ident=([128, 128], BF16), c_ustrict=([128, 128], BF16), flag=([128, 1], F32),
    gains=([128, 2 * GC_PER_LAYER], F32), c_resetmask=([128, T], F32), c_onesrow=([128, T], F32),
    c_causal64=([64, 64], F32), c_sbmask=([128, 4, 512], BF16), c_oh=([32, OHW], F32),
    c_antiident=([128, 128], BF16), c_pow2=([128, NBIS], F32),
    c_diagm=([128, 128], F32), c_diagn=([128, 128], F32),
)


def host_consts():
    bf = ml_dtypes.bfloat16
    c = {}
    c["c_ones"] = np.ones((128, 128), bf)
    c["c_ident"] = np.eye(128, dtype=np.float32).astype(bf)
    j = np.arange(128)[:, None]
    s = np.arange(128)[None, :]
    c["c_ustrict"] = (j > s).astype(np.float32).astype(bf)
    t = np.arange(T)
    c["c_resetmask"] = np.broadcast_to((t % 64 != 0).astype(np.float32)[None, :], (128, T)).copy()
    c["c_onesrow"] = np.ones((128, T), np.float32)
    jj = np.arange(64)[:, None]
    ii = np.arange(64)[None, :]
    c["c_causal64"] = (jj <= ii).astype(np.float32)
    r = np.arange(4)[None, :, None]
    sp = np.arange(128)[:, None, None]
    tp = np.arange(512)[None, None, :]
    c["c_sbmask"] = ((r * 128 + sp) < tp).astype(np.float32).astype(bf)
    dist = np.arange(OHW) - 512
    d = np.maximum(dist, 1).astype(np.float32)
    large = 16 + (np.log(d / 16) / np.log(128 / 16) * 16).astype(np.int32)
    large = np.minimum(large, 31)
    bucket = np.where(dist < 16, np.maximum(dist, 0), large)
    oh = np.zeros((32, OHW), np.float32)
    for dd in range(OHW):
        if dist[dd] >= 0:
            oh[bucket[dd], dd] = 1.0
    c["c_oh"] = oh
    c["c_antiident"] = np.eye(128, dtype=np.float32)[::-1].copy().astype(bf)
    tq = np.arange(128)[:, None]
    sq_ = np.arange(128)[None, :]
    c["c_diagm"] = (sq_ <= tq).astype(np.float32)
    c["c_diagn"] = np.where(sq_ <= tq, 0.0, -BIG).astype(np.float32)
    c["c_pow2"] = np.broadcast_to((0.5 ** np.arange(1, NBIS + 1)).astype(np.float32)[None, :], (128, NBIS)).copy()
    return c


def host_gains(inp):
    g = np.zeros((128, 2 * GC_PER_LAYER), np.float32)
    for l in range(2):
        gc = gcols(l)
        for nm, key in [("mix_pre", "norm_mix_pre"), ("mix_post", "norm_mix_post"), ("mlp_pre", "norm_mlp_pre"),
                        ("mlp_post", "norm_mlp_post")]:
            g[:, gc[nm]:gc[nm] + 32] = np.asarray(inp[key][l], np.float32).reshape(32, 128).T
        g[:, gc["gla_gain"]:gc["gla_gain"] + 2] = np.asarray(inp["gla_head_gain"][l], np.float32).reshape(2, 128).T
        g[:, gc["gla_bias"]:gc["gla_bias"] + 4] = np.asarray(inp["gla_gate_bias"][l], np.float32).reshape(4, 128).T
    return g


class Prog:
    def __init__(self):
        self.nc = bass.Bass("TRN2", target_bir_lowering=False)
        self.t = {}

    def dram(self, name, shape, dtype, kind):
        self.t[name] = self.nc.dram_tensor(name, list(shape), dtype, kind=kind).ap()
        return self.t[name]


def declare_consts(P):
    io = {}
    for k, (shp, dt_) in CONST_SPECS.items():
        io[k] = P.dram(k, shp, dt_, "ExternalInput")
    return io


def issue_collectives(S, nc, items):
    sem = S.xsems[0]
    for (src, dst) in items:
        ins = nc.gpsimd.collective_compute("AllGather", ALU.bypass, replica_groups=PAIRS, ins=[src.opt()],
                                           outs=[dst.opt()])
        sem.count += 1
        ins.then_inc(sem.h, 1)


def emit_layer(S, C, io, nc, l, xT, xoutT, w_in, gate_up, rel_bias, w_branch, w_out, w_up, w_down, sc):
    gc = gcols(l)
    qkT, skT, dkik, svt, dvt, gaT, tm = sc["qkT"], sc["skT"], sc["dkik"], sc["svt"], sc["dvt"], sc["gaT"], sc["tm"]
    gatesT, qgT, oaloc, sfin = sc["gatesT"], sc["qgT"], sc["oaloc"], sc["sfin"]
    phase_begin(S)
    hT, hB = S.alloc(KC * T)
    mark = S.aoff
    phase_norm(S, C, xT, gc["mix_pre"], hT, hB)
    S.barrier()
    S.aoff = mark
    segs = []
    for nm in ["gq", "gk", "gg", "dq", "iq", "sq"]:
        c0, w = SEG[nm]
        segs.append((c0, w, qkT[FM_ROWS[nm]:FM_ROWS[nm] + w, :], "copy"))
    segs.append((SEG["sk"][0], 1024, skT, "copy"))
    segs.append((SEG["dk"][0], 128, dkik[0:128, :], "copy"))
    segs.append((SEG["ik"][0], 64, dkik[128:192, :], "copy"))
    segs.append((SEG["ga"][0], 16, gaT, "f32"))
    phase_proj_fm(S, C, hT, hB, w_in, segs)
    S.barrier()
    S.aoff = mark
    tsegs = []
    for nm in ["gv", "iw"]:
        c0, w = SEG[nm]
        tsegs.append((c0, w, tm[:, TM_COLS[nm]:TM_COLS[nm] + w]))
    tsegs.append((SEG["sv"][0], 1024, svt))
    tsegs.append((SEG["dv"][0], 128, dvt))
    phase_proj_tm(S, C, hT, hB, w_in, tsegs)
    S.barrier()
    issue_collectives(S, nc, [(sc[k], sc[k + "_g"]) for k in ["skT", "dkik", "svt", "dvt"]])
    S.aoff = mark
    phase_gla_local(S, C, io, qkT, gaT, tm[:, 0:1024], gate_up, gc, qgT, oaloc, sfin)
    S.barrier()
    issue_collectives(S, nc, [(sc["sfin"], sc["sfin_g"])])
    S.barrier()
    S.aoff = mark
    skT_o, dkik_o, svt_o, dvt_o = sc["skT_g"][0:1024, :], sc["dkik_g"][0:192, :], sc["svt_g"][0:T, :], sc["dvt_g"][0:T, :]
    sfin_o = sc["sfin_g"][0:512, :]
    oT, mT, yT, x1T, uT = sc["oT"], sc["mT"], sc["yT"], sc["x1T"], sc["uT"]
    co = make_gates_co(S, C, hT, hB, w_in, SEG["gates"][0], 12288, gatesT)
    phase_dsa(S, C, io, qkT[FM_ROWS["dq"]:FM_ROWS["dq"] + 1024, :], dkik[0:128, :], dvt,
              qkT[FM_ROWS["iq"]:FM_ROWS["iq"] + 2048, :], dkik[128:192, :], tm[:, TM_COLS["iw"]:TM_COLS["iw"] + 32],
              dkik_o[0:128, :], dvt_o, dkik_o[128:192, :], rel_bias, sc["gvec"], oT, nc, co=co, low_mark=S.abase)
    phase_begin(S)
    phase_sb(S, C, io, qkT[FM_ROWS["sq"]:FM_ROWS["sq"] + 1024, :], skT, svt, skT_o, svt_o, oT)
    phase_begin(S)
    phase_gla_fin(S, C, sfin_o, qgT, oaloc, qkT[FM_ROWS["gg"]:FM_ROWS["gg"] + 1024, :], gc, oT)
    phase_begin(S)
    phase_merge(S, C, oT, gatesT, w_branch, mT)
    phase_begin(S)
    phase_linear_resid(S, C, mT, w_out, D, yT, xT, x1T, gc["mix_post"])
    phase_begin(S)
    hT, hB = S.alloc(KC * T)
    mark = S.aoff
    phase_norm(S, C, x1T, gc["mlp_pre"], hT, hB)
    S.barrier()
    S.aoff = mark
    phase_proj_fm(S, C, hT, hB, w_up, [(0, DFF, uT, "relu2")])
    phase_begin(S)
    phase_linear_resid(S, C, uT, w_down, DFF, yT, x1T, xoutT, gc["mlp_post"])


SCRATCH = dict(qkT=([R_FM, T], BF16), skT=([1024, T], BF16), dkik=([192, T], BF16), svt=([T, 1024], BF16),
               dvt=([T, 128], BF16), gaT=([16, T], F32), tm=([T, TM_W], BF16),
               gatesT=([12288, T], BF16), qgT=([512, T], BF16), oaloc=([1024, T], F32), sfin=([512, 256], F32),
               skT_g=([2048, T], BF16), dkik_g=([384, T], BF16), svt_g=([2 * T, 1024], BF16), dvt_g=([2 * T, 128], BF16),
               sfin_g=([1024, 256], F32),
               gvec=([8, OHW], BF16), oT=([3072, T], BF16), mT=([D, T], BF16), yT=([D, T], F32), x1T=([D, T], F32),
               uT=([DFF, T], BF16), x2T=([D, T], F32))


def build_full():
    P = Prog()
    nc = P.nc
    io = declare_consts(P)
    xT = P.dram("xT", [D, T], F32, "ExternalInput")
    w_in = P.dram("w_in", [2, D, IN_COLS], F32, "ExternalInput")
    gate_up = P.dram("gate_up", [2, 16, 512], F32, "ExternalInput")
    rel_bias = P.dram("rel_bias", [32, 8], F32, "ExternalInput")
    w_branch = P.dram("w_branch", [2, 3, 1024, D], F32, "ExternalInput")
    w_out = P.dram("w_out", [2, D, D], F32, "ExternalInput")
    w_up = P.dram("w_up", [2, D, DFF], F32, "ExternalInput")
    w_down = P.dram("w_down", [2, DFF, D], F32, "ExternalInput")
    sc = {k: P.dram(k, shp, dt_, "Internal") for k, (shp, dt_) in SCRATCH.items()}
    xoutT = P.dram("xoutT", [D, T], F32, "ExternalOutput")
    with ExitStack() as st:
        S = Sched(nc, st)
        C = setup_consts(S, nc, io)
        xin = xT
        for l in range(2):
            xo = sc["x2T"] if l == 0 else xoutT
            emit_layer(S, C, io, nc, l, xin, xo, w_in[l], gate_up[l], rel_bias, [w_branch[l, i] for i in range(3)],
                       w_out[l], w_up[l], w_down[l], sc)
            xin = sc["x2T"]
        S.finish()
    return nc


def kernel(**inputs):
    x = np.asarray(inputs["x"], np.float32)
    consts = host_consts()
    consts["gains"] = host_gains(inputs)
    n = 8
    shared = dict(
        w_in=np.asarray(inputs["w_in"], np.float32), gate_up=np.asarray(inputs["gla_gate_up"], np.float32),
        rel_bias=np.asarray(inputs["rel_bias"], np.float32), w_branch=np.asarray(inputs["w_branch"], np.float32),
        w_out=np.asarray(inputs["w_out"], np.float32), w_up=np.asarray(inputs["w_mlp_up"], np.float32),
        w_down=np.asarray(inputs["w_mlp_down"], np.float32))
    in_maps = []
    for c in range(n):
        b, half = c // 2, c % 2
        m = dict(consts)
        m.update(shared)
        m["flag"] = np.full((128, 1), float(half), np.float32)
        m["xT"] = np.ascontiguousarray(x[b, half * T:(half + 1) * T, :].T)
        in_maps.append(m)
    nc = build_full()
    res = run_bass_kernel_spmd(nc, in_maps, core_ids=list(range(n))).results
    out = np.empty((4, 2048, D), np.float32)
    for c in range(n):
        b, half = c // 2, c % 2
        out[b, half * T:(half + 1) * T, :] = np.asarray(res[c]["xoutT"]).T
    return out
```

```python
import numpy as np
from contextlib import ExitStack
import concourse.bass as bass
import concourse.mybir as mybir
from concourse.bass_utils import run_bass_kernel_spmd
import ml_dtypes

F32 = mybir.dt.float32
BF16 = mybir.dt.bfloat16
AF = mybir.ActivationFunctionType
ALU = mybir.AluOpType
AX = mybir.AxisListType
SEM_WRAP = 30000

T = 1024
D = 4096
KC = D // 128
DFF = 16384
EPS = 1e-6
NDSEM = 56
ARENA_COLS = 105984

SEG = {}
_o = 0
for _n, _w in [("gq", 512), ("gk", 512), ("gv", 1024), ("gg", 1024), ("ga", 16), ("dq", 1024),
               ("dk", 128), ("dv", 128), ("iq", 2048), ("ik", 64), ("iw", 32), ("sq", 1024),
               ("sk", 1024), ("sv", 1024), ("gates", 12288)]:
    SEG[_n] = (_o, _w)
    _o += _w
IN_COLS = _o


class Sem:
    __slots__ = ("h", "idx", "count")

    def __init__(self, h, idx):
        self.h = h
        self.idx = idx
        self.count = 0


class Buf:
    __slots__ = ("name", "w", "r")

    def __init__(self, name=""):
        self.name = name
        self.w = None
        self.r = {}


class Eng:
    def __init__(self, S, name, eng, nsems):
        self.name = name
        self.eng = eng
        self.sems = [S.new_sem(f"{name}{i}") for i in range(nsems)]
        self.count = 0
        self.known = {}
        self.pending = False
        self.is_pe = name == "pe"

    def tag_next(self):
        c = self.count
        return (self.sems[c // SEM_WRAP], c % SEM_WRAP + 1)

    def tag_last(self):
        c = self.count - 1
        if c < 0:
            return None
        return (self.sems[c // SEM_WRAP], c % SEM_WRAP + 1)


class Sched:
    def __init__(self, nc, stack):
        self.nc = nc
        self.stack = stack
        self.nsem = 0
        self.pe = Eng(self, "pe", nc.tensor, 8)
        self.act = Eng(self, "act", nc.scalar, 4)
        self.dve = Eng(self, "dve", nc.vector, 6)
        self.pool = Eng(self, "pool", nc.gpsimd, 3)
        self.sp = Eng(self, "sp", nc.sync, 1)
        self.engs = [self.pe, self.act, self.dve, self.pool, self.sp]
        self.dsems = [self.new_sem(f"dma{i}") for i in range(NDSEM)]
        self.dsem_i = 0
        self.xsems = [self.new_sem("cc")]
        self.arena = stack.enter_context(nc.sbuf_tensor("arena", [128, ARENA_COLS], BF16))
        self.aoff = 0
        self.ps = []
        self.psb = []
        for i in range(8):
            t = stack.enter_context(nc.psum_tensor(f"psum{i}", [128, 512], F32))
            self.ps.append(t)
            self.psb.append(Buf(f"ps{i}"))

    def new_sem(self, name):
        h = self.stack.enter_context(self.nc.semaphore(name))
        s = Sem(h, self.nsem)
        self.nsem += 1
        return s

    def alloc(self, cols, dtype=BF16):
        n = cols * (2 if dtype == F32 else 1)
        n = (n + 15) // 16 * 16
        assert self.aoff + n <= ARENA_COLS, (self.aoff, n)
        v = self.arena[:, self.aoff:self.aoff + n]
        self.aoff += n
        if dtype == F32:
            v = v.bitcast(F32)
        if v.shape[1] != cols:
            v = v[:, 0:cols]
        return v, Buf()

    def dsem(self):
        s = self.dsems[self.dsem_i]
        self.dsem_i += 1
        assert self.dsem_i <= NDSEM
        return s

    def _wait(self, E, reads, writes):
        need = {}

        def add(tag):
            s, v = tag
            if need.get(s.idx, (None, 0))[1] < v:
                need[s.idx] = (s, v)

        for b in reads:
            if b.w is not None:
                add(b.w)
        for b in writes:
            if b.w is not None:
                add(b.w)
            for t in b.r.values():
                add(t)
        for idx, (s, v) in need.items():
            if E.is_pe and s in E.sems:
                continue
            if E.known.get(idx, 0) >= v:
                continue
            E.eng.wait_ge(s.h, v)
            E.known[idx] = v

    def _record(self, tag, reads, writes):
        s, v = tag
        for b in reads:
            old = b.r.get(s.idx)
            if old is None or old[1] < v:
                b.r[s.idx] = tag
        for b in writes:
            b.w = tag
            b.r = {}

    def op(self, E, fn, reads=(), writes=(), signal=True):
        self._wait(E, reads, writes)
        ins = fn(E.eng)
        tag = E.tag_next()
        if signal:
            ins.then_inc(tag[0].h, 1)
            E.count += 1
            E.pending = False
        else:
            E.pending = True
        self._record(tag, reads, writes)
        return ins

    def dma(self, Q, out, in_, sem, reads=(), writes=(), **kw):
        self._wait(Q, reads, writes)
        ins = Q.eng.dma_start(out=out, in_=in_, **kw)
        sem.count += 16
        ins.then_inc(sem.h, 16)
        self._record((sem, sem.count), reads, writes)
        return ins

    def barrier(self, skip_x=False):
        tags = []
        for E in self.engs:
            assert not E.pending, E.name
            t = E.tag_last()
            if t is not None:
                tags.append(t)
        for s in self.dsems + ([] if skip_x else self.xsems):
            if s.count > 0:
                tags.append((s, s.count))
        for E in self.engs:
            for (s, v) in tags:
                if s in E.sems:
                    continue
                if E.known.get(s.idx, 0) >= v:
                    continue
                E.eng.wait_ge(s.h, v)
                E.known[s.idx] = v
        self.aoff = 0
        self.dsem_i = 0
        for b in self.psb:
            b.w = None
            b.r = {}

    def finish(self):
        self.barrier()


def mm(S, out, lhsT, rhs, start, stop, reads, writes, sig=True):
    S.op(S.pe, lambda e: e.matmul(out, lhsT=lhsT, rhs=rhs, start=start, stop=stop),
         reads=reads, writes=writes, signal=(stop or sig))


class Consts:
    pass


def setup_consts(S, nc, io):
    C = Consts()
    C.ones, b0 = S.alloc(128)
    C.ident, b1 = S.alloc(128)
    C.ustrict, b2 = S.alloc(128)
    C.flag, b3 = S.alloc(1, F32)
    C.onesf, b4 = S.alloc(128)
    C.gains, b5 = S.alloc(io["gains"].shape[1], F32)
    sems = [S.dsem() for _ in range(5)]
    S.dma(S.sp, C.ones, io["c_ones"], sems[0], writes=[b0])
    S.dma(S.sp, C.ident, io["c_ident"], sems[1], writes=[b1])
    S.dma(S.sp, C.ustrict, io["c_ustrict"], sems[2], writes=[b2])
    S.dma(S.sp, C.flag, io["flag"], sems[3], writes=[b3])
    S.dma(S.sp, C.gains, io["gains"], sems[4], writes=[b5])
    S.op(S.dve, lambda e: e.tensor_scalar(out=C.onesf, in0=C.ones, scalar1=C.flag[:, 0:1], scalar2=None,
                                          op0=ALU.mult), reads=[b0, b3], writes=[b4])
    S.barrier()
    S.abase = S.aoff = (sum([128, 128, 128, 16, 128]) + io["gains"].shape[1] * 2 + 15) // 16 * 16
    return C


def phase_begin(S):
    S.barrier()
    S.aoff = S.abase


def phase_norm(S, C, xT, gcol, hT, hB):
    xs = []
    for i in range(3):
        v, b = S.alloc(T, F32)
        xs.append((v, b, S.dsem()))
    sq = [S.alloc(T) for _ in range(2)]
    rstd, rb = S.alloc(T, F32)
    ss = [S.ps[0], S.ps[1]]
    ssb = [S.psb[0], S.psb[1]]
    for c in range(KC):
        v, b, sem = xs[c % 3]
        S.dma(S.sp, v, xT[c * 128:(c + 1) * 128, :], sem, writes=[b])
        q, qb = sq[c % 2]
        S.op(S.act, lambda e: e.activation(out=q, in_=v, func=AF.Square), reads=[b], writes=[qb])
        for t in range(2):
            mm(S, ss[t][:, :], C.ones, q[:, t * 512:(t + 1) * 512], c == 0, c == KC - 1, [qb], [ssb[t]])
    for t in range(2):
        sl = slice(t * 512, (t + 1) * 512)
        S.op(S.dve, lambda e: e.tensor_scalar(out=rstd[:, sl], in0=ss[t][:, :], scalar1=1.0 / D, scalar2=EPS,
                                              op0=ALU.mult, op1=ALU.add), reads=[ssb[t]], writes=[rb])
    S.op(S.act, lambda e: e.activation(out=rstd, in_=rstd, func=AF.Sqrt), reads=[rb], writes=[rb])
    S.op(S.dve, lambda e: e.reciprocal(out=rstd, in_=rstd), reads=[rb], writes=[rb])
    for c in range(KC):
        v, b, sem = xs[c % 3]
        S.dma(S.sp, v, xT[c * 128:(c + 1) * 128, :], sem, writes=[b])
        S.op(S.dve, lambda e: e.scalar_tensor_tensor(out=hT[:, c * T:(c + 1) * T], in0=v,
                                                     scalar=C.gains[:, gcol + c:gcol + c + 1], in1=rstd,
                                                     op0=ALU.mult, op1=ALU.mult), reads=[b, rb], writes=[hB])


class Slabs:
    def __init__(self, S, nk, wmax, nbuf=2):
        self.S = S
        self.nk = nk
        self.wmax = wmax
        self.bufs = []
        for i in range(nbuf):
            v, b = S.alloc(nk * wmax)
            self.bufs.append((v, b, S.dsem()))
        self.i = 0

    def load(self, W, k0, c0, w, kstep=8):
        S = self.S
        v, b, sem = self.bufs[self.i % len(self.bufs)]
        self.i += 1
        view = v[:, 0:self.nk * w].rearrange("p (k c) -> p k c", k=self.nk)
        for ks in range(0, self.nk, kstep):
            ke = min(self.nk, ks + kstep)
            src = W[(k0 + ks) * 128:(k0 + ke) * 128, c0:c0 + w].rearrange("(k p) c -> p k c", p=128)
            S.dma(S.pool, view[:, ks:ke, :], src, sem, writes=[b])
        return view, b


def phase_proj_fm(S, C, hT, hB, W, segs, nk=KC):
    slabs = Slabs(S, nk, 512)
    stg = []
    for i in range(3):
        v, b = S.alloc(T)
        stg.append((v, b, S.dsem()))
    stgf = []
    for i in range(2):
        v, b = S.alloc(T, F32)
        stgf.append((v, b, S.dsem()))
    relu_tmp = [S.alloc(512, F32) for _ in range(2)]
    work = []
    for (c0, width, dst, epi) in segs:
        for s0 in range(0, width, 512):
            work.append((c0 + s0, min(512, width - s0), dst, s0, epi))
    pi = 0
    si = 0
    nxt = slabs.load(W, 0, work[0][0], work[0][1])
    for wi, (c0, w, dst, r0, epi) in enumerate(work):
        view, wb = nxt
        if wi + 1 < len(work):
            nxt = slabs.load(W, 0, work[wi + 1][0], work[wi + 1][1])
        for n0 in range(0, w, 128):
            m = min(128, w - n0)
            banks = [(pi * 2) % 8, (pi * 2 + 1) % 8]
            pi += 1
            for k in range(nk):
                for t in range(2):
                    mm(S, S.ps[banks[t]][0:m, :], view[:, k, n0:n0 + m], hT[:, k * T + t * 512:k * T + (t + 1) * 512],
                       k == 0, k == nk - 1, [wb, hB], [S.psb[banks[t]]], sig=False)
            if epi == "f32":
                v, b, sem = stgf[si % 2]
            else:
                v, b, sem = stg[si % 3]
            si += 1
            for t in range(2):
                o = v[0:m, t * 512:(t + 1) * 512]
                p = S.ps[banks[t]][0:m, :]
                pb = S.psb[banks[t]]
                if epi == "copy" or epi == "f32":
                    if t == 0:
                        S.op(S.act, lambda e: e.copy(out=o, in_=p), reads=[pb], writes=[b])
                    else:
                        S.op(S.dve, lambda e: e.tensor_copy(out=o, in_=p), reads=[pb], writes=[b])
                elif epi == "sigmoid":
                    S.op(S.act, lambda e: e.activation(out=o, in_=p, func=AF.Sigmoid), reads=[pb], writes=[b])
                elif epi == "relu2":
                    rt, rtb = relu_tmp[(si + t) % 2]
                    S.op(S.act, lambda e: e.activation(out=rt[0:m, :], in_=p, func=AF.Relu), reads=[pb], writes=[rtb])
                    S.op(S.dve, lambda e: e.tensor_tensor(out=o, in0=rt[0:m, :], in1=rt[0:m, :], op=ALU.mult),
                         reads=[rtb], writes=[b])
                else:
                    raise ValueError(epi)
            S.dma(S.sp, dst[r0 + n0:r0 + n0 + m, :], v[0:m, :], sem, reads=[b])


def make_gates_co(S, C, hT, hB, W, c0, width, dst, banks=(6, 7), sw=256):
    slabs = Slabs(S, KC, sw)
    stg = []
    for i in range(3):
        v, b = S.alloc(T)
        stg.append((v, b, S.dsem()))

    def gen():
        work = [(c0 + s0, min(sw, width - s0), s0) for s0 in range(0, width, sw)]
        nxt = slabs.load(W, 0, work[0][0], work[0][1])
        si = 0
        for wi, (cc, w, r0) in enumerate(work):
            view, wb = nxt
            if wi + 1 < len(work):
                nxt = slabs.load(W, 0, work[wi + 1][0], work[wi + 1][1])
            for n0 in range(0, w, 128):
                for k in range(KC):
                    for t in range(2):
                        mm(S, S.ps[banks[t]][:, :], view[:, k, n0:n0 + 128], hT[:, k * T + t * 512:k * T + (t + 1) * 512],
                           k == 0, k == KC - 1, [wb, hB], [S.psb[banks[t]]], sig=False)
                    yield
                v, b, sem = stg[si % 3]
                si += 1
                for t in range(2):
                    S.op(S.act, lambda e: e.activation(out=v[:, t * 512:(t + 1) * 512], in_=S.ps[banks[t]][:, :],
                                                       func=AF.Sigmoid), reads=[S.psb[banks[t]]], writes=[b])
                S.dma(S.sp, dst[r0 + n0:r0 + n0 + 128, :], v, sem, reads=[b])
                yield
    return gen()


def phase_proj_tm(S, C, hT, hB, W, segs):
    slabs = Slabs(S, KC, 512)
    stg = []
    for i in range(3):
        v, b = S.alloc(512)
        stg.append((v, b, S.dsem()))
    work = []
    for (c0, width, dst) in segs:
        for s0 in range(0, width, 512):
            work.append((c0 + s0, min(512, width - s0), dst, s0))
    pi = 0
    si = 0
    nxt = slabs.load(W, 0, work[0][0], work[0][1])
    for wi, (c0, w, dst, r0) in enumerate(work):
        view, wb = nxt
        if wi + 1 < len(work):
            nxt = slabs.load(W, 0, work[wi + 1][0], work[wi + 1][1])
        for tb in range(T // 128):
            bank = pi % 8
            pi += 1
            for k in range(KC):
                mm(S, S.ps[bank][:, 0:w], hT[:, k * T + tb * 128:k * T + (tb + 1) * 128], view[:, k, :],
                   k == 0, k == KC - 1, [wb, hB], [S.psb[bank]], sig=False)
            v, b, sem = stg[si % 3]
            si += 1
            if tb % 2 == 0:
                S.op(S.act, lambda e: e.copy(out=v[:, 0:w], in_=S.ps[bank][:, 0:w]), reads=[S.psb[bank]], writes=[b])
            else:
                S.op(S.dve, lambda e: e.tensor_copy(out=v[:, 0:w], in_=S.ps[bank][:, 0:w]), reads=[S.psb[bank]],
                     writes=[b])
            S.dma(S.sp, dst[tb * 128:(tb + 1) * 128, r0:r0 + w], v[:, 0:w], sem, reads=[b])


def phase_linear_resid(S, C, inT, W, K, yT, x_src, x_dst, gcol):
    N = D
    kch = K // 128
    FG = 16
    nfg = kch // FG
    NG = 3
    wsl = []
    for i in range(2):
        v, b = S.alloc(FG * NG * 128)
        wsl.append((v, b, S.dsem()))
    usl = []
    for i in range(2):
        v, b = S.alloc(FG * T)
        usl.append((v, b, S.dsem()))
    ystg = []
    for i in range(2):
        v, b = S.alloc(T, F32)
        ystg.append((v, b, S.dsem()))
    sq = [S.alloc(T) for _ in range(2)]
    ss = [S.ps[6], S.ps[7]]
    ssb = [S.psb[6], S.psb[7]]
    groups = []
    n = 0
    nch = N // 128
    while n < nch:
        g = min(NG, nch - n)
        groups.append((n, g))
        n += g
    li = 0
    yi = 0
    first_ss = True
    for (n0, g) in groups:
        for fg in range(nfg):
            wv, wb, wsem = wsl[li % 2]
            uv, ub, usem = usl[li % 2]
            li += 1
            wview = wv[:, 0:FG * g * 128].rearrange("p (k c) -> p k c", k=FG)
            for ks in range(0, FG, 8):
                src = W[(fg * FG + ks) * 128:(fg * FG + ks + 8) * 128, n0 * 128:(n0 + g) * 128].rearrange(
                    "(k p) c -> p k c", p=128)
                S.dma(S.pool, wview[:, ks:ks + 8, :], src, wsem, writes=[wb])
            uview = uv.rearrange("p (k t) -> p k t", k=FG)
            for ks in range(0, FG, 8):
                src = inT[(fg * FG + ks) * 128:(fg * FG + ks + 8) * 128, :].rearrange("(k p) t -> p k t", p=128)
                S.dma(S.sp, uview[:, ks:ks + 8, :], src, usem, writes=[ub])
            for j in range(g):
                for k in range(FG):
                    for t in range(2):
                        bank = j * 2 + t
                        mm(S, S.ps[bank][:, :], wview[:, k, j * 128:(j + 1) * 128], uview[:, k, t * 512:(t + 1) * 512],
                           fg == 0 and k == 0, fg == nfg - 1 and k == FG - 1, [wb, ub], [S.psb[bank]],
                           sig=(k == FG - 1 and j == g - 1 and t == 1))
        for j in range(g):
            v, b, sem = ystg[yi % 2]
            q, qb = sq[yi % 2]
            yi += 1
            for t in range(2):
                bank = j * 2 + t
                sl = slice(t * 512, (t + 1) * 512)
                S.op(S.act, lambda e: e.copy(out=v[:, sl], in_=S.ps[bank][:, :]), reads=[S.psb[bank]], writes=[b])
                S.op(S.dve, lambda e: e.tensor_tensor(out=q[:, sl], in0=S.ps[bank][:, :], in1=v[:, sl], op=ALU.mult),
                     reads=[S.psb[bank], b], writes=[qb])
            last = (n0 + j == nch - 1)
            for t in range(2):
                mm(S, ss[t][:, :], C.ones, q[:, t * 512:(t + 1) * 512], first_ss, last, [qb], [ssb[t]])
            first_ss = False
            S.dma(S.sp, yT[(n0 + j) * 128:(n0 + j + 1) * 128, :], v, sem, reads=[b])
    rstd, rb = S.alloc(T, F32)
    for t in range(2):
        sl = slice(t * 512, (t + 1) * 512)
        S.op(S.dve, lambda e: e.tensor_scalar(out=rstd[:, sl], in0=ss[t][:, :], scalar1=1.0 / D, scalar2=EPS,
                                              op0=ALU.mult, op1=ALU.add), reads=[ssb[t]], writes=[rb])
    S.op(S.act, lambda e: e.activation(out=rstd, in_=rstd, func=AF.Sqrt), reads=[rb], writes=[rb])
    S.op(S.dve, lambda e: e.reciprocal(out=rstd, in_=rstd), reads=[rb], writes=[rb])
    ydone = Buf()
    for (v, b, sem) in ystg:
        ydone.w = (sem, sem.count) if ydone.w is None else ydone.w
    ysrc = []
    for i in range(2):
        v, b = S.alloc(T, F32)
        ysrc.append((v, b, S.dsem()))
    xsrc = []
    for i in range(2):
        v, b = S.alloc(T, F32)
        xsrc.append((v, b, S.dsem()))
    xo = []
    for i in range(2):
        v, b = S.alloc(T, F32)
        xo.append((v, b, S.dsem()))
    ystore_bufs = [b for (_, b, _) in ystg]
    for c in range(KC):
        yv, yb, ysem = ysrc[c % 2]
        xv, xb, xsem = xsrc[c % 2]
        ov, ob, osem = xo[c % 2]
        S.dma(S.sp, yv, yT[c * 128:(c + 1) * 128, :], ysem, reads=[], writes=[yb] + (ystore_bufs if c == 0 else []))
        S.dma(S.sp, xv, x_src[c * 128:(c + 1) * 128, :], xsem, writes=[xb])
        S.op(S.dve, lambda e: e.scalar_tensor_tensor(out=yv, in0=yv, scalar=C.gains[:, gcol + c:gcol + c + 1], in1=rstd,
                                                     op0=ALU.mult, op1=ALU.mult), reads=[yb, rb], writes=[yb])
        S.op(S.pool, lambda e: e.tensor_tensor(out=ov, in0=yv, in1=xv, op=ALU.add), reads=[yb, xb], writes=[ob])
        S.dma(S.sp, x_dst[c * 128:(c + 1) * 128, :], ov, osem, reads=[ob])


def phase_merge(S, C, oT, gatesT, Wb, mT):
    o_sb, ob = S.alloc(24 * T)
    osem = S.dsem()
    oview = o_sb.rearrange("p (k t) -> p k t", k=24)
    for i in range(3):
        S.dma(S.sp, oview[:, i * 8:(i + 1) * 8, :], oT[i * 1024:(i + 1) * 1024, :].rearrange("(k p) t -> p k t", p=128),
              osem, writes=[ob])
    slabs = Slabs(S, 8, 512, nbuf=6)
    gt = []
    for i in range(2):
        v, b = S.alloc(3 * T)
        gt.append((v, b, S.dsem()))
    acc = [S.alloc(512, F32) for _ in range(2)]
    tmp = [S.alloc(512, F32) for _ in range(4)]
    mst = []
    for i in range(2):
        v, b = S.alloc(T)
        mst.append((v, b, S.dsem()))
    pi = 0
    ci = 0
    for c0 in range(0, D, 512):
        wv = [slabs.load(Wb[i], 0, c0, 512) for i in range(3)]
        for n in range(4):
            cc = c0 // 128 + n
            gv, gb, gsem = gt[cc % 2]
            gview = gv.rearrange("p (i t) -> p i t", i=3)
            src = gatesT.rearrange("(i c) t -> c i t", i=3)[cc * 128:(cc + 1) * 128, :, :]
            S.dma(S.sp, gview, src, gsem, writes=[gb])
            mv, mb, msem = mst[cc % 2]
            for t in range(2):
                banks = [(pi * 3 + i) % 6 for i in range(3)]
                pi += 1
                for i in range(3):
                    for k in range(8):
                        mm(S, S.ps[banks[i]][:, :], wv[i][0][:, k, n * 128:(n + 1) * 128],
                           oview[:, i * 8 + k, t * 512:(t + 1) * 512], k == 0, k == 7, [wv[i][1], ob], [S.psb[banks[i]]],
                           sig=False)
                a, ab = acc[ci % 2]
                t1, t1b = tmp[(2 * ci) % 4]
                t2, t2b = tmp[(2 * ci + 1) % 4]
                ci += 1
                sl = slice(t * 512, (t + 1) * 512)
                S.op(S.dve, lambda e: e.tensor_tensor(out=a, in0=S.ps[banks[0]][:, :], in1=gview[:, 0, sl], op=ALU.mult),
                     reads=[S.psb[banks[0]], gb], writes=[ab])
                S.op(S.dve, lambda e: e.tensor_tensor(out=t1, in0=S.ps[banks[1]][:, :], in1=gview[:, 1, sl], op=ALU.mult),
                     reads=[S.psb[banks[1]], gb], writes=[t1b])
                S.op(S.dve, lambda e: e.tensor_tensor(out=t2, in0=S.ps[banks[2]][:, :], in1=gview[:, 2, sl], op=ALU.mult),
                     reads=[S.psb[banks[2]], gb], writes=[t2b])
                S.op(S.pool, lambda e: e.tensor_tensor(out=a, in0=a, in1=t1, op=ALU.add), reads=[ab, t1b], writes=[ab])
                S.op(S.pool, lambda e: e.tensor_tensor(out=mv[:, sl], in0=a, in1=t2, op=ALU.add), reads=[ab, t2b],
                     writes=[mb])
            S.dma(S.sp, mT[cc * 128:(cc + 1) * 128, :], mv, msem, reads=[mb])


GC_PER_LAYER = 134


def gcols(l):
    b = l * GC_PER_LAYER
    return dict(mix_pre=b, mix_post=b + 32, mlp_pre=b + 64, mlp_post=b + 96, gla_gain=b + 128, gla_bias=b + 130)


def phase_gla_local(S, C, io, qkT, gaT, gvTM, gate_up, gc, qgT, oaloc, sfin):
    c_dk = 128 ** -0.5
    rmask, rmb = S.alloc(T, F32)
    onesr, orb = S.alloc(T, F32)
    cz, czb = S.alloc(64, F32)
    gu, gub = S.alloc(512, F32)
    ga, gab = S.alloc(T, F32)
    nb, nbb = S.alloc(4, F32)
    sems = [S.dsem() for _ in range(5)]
    S.dma(S.sp, rmask, io["c_resetmask"], sems[0], writes=[rmb])
    S.dma(S.sp, onesr, io["c_onesrow"], sems[1], writes=[orb])
    S.dma(S.sp, cz[0:64, :], io["c_causal64"], sems[2], writes=[czb])
    S.dma(S.sp, gu[0:16, :], gate_up, sems[3], writes=[gub])
    S.dma(S.sp, ga[0:16, :], gaT, sems[4], writes=[gab])
    S.op(S.dve, lambda e: e.tensor_scalar(out=nb, in0=C.gains[:, gc["gla_bias"]:gc["gla_bias"] + 4], scalar1=-1.0,
                                          scalar2=None, op0=ALU.mult), writes=[nbb])
    ld = []
    for i in range(2):
        q, qb = S.alloc(T)
        k, kb = S.alloc(T)
        v, vb = S.alloc(16 * 256)
        ld.append((q, qb, k, kb, v, vb, S.dsem(), S.dsem(), S.dsem()))
    e1, e1b = S.alloc(T, F32)
    sp, spb = S.alloc(T, F32)
    bs, bsb = S.alloc(T, F32)
    bg, bgb = S.alloc(T, F32)
    ex, exb = S.alloc(T, F32)
    dl, dlb = S.alloc(T, F32)
    edec, edb = S.alloc(16, F32)
    qe, qeb = S.alloc(T)
    ke, keb = S.alloc(T)
    kd, kdb = S.alloc(T)
    qg, qgb = S.alloc(T)
    qgsem = S.dsem()
    atm = [S.alloc(64) for _ in range(2)]
    kdt = [S.alloc(128) for _ in range(2)]
    Sst, Sb = S.alloc(256, F32)
    Sbf, Sbfb = S.alloc(256)
    ost = []
    for i in range(2):
        v, b = S.alloc(512, F32)
        ost.append((v, b, S.dsem()))
    sfsem = S.dsem()
    oi = 0
    for h in range(4):
        q, qb, k, kb, v, vb, s0, s1, s2 = ld[h % 2]
        S.dma(S.sp, q, qkT[h * 128:(h + 1) * 128, :], s0, writes=[qb])
        S.dma(S.sp, k, qkT[512 + h * 128:512 + (h + 1) * 128, :], s1, writes=[kb])
        vview = v[0:64, :].rearrange("p (n e) -> p n e", n=16)
        S.dma(S.sp, vview, gvTM[:, h * 256:(h + 1) * 256].rearrange("(n j) e -> j n e", j=64), s2, writes=[vb])
        for t in range(2):
            mm(S, S.ps[t][:, :], gu[0:16, h * 128:(h + 1) * 128], ga[0:16, t * 512:(t + 1) * 512], True, True,
               [gub, gab], [S.psb[t]])
            S.op(S.act, lambda e: e.activation(out=e1[:, t * 512:(t + 1) * 512], in_=S.ps[t][:, :], func=AF.Exp,
                                               scale=-1.0, bias=nb[:, h:h + 1]), reads=[S.psb[t], nbb], writes=[e1b])
        S.op(S.act, lambda e: e.activation(out=sp, in_=e1, func=AF.Ln, bias=1.0), reads=[e1b], writes=[spb])
        S.op(S.dve, lambda e: e.tensor_tensor_scan(out=bs, data0=rmask, data1=sp, initial=0.0, op0=ALU.mult,
                                                   op1=ALU.add), reads=[rmb, spb], writes=[bsb])
        S.op(S.dve, lambda e: e.tensor_tensor_scan(out=bg, data0=onesr, data1=sp, initial=0.0, op0=ALU.mult,
                                                   op1=ALU.add), reads=[orb, spb], writes=[bgb])
        bs3 = bs.rearrange("p (n j) -> p n j", j=64)
        S.op(S.act, lambda e: e.activation(out=ex, in_=bs, func=AF.Exp, scale=-1.0 / 16), reads=[bsb], writes=[exb])
        S.op(S.dve, lambda e: e.scalar_tensor_tensor(out=qe, in0=q, scalar=c_dk, in1=ex, op0=ALU.mult, op1=ALU.mult),
             reads=[qb, exb], writes=[qeb])
        S.op(S.act, lambda e: e.activation(out=ex, in_=bs, func=AF.Exp, scale=1.0 / 16), reads=[bsb], writes=[exb])
        S.op(S.dve, lambda e: e.tensor_tensor(out=ke, in0=k, in1=ex, op=ALU.mult), reads=[kb, exb], writes=[keb])
        S.op(S.dve, lambda e: e.tensor_tensor(out=dl.rearrange("p (n j) -> p n j", j=64), in0=bs3,
                                              in1=bs3[:, :, 63:64].to_broadcast([128, 16, 64]), op=ALU.subtract),
             reads=[bsb], writes=[dlb])
        S.op(S.act, lambda e: e.activation(out=ex, in_=dl, func=AF.Exp, scale=1.0 / 16), reads=[dlb], writes=[exb])
        S.op(S.dve, lambda e: e.tensor_tensor(out=kd, in0=k, in1=ex, op=ALU.mult), reads=[kb, exb], writes=[kdb])
        S.op(S.act, lambda e: e.activation(out=edec, in_=bs3[:, :, 63], func=AF.Exp, scale=-1.0 / 16), reads=[bsb],
             writes=[edb])
        S.op(S.act, lambda e: e.activation(out=ex, in_=bg, func=AF.Exp, scale=-1.0 / 16), reads=[bgb], writes=[exb])
        S.op(S.dve, lambda e: e.scalar_tensor_tensor(out=qg, in0=q, scalar=c_dk, in1=ex, op0=ALU.mult, op1=ALU.mult),
             reads=[qb, exb], writes=[qgb])
        S.dma(S.sp, qgT[h * 128:(h + 1) * 128, :], qg, qgsem, reads=[qgb])
        for n in range(16):
            cs = slice(n * 64, (n + 1) * 64)
            mm(S, S.ps[2][0:64, 0:64], ke[:, cs], qe[:, cs], True, True, [keb, qeb], [S.psb[2]])
            am, amb = atm[n % 2]
            S.op(S.dve, lambda e: e.tensor_tensor(out=am[0:64, :], in0=S.ps[2][0:64, 0:64], in1=cz[0:64, :], op=ALU.mult),
                 reads=[S.psb[2], czb], writes=[amb])
            pt = S.ps[3][:, :].bitcast(BF16)
            S.op(S.pe, lambda e: e.transpose(out=pt[0:64, 0:128], in_=kd[:, cs], identity=C.ident), reads=[kdb],
                 writes=[S.psb[3]])
            kt, ktb = kdt[n % 2]
            S.op(S.act, lambda e: e.copy(out=kt[0:64, :], in_=pt[0:64, 0:128]), reads=[S.psb[3]], writes=[ktb])
            for eh in range(2):
                ob_ = S.psb[4 + eh]
                oc = S.ps[4 + eh][:, (n % 8) * 64:(n % 8 + 1) * 64]
                mm(S, oc, vview[:, n, eh * 128:(eh + 1) * 128], am[0:64, :], True, n == 0, [vb, amb], [ob_])
                if n > 0:
                    mm(S, oc, Sbf[:, eh * 128:(eh + 1) * 128], qe[:, cs], False, True, [Sbfb, qeb], [ob_])
            mm(S, S.ps[6][:, 0:256], kt[0:64, :], vview[:, n, :], True, True, [ktb, vb], [S.psb[6]])
            if n == 0:
                S.op(S.dve, lambda e: e.tensor_copy(out=Sst, in_=S.ps[6][:, 0:256]), reads=[S.psb[6]], writes=[Sb])
            else:
                S.op(S.dve, lambda e: e.scalar_tensor_tensor(out=Sst, in0=Sst, scalar=edec[:, n:n + 1],
                                                             in1=S.ps[6][:, 0:256], op0=ALU.mult, op1=ALU.add),
                     reads=[Sb, edb, S.psb[6]], writes=[Sb])
            if n < 15:
                S.op(S.act, lambda e: e.copy(out=Sbf, in_=Sst), reads=[Sb], writes=[Sbfb])
            if n % 8 == 7:
                tt = n // 8
                for eh in range(2):
                    ov, ob2, osem = ost[oi % 2]
                    oi += 1
                    S.op(S.act, lambda e: e.copy(out=ov, in_=S.ps[4 + eh][:, :]), reads=[S.psb[4 + eh]], writes=[ob2])
                    S.dma(S.sp, oaloc[h * 256 + eh * 128:h * 256 + (eh + 1) * 128, tt * 512:(tt + 1) * 512], ov, osem,
                          reads=[ob2])
        S.dma(S.sp, sfin[h * 128:(h + 1) * 128, :], Sst, sfsem, reads=[Sb])


def phase_gla_fin(S, C, sfin_o, qgT, oaloc, ggT, gc, oT):
    c256 = 1.0 / 256
    ld = []
    for i in range(2):
        si, sib = S.alloc(256, F32)
        qg, qgb = S.alloc(T)
        ol, olb = S.alloc(2 * T, F32)
        gg, ggb = S.alloc(2 * T)
        ld.append((si, sib, qg, qgb, ol, olb, gg, ggb, S.dsem(), S.dsem(), S.dsem(), S.dsem()))
    sbf, sbfb = S.alloc(256)
    sq, sqb = S.alloc(2 * T)
    rstd, rb = S.alloc(T, F32)
    sg, sgb = S.alloc(2 * T, F32)
    outs = []
    for i in range(2):
        v, b = S.alloc(2 * T)
        outs.append((v, b, S.dsem()))
    for h in range(4):
        si, sib, qg, qgb, ol, olb, gg, ggb, s0, s1, s2, s3 = ld[h % 2]
        S.dma(S.sp, si, sfin_o[h * 128:(h + 1) * 128, :], s0, writes=[sib])
        S.dma(S.sp, qg, qgT[h * 128:(h + 1) * 128, :], s1, writes=[qgb])
        olv = ol.rearrange("p (e t) -> p e t", e=2)
        S.dma(S.sp, olv, oaloc[h * 256:(h + 1) * 256, :].rearrange("(e p) t -> p e t", p=128), s2, writes=[olb])
        ggv = gg.rearrange("p (e t) -> p e t", e=2)
        S.dma(S.sp, ggv, ggT[h * 256:(h + 1) * 256, :].rearrange("(e p) t -> p e t", p=128), s3, writes=[ggb])
        S.op(S.dve, lambda e: e.tensor_scalar(out=sbf, in0=si, scalar1=C.flag[:, 0:1], scalar2=None, op0=ALU.mult),
             reads=[sib], writes=[sbfb])
        for eh in range(2):
            for t in range(2):
                bank = eh * 2 + t
                mm(S, S.ps[bank][:, :], sbf[:, eh * 128:(eh + 1) * 128], qg[:, t * 512:(t + 1) * 512], True, True,
                   [sbfb, qgb], [S.psb[bank]])
                sl = slice(t * 512, (t + 1) * 512)
                S.op(S.dve, lambda e: e.tensor_tensor(out=olv[:, eh, sl], in0=S.ps[bank][:, :], in1=olv[:, eh, sl],
                                                      op=ALU.add), reads=[S.psb[bank], olb], writes=[olb])
        S.op(S.act, lambda e: e.activation(out=sq, in_=ol, func=AF.Square), reads=[olb], writes=[sqb])
        sqv = sq.rearrange("p (e t) -> p e t", e=2)
        for t in range(2):
            for eh in range(2):
                mm(S, S.ps[4 + t][:, :], C.ones, sqv[:, eh, t * 512:(t + 1) * 512], eh == 0, eh == 1, [sqb], [S.psb[4 + t]])
            S.op(S.dve, lambda e: e.tensor_scalar(out=rstd[:, t * 512:(t + 1) * 512], in0=S.ps[4 + t][:, :], scalar1=c256,
                                                  scalar2=EPS, op0=ALU.mult, op1=ALU.add), reads=[S.psb[4 + t]],
                 writes=[rb])
        S.op(S.act, lambda e: e.activation(out=rstd, in_=rstd, func=AF.Sqrt), reads=[rb], writes=[rb])
        S.op(S.dve, lambda e: e.reciprocal(out=rstd, in_=rstd), reads=[rb], writes=[rb])
        S.op(S.act, lambda e: e.activation(out=sg, in_=gg, func=AF.Silu), reads=[ggb], writes=[sgb])
        sgv = sg.rearrange("p (e t) -> p e t", e=2)
        ov, ob2, osem = outs[h % 2]
        ovv = ov.rearrange("p (e t) -> p e t", e=2)
        for eh in range(2):
            S.op(S.dve, lambda e: e.scalar_tensor_tensor(out=olv[:, eh, :], in0=olv[:, eh, :],
                                                         scalar=C.gains[:, gc["gla_gain"] + eh:gc["gla_gain"] + eh + 1],
                                                         in1=rstd, op0=ALU.mult, op1=ALU.mult), reads=[olb, rb],
                 writes=[olb])
            S.op(S.pool, lambda e: e.tensor_tensor(out=ovv[:, eh, :], in0=olv[:, eh, :], in1=sgv[:, eh, :], op=ALU.mult),
                 reads=[olb, sgb], writes=[ob2])
        S.dma(S.sp, oT[h * 256:(h + 1) * 256, :].rearrange("(e p) t -> p e t", p=128), ovv, osem, reads=[ob2])


def phase_sb(S, C, io, sqT, skT, svTM, skT_o, svTM_o, oT):
    c = 128 ** -0.5
    msk, mskb = S.alloc(4 * 512)
    msem = S.dsem()
    S.dma(S.sp, msk.rearrange("p (r t) -> p r t", r=4), io["c_sbmask"], msem, writes=[mskb])
    mview = msk.rearrange("p (r t) -> p r t", r=4)
    ld = []
    for i in range(2):
        q, qb = S.alloc(T)
        k, kb = S.alloc(T)
        ko, kob = S.alloc(T)
        v, vb = S.alloc(T)
        vo, vob = S.alloc(T)
        ld.append((q, qb, k, kb, ko, kob, v, vb, vo, vob, [S.dsem() for _ in range(5)]))
    NB = 10
    ZB = [0, 1, 4]
    AB = [2, 3, 5]
    e1 = [S.alloc(512, F32) for _ in range(NB)]
    sp = [S.alloc(512, F32) for _ in range(NB)]
    mt = [S.alloc(512) for _ in range(NB)]
    ms = [S.alloc(512) for _ in range(5)]
    tt_ = [S.alloc(512, F32) for _ in range(NB)]
    wt = [S.alloc(512) for _ in range(NB)]
    ost = []
    for i in range(2):
        v_, b_ = S.alloc(512)
        ost.append((v_, b_, S.dsem()))
    descs = []
    units = []
    for h in range(8):
        for tt in range(2):
            blocks = [("own", g) for g in range(4 * tt + 3, -1, -1)] + [("oth", g) for g in range(7, -1, -1)]
            u = len(units)
            units.append((h, tt, len(blocks)))
            for bidx, (kind, g) in enumerate(blocks):
                descs.append((u, h, tt, bidx, len(blocks), kind, g))
    N = len(descs)

    def views(h):
        q, qb, k, kb, ko, kob, v, vb, vo, vob, sems = ld[h % 2]
        return (q, qb, k, kb, ko, kob, v.rearrange("p (b d) -> p b d", b=8), vb,
                vo.rearrange("p (b d) -> p b d", b=8), vob, vo, sems)

    def load_head(h):
        q, qb, k, kb, ko, kob, vv, vb, vov, vob, vo, sems = views(h)
        S.dma(S.sp, q, sqT[h * 128:(h + 1) * 128, :], sems[0], writes=[qb])
        S.dma(S.sp, k, skT[h * 128:(h + 1) * 128, :], sems[1], writes=[kb])
        S.dma(S.sp, ko, skT_o[h * 128:(h + 1) * 128, :], sems[2], writes=[kob])
        S.dma(S.sp, vv, svTM[:, h * 128:(h + 1) * 128].rearrange("(b p) d -> p b d", p=128), sems[3], writes=[vb])
        S.dma(S.sp, vov, svTM_o[:, h * 128:(h + 1) * 128].rearrange("(b p) d -> p b d", p=128), sems[4], writes=[vob])

    def info(gi):
        u, h, tt, bidx, nblk, kind, g = descs[gi]
        masked = kind == "own" and g >= 4 * tt
        return u, h, tt, bidx, nblk, kind, g, masked, g - 4 * tt, gi % NB, gi % 3

    def stage1(gi):
        u, h, tt, bidx, nblk, kind, g, masked, r, si, zi = info(gi)
        q, qb, k, kb, ko, kob, vv, vb, vov, vob, vo, sems = views(h)
        if tt == 0 and bidx == 0:
            S.op(S.act, lambda e: e.mul(out=vo, in_=vo, mul=C.flag[:, 0:1]), reads=[vob], writes=[vob])
            if h + 1 < 8:
                load_head(h + 1)
        tsl = slice(tt * 512, (tt + 1) * 512)
        kk, kkb = (k, kb) if kind == "own" else (ko, kob)
        e_, eb_ = e1[si]
        s_, sb_ = sp[si]
        m_, mb_ = mt[si]
        zb = ZB[zi]
        mm(S, S.ps[zb][:, :], kk[:, g * 128:(g + 1) * 128], q[:, tsl], True, True, [kkb, qb], [S.psb[zb]])
        S.op(S.act, lambda e: e.activation(out=e_, in_=S.ps[zb][:, :], func=AF.Exp, scale=-c), reads=[S.psb[zb]],
             writes=[eb_])
        S.op(S.act, lambda e: e.activation(out=s_, in_=e_, func=AF.Ln, bias=1.0), reads=[eb_], writes=[sb_])
        S.op(S.dve, lambda e: e.scalar_tensor_tensor(out=m_, in0=S.ps[zb][:, :], scalar=c, in1=s_, op0=ALU.mult,
                                                     op1=ALU.add), reads=[S.psb[zb], sb_], writes=[mb_])
        if masked:
            S.op(S.pool, lambda e: e.tensor_tensor(out=m_, in0=m_, in1=mview[:, r, :], op=ALU.mult),
                 reads=[mb_, mskb], writes=[mb_])
        if bidx < nblk - 1:
            nm = ms[gi % 5]
            if bidx == 0:
                S.op(S.pool, lambda e: e.tensor_copy(out=nm[0], in_=m_), reads=[mb_], writes=[nm[1]])
            else:
                pm = ms[(gi - 1) % 5]
                S.op(S.pool, lambda e: e.tensor_tensor(out=nm[0], in0=pm[0], in1=m_, op=ALU.add),
                     reads=[pm[1], mb_], writes=[nm[1]])

    def stage2a(gi):
        u, h, tt, bidx, nblk, kind, g, masked, r, si, zi = info(gi)
        ab = AB[zi]
        s_, sb_ = sp[si]
        m_, mb_ = mt[si]
        t_, tb_ = tt_[si]
        mm(S, S.ps[ab][:, :], C.ustrict, m_, True, bidx == 0, [mb_], [S.psb[ab]])
        if bidx > 0:
            pm = ms[(gi - 1) % 5]
            mm(S, S.ps[ab][:, :], C.ones, pm[0], False, True, [pm[1]], [S.psb[ab]])
        S.op(S.dve, lambda e: e.tensor_tensor(out=t_, in0=S.ps[ab][:, :], in1=s_, op=ALU.add),
             reads=[S.psb[ab], sb_], writes=[tb_])

    def stage2b(gi):
        u, h, tt, bidx, nblk, kind, g, masked, r, si, zi = info(gi)
        t_, tb_ = tt_[si]
        w_, wb_ = wt[si]
        S.op(S.act, lambda e: e.activation(out=w_, in_=t_, func=AF.Exp, scale=-1.0), reads=[tb_], writes=[wb_])
        if masked:
            S.op(S.pool, lambda e: e.tensor_tensor(out=w_, in0=w_, in1=mview[:, r, :], op=ALU.mult),
                 reads=[wb_, mskb], writes=[wb_])

    def stage3(gi):
        u, h, tt, bidx, nblk, kind, g, masked, r, si, zi = info(gi)
        q, qb, k, kb, ko, kob, vv, vb, vov, vob, vo, sems = views(h)
        vsrc, vsb = (vv, vb) if kind == "own" else (vov, vob)
        w_, wb_ = wt[si]
        obank = 6 + (u % 2)
        mm(S, S.ps[obank][:, :], vsrc[:, g, :], w_, bidx == 0, bidx == nblk - 1, [vsb, wb_], [S.psb[obank]])
        if bidx == nblk - 1:
            ov, ob2, osem = ost[u % 2]
            S.op(S.act, lambda e: e.copy(out=ov, in_=S.ps[obank][:, :]), reads=[S.psb[obank]], writes=[ob2])
            S.dma(S.sp, oT[2048 + h * 128:2048 + (h + 1) * 128, tt * 512:(tt + 1) * 512], ov, osem, reads=[ob2])

    load_head(0)
    for i in range(N + 7):
        if i < N:
            stage1(i)
        if 0 <= i - 1 < N:
            stage2a(i - 1)
        if 0 <= i - 5 < N:
            stage2b(i - 5)
        if 0 <= i - 7 < N:
            stage3(i - 7)


BIG = 1.0e30
OHW = 1152
NBIS = 26
TOPK_MODE = "bisect"


def phase_dsa(S, C, io, dqT, dkT, dvTM, iqT, ikT, iwTM, dkT_o, dvTM_o, ikT_o, rel_bias, gvec, oT, nc, co=None,
              low_mark=None):
    c = 128 ** -0.5
    wconst = (64 ** -0.5) * (32 ** -0.5)

    def tick(n):
        if co is not None:
            for _ in range(n):
                next(co, None)

    dsa_lo = S.aoff
    rb31, rb31b = S.alloc(8, F32)
    rbsem = S.dsem()
    S.dma(S.sp, rb31, rel_bias[31:32, :].partition_broadcast(128), rbsem, writes=[rb31b])
    iq, iqb = S.alloc(16 * T)
    iqv = iq.rearrange("p (a t) -> p a t", a=16)
    ik, ikb = S.alloc(2 * T)
    ik2, ik2b = S.alloc(2 * T)
    iw, iwb = S.alloc(8 * 32)
    wsc, wscb = S.alloc(8 * 32, F32)
    wscv = wsc.rearrange("p (b h) -> p b h", b=8)
    nbig, nbigb = S.alloc(1, F32)
    selT, selTb = S.alloc(16 * T)
    selTv = selT.rearrange("p (b t) -> p b t", b=16)
    s2 = [S.dsem() for _ in range(3)]
    S.op(S.dve, lambda e: e.memset(ik, 0.0), writes=[ikb])
    S.op(S.dve, lambda e: e.memset(ik2, 0.0), writes=[ik2b])
    for half in range(2):
        S.dma(S.sp, iqv[half * 64:(half + 1) * 64, :, :],
              iqT[half * 1024:(half + 1) * 1024, :].rearrange("(a d) t -> d a t", d=64), s2[0], writes=[iqb])
    S.dma(S.sp, ik[0:64, 0:T], ikT_o, s2[1], writes=[ikb])
    S.dma(S.sp, ik[0:64, T:2 * T], ikT, s2[1], writes=[ikb])
    ik2sem = S.dsem()
    S.dma(S.sp, ik2[64:128, 0:T], ikT_o, ik2sem, writes=[ik2b])
    S.dma(S.sp, ik2[64:128, T:2 * T], ikT, ik2sem, writes=[ik2b])
    S.dma(S.sp, iw.rearrange("p (b h) -> p b h", b=8), iwTM.rearrange("(b p) h -> p b h", p=128), s2[2], writes=[iwb])
    S.op(S.dve, lambda e: e.tensor_scalar(out=wsc, in0=iw, scalar1=wconst, scalar2=None, op0=ALU.mult), reads=[iwb],
         writes=[wscb])
    S.op(S.dve, lambda e: e.tensor_scalar(out=nbig, in0=C.flag, scalar1=-1.0, scalar2=BIG, op0=ALU.add, op1=ALU.mult),
         writes=[nbigb])
    S.op(S.pool, lambda e: e.memset(selT, 0.0), writes=[selTb])
    Is = [S.alloc(2 * T, F32) for _ in range(1)]
    dgm, dgmb = S.alloc(128, F32)
    dgn, dgnb = S.alloc(128, F32)
    dgsem = S.dsem()
    S.dma(S.sp, dgm, io["c_diagm"], dgsem, writes=[dgmb])
    S.dma(S.sp, dgn, io["c_diagn"], dgsem, writes=[dgnb])
    I2, I2b = S.alloc(2 * T, F32)
    Dt, Dtb = S.alloc(NBIS, F32)
    p2, p2b = S.alloc(NBIS, F32)
    p2sem = S.dsem()
    S.dma(S.sp, p2, io["c_pow2"], p2sem, writes=[p2b])
    rts = [S.alloc(512, F32) for _ in range(2)]
    m8, m8b = S.alloc(8, F32)
    thr, thrb = S.alloc(1, F32)
    sel, selb = S.alloc(2 * T)
    pi = 0
    ri_ = 0
    for j in range(8):
        L = T + (j + 1) * 128
        I, Ib = Is[0]
        S.op(S.dve, lambda e: e.memset(I[:, 0:L], 0.0), writes=[Ib])
        S.op(S.dve, lambda e: e.tensor_scalar(out=I[:, 0:T], in0=I[:, 0:T], scalar1=nbig[:, 0:1], scalar2=None,
                                              op0=ALU.add), reads=[nbigb, Ib], writes=[Ib])
        for hh in range(32):
            half, a = hh // 16, hh % 16
            chunks = []
            for c0 in range(0, L, 512):
                w = min(512, L - c0)
                bank = pi % 4
                pi += 1
                kt_, ktb_ = (ik, ikb) if half == 0 else (ik2, ik2b)
                mm(S, S.ps[bank][:, 0:w], iqv[:, a, j * 128:(j + 1) * 128], kt_[:, c0:c0 + w], True, True,
                   [iqb, ktb_], [S.psb[bank]])
                chunks.append((c0, w, bank))
            tick(len(chunks))
            for (c0, w, bank) in chunks:
                rt, rtb = rts[ri_ % 2]
                ri_ += 1
                S.op(S.act, lambda e: e.activation(out=rt[:, 0:w], in_=S.ps[bank][:, 0:w], func=AF.Relu),
                     reads=[S.psb[bank]], writes=[rtb])
                S.op(S.dve, lambda e: e.scalar_tensor_tensor(out=I[:, c0:c0 + w], in0=rt[:, 0:w],
                                                             scalar=wscv[:, j, hh:hh + 1], in1=I[:, c0:c0 + w],
                                                             op0=ALU.mult, op1=ALU.add), reads=[rtb, wscb, Ib],
                     writes=[Ib])
            tick(len(chunks))
        dg = I[:, T + j * 128:T + (j + 1) * 128]
        S.op(S.dve, lambda e: e.tensor_tensor(out=dg, in0=dg, in1=dgm, op=ALU.mult), reads=[Ib, dgmb], writes=[Ib])
        S.op(S.dve, lambda e: e.tensor_tensor(out=dg, in0=dg, in1=dgn, op=ALU.add), reads=[Ib, dgnb], writes=[Ib])
        if True:
            W1 = I2[:, 0:L]
            S.op(S.dve, lambda e: e.scalar_tensor_tensor(out=W1, in0=I[:, 0:L], scalar=-BIG / 2, in1=I[:, 0:L],
                                                         op0=ALU.is_ge, op1=ALU.mult), reads=[Ib], writes=[I2b])
            S.op(S.dve, lambda e: e.tensor_reduce(out=thr, in_=W1, axis=AX.X, op=ALU.min), reads=[I2b], writes=[thrb])
            S.op(S.dve, lambda e: e.reduce_max(out=m8[:, 1:2], in_=I[:, 0:L], axis=AX.X), reads=[Ib], writes=[m8b])
            S.op(S.dve, lambda e: e.tensor_tensor(out=m8[:, 2:3], in0=m8[:, 1:2], in1=thr, op=ALU.subtract),
                 reads=[m8b, thrb], writes=[m8b])
            S.op(S.dve, lambda e: e.tensor_scalar(out=Dt, in0=p2, scalar1=m8[:, 2:3], scalar2=None, op0=ALU.mult),
                 reads=[m8b, p2b], writes=[Dtb])
            for it in range(NBIS):
                S.op(S.dve, lambda e: e.tensor_tensor(out=m8[:, 3:4], in0=Dt[:, it:it + 1], in1=thr, op=ALU.add),
                     reads=[Dtb, thrb], writes=[m8b])
                S.op(S.dve, lambda e: e.tensor_scalar(out=sel[:, 0:L], in0=I[:, 0:L], scalar1=m8[:, 3:4], scalar2=None,
                                                      op0=ALU.is_ge, op1=ALU.add, accum_out=m8[:, 4:5]),
                     reads=[Ib, m8b], writes=[selb, m8b])
                S.op(S.dve, lambda e: e.tensor_scalar(out=m8[:, 5:6], in0=m8[:, 4:5], scalar1=255.5,
                                                      scalar2=Dt[:, it:it + 1], op0=ALU.is_ge, op1=ALU.mult),
                     reads=[m8b, Dtb], writes=[m8b])
                S.op(S.dve, lambda e: e.tensor_tensor(out=thr, in0=thr, in1=m8[:, 5:6], op=ALU.add),
                     reads=[thrb, m8b], writes=[thrb])
                tick(6)
        S.op(S.dve, lambda e: e.tensor_scalar(out=sel[:, 0:L], in0=I[:, 0:L], scalar1=thr[:, 0:1], scalar2=None,
                                              op0=ALU.is_ge), reads=[Ib, thrb], writes=[selb])
        nblk = 8 + j + 1
        for b0 in range(0, nblk, 4):
            nb_ = min(4, nblk - b0)
            bank = 4 + (b0 // 4) % 2
            pt = S.ps[bank][:, :].bitcast(BF16)
            for bb in range(nb_):
                S.op(S.pe, lambda e: e.transpose(out=pt[:, bb * 128:(bb + 1) * 128],
                                                 in_=sel[:, (b0 + bb) * 128:(b0 + bb + 1) * 128], identity=C.ident),
                     reads=[selb], writes=[S.psb[bank]])
            S.op(S.act, lambda e: e.copy(out=selTv[:, b0:b0 + nb_, j * 128:(j + 1) * 128],
                                         in_=pt[:, 0:nb_ * 128].rearrange("p (b t) -> p b t", b=nb_)),
                 reads=[S.psb[bank]], writes=[selTb])
    if co is not None:
        for _ in co:
            pass
    hi_mark = S.aoff
    if low_mark is not None:
        S.barrier()
        S.aoff = low_mark
    rbs, rbsb = S.alloc(8, F32)
    oh, ohb = S.alloc(OHW, F32)
    gvs, gvsb = S.alloc(OHW)
    eb, ebb = S.alloc(8 * 5 * 512)
    ebv = eb.rearrange("p (h r t) -> p h r t", h=8, r=5)
    sems = [S.dsem() for _ in range(5)]
    S.dma(S.sp, rbs[0:32, :], rel_bias, sems[0], writes=[rbsb])
    S.dma(S.sp, oh[0:32, :], io["c_oh"], sems[1], writes=[ohb])
    S.op(S.act, lambda e: e.activation(out=rbs[0:32, :], in_=rbs[0:32, :], func=AF.Exp), reads=[rbsb], writes=[rbsb])
    for i in range(3):
        mm(S, S.ps[i][0:8, 0:384], rbs[0:32, 0:8], oh[0:32, i * 384:(i + 1) * 384], True, True, [rbsb, ohb], [S.psb[i]])
        S.op(S.act, lambda e: e.copy(out=gvs[0:8, i * 384:(i + 1) * 384], in_=S.ps[i][0:8, 0:384]), reads=[S.psb[i]],
             writes=[gvsb])
    gvb = Buf()
    S.dma(S.sp, gvec, gvs[0:8, :], sems[3], reads=[gvsb], writes=[gvb])
    aid, aidb = S.alloc(128)
    S.dma(S.sp, aid, io["c_antiident"], sems[4], writes=[aidb])
    hk = []
    for i in range(3):
        v_, b_ = S.alloc(512)
        hk.append((v_, b_, S.dsem()))
    ti = 0
    for h in range(8):
        for ri in range(5):
            r = ri - 1
            src = bass.AP(gvec.tensor, h * OHW + 385 - 128 * r, [[1, 128], [1, 512]])
            hv, hb, hsem = hk[ti % 3]
            bank = 4 + (ti % 4)
            ti += 1
            S.dma(S.sp, hv, src, hsem, reads=[gvb], writes=[hb])
            mm(S, S.ps[bank][:, :], aid, hv, True, True, [aidb, hb], [S.psb[bank]])
            S.op(S.act, lambda e: e.copy(out=ebv[:, h, ri, :], in_=S.ps[bank][:, :]), reads=[S.psb[bank]], writes=[ebb])
    dk, dkb = S.alloc(2 * T)
    dv, dvb = S.alloc(2 * T)
    dvv = dv.rearrange("p (b d) -> p b d", b=16)
    s3 = [S.dsem() for _ in range(2)]
    S.dma(S.sp, dk[:, 0:T], dkT_o, s3[0], writes=[dkb])
    S.dma(S.sp, dk[:, T:2 * T], dkT, s3[0], writes=[dkb])
    S.dma(S.sp, dvv[:, 0:8, :], dvTM_o.rearrange("(b p) d -> p b d", p=128), s3[1], writes=[dvb])
    S.dma(S.sp, dvv[:, 8:16, :], dvTM.rearrange("(b p) d -> p b d", p=128), s3[1], writes=[dvb])
    S.op(S.act, lambda e: e.mul(out=dv[:, 0:T], in_=dv[:, 0:T], mul=C.flag[:, 0:1]), reads=[dvb], writes=[dvb])
    qs = []
    for i in range(2):
        v, b = S.alloc(T)
        qs.append((v, b, S.dsem()))
    pts = [S.alloc(512) for _ in range(8)]
    rec, recb = S.alloc(512, F32)
    ost = []
    for i in range(2):
        v_, b_ = S.alloc(512)
        ost.append((v_, b_, S.dsem()))
    if low_mark is not None:
        assert S.aoff <= dsa_lo, (S.aoff, dsa_lo)
        S.aoff = hi_mark
    bi = 0
    oi = 0
    for h in range(8):
        q, qb, qsem = qs[h % 2]
        S.dma(S.sp, q, dqT[h * 128:(h + 1) * 128, :], qsem, writes=[qb])
        for tt in range(2):
            tsl = slice(tt * 512, (tt + 1) * 512)
            blocks = [("oth", g) for g in range(8)] + [("own", g) for g in range(4 * tt + 4)]
            nblk = len(blocks)
            ob_ = 2 + (oi % 2)
            db_ = 4 + (oi % 2)
            base = bi
            bi += nblk

            def stage1(bidx):
                kind, g = blocks[bidx]
                bix = g if kind == "oth" else 8 + g
                r = g - 4 * tt if kind == "own" else g - 8 - 4 * tt
                near = r >= -1
                lb = (base + bidx) % 2
                p_, pb_ = pts[(base + bidx) % 8]
                mm(S, S.ps[lb][:, :], dk[:, bix * 128:(bix + 1) * 128], q[:, tsl], True, True, [dkb, qb], [S.psb[lb]])
                if near:
                    S.op(S.act, lambda e: e.activation(out=p_, in_=S.ps[lb][:, :], func=AF.Exp, scale=c),
                         reads=[S.psb[lb]], writes=[pb_])
                    S.op(S.pool, lambda e: e.tensor_tensor(out=p_, in0=p_, in1=ebv[:, h, r + 1, :], op=ALU.mult),
                         reads=[pb_, ebb], writes=[pb_])
                else:
                    S.op(S.act, lambda e: e.activation(out=p_, in_=S.ps[lb][:, :], func=AF.Exp, scale=c,
                                                       bias=rb31[:, h:h + 1]), reads=[S.psb[lb], rb31b], writes=[pb_])
                S.op(S.dve, lambda e: e.tensor_tensor(out=p_, in0=p_, in1=selTv[:, bix, tsl], op=ALU.mult),
                     reads=[pb_, selTb], writes=[pb_])

            def stage2(bidx):
                kind, g = blocks[bidx]
                bix = g if kind == "oth" else 8 + g
                p_, pb_ = pts[(base + bidx) % 8]
                mm(S, S.ps[ob_][:, :], dvv[:, bix, :], p_, bidx == 0, bidx == nblk - 1, [dvb, pb_], [S.psb[ob_]])
                mm(S, S.ps[db_][:, :], C.onesf if kind == "oth" else C.ones, p_, bidx == 0, bidx == nblk - 1, [pb_],
                   [S.psb[db_]])

            for i in range(nblk + 4):
                if i < nblk:
                    stage1(i)
                if 0 <= i - 4 < nblk:
                    stage2(i - 4)
            ov, ob2, osem = ost[oi % 2]
            oi += 1
            S.op(S.dve, lambda e: e.reciprocal(out=rec, in_=S.ps[db_][:, :]), reads=[S.psb[db_]], writes=[recb])
            S.op(S.dve, lambda e: e.tensor_tensor(out=ov, in0=S.ps[ob_][:, :], in1=rec, op=ALU.mult),
                 reads=[S.psb[ob_], recb], writes=[ob2])
            S.dma(S.sp, oT[1024 + h * 128:1024 + (h + 1) * 128, tsl], ov, osem, reads=[ob2])


R_FM = 6144
FM_ROWS = dict(gq=0, gk=512, gg=1024, dq=2048, iq=3072, sq=5120)
TM_COLS = dict(gv=0, iw=1024)
TM_W = 1056
PAIRS = [[0, 1], [2, 3], [4, 5], [6, 7]]

CONST_SPECS = dict(
    c_ones=([128, 128], BF16), c_ident=([128, 128], BF16), c_ustrict=([128, 128], BF16), flag=([128, 1], F32),
    gains=([128, 2 * GC_PER_LAYER], F32), c_resetmask=([128, T], F32), c_onesrow=([128, T], F32),
    c_causal64=([64, 64], F32), c_sbmask=([128, 4, 512], BF16), c_oh=([32, OHW], F32),
    c_antiident=([128, 128], BF16), c_pow2=([128, NBIS], F32),
    c_diagm=([128, 128], F32), c_diagn=([128, 128], F32),
)


def host_consts():
    bf = ml_dtypes.bfloat16
    c = {}
    c["c_ones"] = np.ones((128, 128), bf)
    c["c_ident"] = np.eye(128, dtype=np.float32).astype(bf)
    j = np.arange(128)[:, None]
    s = np.arange(128)[None, :]
    c["c_ustrict"] = (j > s).astype(np.float32).astype(bf)
    t = np.arange(T)
    c["c_resetmask"] = np.broadcast_to((t % 64 != 0).astype(np.float32)[None, :], (128, T)).copy()
    c["c_onesrow"] = np.ones((128, T), np.float32)
    jj = np.arange(64)[:, None]
    ii = np.arange(64)[None, :]
    c["c_causal64"] = (jj <= ii).astype(np.float32)
    r = np.arange(4)[None, :, None]
    sp = np.arange(128)[:, None, None]
    tp = np.arange(512)[None, None, :]
    c["c_sbmask"] = ((r * 128 + sp) < tp).astype(np.float32).astype(bf)
    dist = np.arange(OHW) - 512
    d = np.maximum(dist, 1).astype(np.float32)
    large = 16 + (np.log(d / 16) / np.log(128 / 16) * 16).astype(np.int32)
    large = np.minimum(large, 31)
    bucket = np.where(dist < 16, np.maximum(dist, 0), large)
    oh = np.zeros((32, OHW), np.float32)
    for dd in range(OHW):
        if dist[dd] >= 0:
            oh[bucket[dd], dd] = 1.0
    c["c_oh"] = oh
    c["c_antiident"] = np.eye(128, dtype=np.float32)[::-1].copy().astype(bf)
    tq = np.arange(128)[:, None]
    sq_ = np.arange(128)[None, :]
    c["c_diagm"] = (sq_ <= tq).astype(np.float32)
    c["c_diagn"] = np.where(sq_ <= tq, 0.0, -BIG).astype(np.float32)
    c["c_pow2"] = np.broadcast_to((0.5 ** np.arange(1, NBIS + 1)).astype(np.float32)[None, :], (128, NBIS)).copy()
    return c


def host_gains(inp):
    g = np.zeros((128, 2 * GC_PER_LAYER), np.float32)
    for l in range(2):
        gc = gcols(l)
        for nm, key in [("mix_pre", "norm_mix_pre"), ("mix_post", "norm_mix_post"), ("mlp_pre", "norm_mlp_pre"),
                        ("mlp_post", "norm_mlp_post")]:
            g[:, gc[nm]:gc[nm] + 32] = np.asarray(inp[key][l], np.float32).reshape(32, 128).T
        g[:, gc["gla_gain"]:gc["gla_gain"] + 2] = np.asarray(inp["gla_head_gain"][l], np.float32).reshape(2, 128).T
        g[:, gc["gla_bias"]:gc["gla_bias"] + 4] = np.asarray(inp["gla_gate_bias"][l], np.float32).reshape(4, 128).T
    return g


class Prog:
    def __init__(self):
        self.nc = bass.Bass("TRN2", target_bir_lowering=False)
        self.t = {}

    def dram(self, name, shape, dtype, kind):
        self.t[name] = self.nc.dram_tensor(name, list(shape), dtype, kind=kind).ap()
        return self.t[name]


def declare_consts(P):
    io = {}
    for k, (shp, dt_) in CONST_SPECS.items():
        io[k] = P.dram(k, shp, dt_, "ExternalInput")
    return io


def issue_collectives(S, nc, items):
    sem = S.xsems[0]
    for (src, dst) in items:
        ins = nc.gpsimd.collective_compute("AllGather", ALU.bypass, replica_groups=PAIRS, ins=[src.opt()],
                                           outs=[dst.opt()])
        sem.count += 1
        ins.then_inc(sem.h, 1)


def emit_layer(S, C, io, nc, l, xT, xoutT, w_in, gate_up, rel_bias, w_branch, w_out, w_up, w_down, sc):
    gc = gcols(l)
    qkT, skT, dkik, svt, dvt, gaT, tm = sc["qkT"], sc["skT"], sc["dkik"], sc["svt"], sc["dvt"], sc["gaT"], sc["tm"]
    gatesT, qgT, oaloc, sfin = sc["gatesT"], sc["qgT"], sc["oaloc"], sc["sfin"]
    phase_begin(S)
    hT, hB = S.alloc(KC * T)
    mark = S.aoff
    phase_norm(S, C, xT, gc["mix_pre"], hT, hB)
    S.barrier()
    S.aoff = mark
    segs = []
    for nm in ["gq", "gk", "gg", "dq", "iq", "sq"]:
        c0, w = SEG[nm]
        segs.append((c0, w, qkT[FM_ROWS[nm]:FM_ROWS[nm] + w, :], "copy"))
    segs.append((SEG["sk"][0], 1024, skT, "copy"))
    segs.append((SEG["dk"][0], 128, dkik[0:128, :], "copy"))
    segs.append((SEG["ik"][0], 64, dkik[128:192, :], "copy"))
    segs.append((SEG["ga"][0], 16, gaT, "f32"))
    phase_proj_fm(S, C, hT, hB, w_in, segs)
    S.barrier()
    S.aoff = mark
    tsegs = []
    for nm in ["gv", "iw"]:
        c0, w = SEG[nm]
        tsegs.append((c0, w, tm[:, TM_COLS[nm]:TM_COLS[nm] + w]))
    tsegs.append((SEG["sv"][0], 1024, svt))
    tsegs.append((SEG["dv"][0], 128, dvt))
    phase_proj_tm(S, C, hT, hB, w_in, tsegs)
    S.barrier()
    issue_collectives(S, nc, [(sc[k], sc[k + "_g"]) for k in ["skT", "dkik", "svt", "dvt"]])
    S.aoff = mark
    phase_gla_local(S, C, io, qkT, gaT, tm[:, 0:1024], gate_up, gc, qgT, oaloc, sfin)
    S.barrier()
    issue_collectives(S, nc, [(sc["sfin"], sc["sfin_g"])])
    S.barrier()
    S.aoff = mark
    skT_o, dkik_o, svt_o, dvt_o = sc["skT_g"][0:1024, :], sc["dkik_g"][0:192, :], sc["svt_g"][0:T, :], sc["dvt_g"][0:T, :]
    sfin_o = sc["sfin_g"][0:512, :]
    oT, mT, yT, x1T, uT = sc["oT"], sc["mT"], sc["yT"], sc["x1T"], sc["uT"]
    co = make_gates_co(S, C, hT, hB, w_in, SEG["gates"][0], 12288, gatesT)
    phase_dsa(S, C, io, qkT[FM_ROWS["dq"]:FM_ROWS["dq"] + 1024, :], dkik[0:128, :], dvt,
              qkT[FM_ROWS["iq"]:FM_ROWS["iq"] + 2048, :], dkik[128:192, :], tm[:, TM_COLS["iw"]:TM_COLS["iw"] + 32],
              dkik_o[0:128, :], dvt_o, dkik_o[128:192, :], rel_bias, sc["gvec"], oT, nc, co=co, low_mark=S.abase)
    phase_begin(S)
    phase_sb(S, C, io, qkT[FM_ROWS["sq"]:FM_ROWS["sq"] + 1024, :], skT, svt, skT_o, svt_o, oT)
    phase_begin(S)
    phase_gla_fin(S, C, sfin_o, qgT, oaloc, qkT[FM_ROWS["gg"]:FM_ROWS["gg"] + 1024, :], gc, oT)
    phase_begin(S)
    phase_merge(S, C, oT, gatesT, w_branch, mT)
    phase_begin(S)
    phase_linear_resid(S, C, mT, w_out, D, yT, xT, x1T, gc["mix_post"])
    phase_begin(S)
    hT, hB = S.alloc(KC * T)
    mark = S.aoff
    phase_norm(S, C, x1T, gc["mlp_pre"], hT, hB)
    S.barrier()
    S.aoff = mark
    phase_proj_fm(S, C, hT, hB, w_up, [(0, DFF, uT, "relu2")])
    phase_begin(S)
    phase_linear_resid(S, C, uT, w_down, DFF, yT, x1T, xoutT, gc["mlp_post"])


SCRATCH = dict(qkT=([R_FM, T], BF16), skT=([1024, T], BF16), dkik=([192, T], BF16), svt=([T, 1024], BF16),
               dvt=([T, 128], BF16), gaT=([16, T], F32), tm=([T, TM_W], BF16),
               gatesT=([12288, T], BF16), qgT=([512, T], BF16), oaloc=([1024, T], F32), sfin=([512, 256], F32),
               skT_g=([2048, T], BF16), dkik_g=([384, T], BF16), svt_g=([2 * T, 1024], BF16), dvt_g=([2 * T, 128], BF16),
               sfin_g=([1024, 256], F32),
               gvec=([8, OHW], BF16), oT=([3072, T], BF16), mT=([D, T], BF16), yT=([D, T], F32), x1T=([D, T], F32),
               uT=([DFF, T], BF16), x2T=([D, T], F32))


def build_full():
    P = Prog()
    nc = P.nc
    io = declare_consts(P)
    xT = P.dram("xT", [D, T], F32, "ExternalInput")
    w_in = P.dram("w_in", [2, D, IN_COLS], F32, "ExternalInput")
    gate_up = P.dram("gate_up", [2, 16, 512], F32, "ExternalInput")
    rel_bias = P.dram("rel_bias", [32, 8], F32, "ExternalInput")
    w_branch = P.dram("w_branch", [2, 3, 1024, D], F32, "ExternalInput")
    w_out = P.dram("w_out", [2, D, D], F32, "ExternalInput")
    w_up = P.dram("w_up", [2, D, DFF], F32, "ExternalInput")
    w_down = P.dram("w_down", [2, DFF, D], F32, "ExternalInput")
    sc = {k: P.dram(k, shp, dt_, "Internal") for k, (shp, dt_) in SCRATCH.items()}
    xoutT = P.dram("xoutT", [D, T], F32, "ExternalOutput")
    with ExitStack() as st:
        S = Sched(nc, st)
        C = setup_consts(S, nc, io)
        xin = xT
        for l in range(2):
            xo = sc["x2T"] if l == 0 else xoutT
            emit_layer(S, C, io, nc, l, xin, xo, w_in[l], gate_up[l], rel_bias, [w_branch[l, i] for i in range(3)],
                       w_out[l], w_up[l], w_down[l], sc)
            xin = sc["x2T"]
        S.finish()
    return nc


def kernel(**inputs):
    x = np.asarray(inputs["x"], np.float32)
    consts = host_consts()
    consts["gains"] = host_gains(inputs)
    n = 8
    shared = dict(
        w_in=np.asarray(inputs["w_in"], np.float32), gate_up=np.asarray(inputs["gla_gate_up"], np.float32),
        rel_bias=np.asarray(inputs["rel_bias"], np.float32), w_branch=np.asarray(inputs["w_branch"], np.float32),
        w_out=np.asarray(inputs["w_out"], np.float32), w_up=np.asarray(inputs["w_mlp_up"], np.float32),
        w_down=np.asarray(inputs["w_mlp_down"], np.float32))
    in_maps = []
    for c in range(n):
        b, half = c // 2, c % 2
        m = dict(consts)
        m.update(shared)
        m["flag"] = np.full((128, 1), float(half), np.float32)
        m["xT"] = np.ascontiguousarray(x[b, half * T:(half + 1) * T, :].T)
        in_maps.append(m)
    nc = build_full()
    res = run_bass_kernel_spmd(nc, in_maps, core_ids=list(range(n))).results
    out = np.empty((4, 2048, D), np.float32)
    for c in range(n):
        b, half = c // 2, c % 2
        out[b, half * T:(half + 1) * T, :] = np.asarray(res[c]["xoutT"]).T
    return out
```

```python
import numpy as np
from contextlib import ExitStack
import concourse.bass as bass
import concourse.mybir as mybir
from concourse.bass_utils import run_bass_kernel_spmd
import ml_dtypes

F32 = mybir.dt.float32
BF16 = mybir.dt.bfloat16
AF = mybir.ActivationFunctionType
ALU = mybir.AluOpType
AX = mybir.AxisListType
SEM_WRAP = 30000

T = 1024
D = 4096
KC = D // 128
DFF = 16384
EPS = 1e-6
NDSEM = 56
ARENA_COLS = 105984

SEG = {}
_o = 0
for _n, _w in [("gq", 512), ("gk", 512), ("gv", 1024), ("gg", 1024), ("ga", 16), ("dq", 1024),
               ("dk", 128), ("dv", 128), ("iq", 2048), ("ik", 64), ("iw", 32), ("sq", 1024),
               ("sk", 1024), ("sv", 1024), ("gates", 12288)]:
    SEG[_n] = (_o, _w)
    _o += _w
IN_COLS = _o


class Sem:
    __slots__ = ("h", "idx", "count")

    def __init__(self, h, idx):
        self.h = h
        self.idx = idx
        self.count = 0


class Buf:
    __slots__ = ("name", "w", "r")

    def __init__(self, name=""):
        self.name = name
        self.w = None
        self.r = {}


class Eng:
    def __init__(self, S, name, eng, nsems):
        self.name = name
        self.eng = eng
        self.sems = [S.new_sem(f"{name}{i}") for i in range(nsems)]
        self.count = 0
        self.known = {}
        self.pending = False
        self.is_pe = name == "pe"

    def tag_next(self):
        c = self.count
        return (self.sems[c // SEM_WRAP], c % SEM_WRAP + 1)

    def tag_last(self):
        c = self.count - 1
        if c < 0:
            return None
        return (self.sems[c // SEM_WRAP], c % SEM_WRAP + 1)


class Sched:
    def __init__(self, nc, stack):
        self.nc = nc
        self.stack = stack
        self.nsem = 0
        self.pe = Eng(self, "pe", nc.tensor, 8)
        self.act = Eng(self, "act", nc.scalar, 4)
        self.dve = Eng(self, "dve", nc.vector, 6)
        self.pool = Eng(self, "pool", nc.gpsimd, 3)
        self.sp = Eng(self, "sp", nc.sync, 1)
        self.engs = [self.pe, self.act, self.dve, self.pool, self.sp]
        self.dsems = [self.new_sem(f"dma{i}") for i in range(NDSEM)]
        self.dsem_i = 0
        self.xsems = [self.new_sem("cc")]
        self.arena = stack.enter_context(nc.sbuf_tensor("arena", [128, ARENA_COLS], BF16))
        self.aoff = 0
        self.ps = []
        self.psb = []
        for i in range(8):
            t = stack.enter_context(nc.psum_tensor(f"psum{i}", [128, 512], F32))
            self.ps.append(t)
            self.psb.append(Buf(f"ps{i}"))

    def new_sem(self, name):
        h = self.stack.enter_context(self.nc.semaphore(name))
        s = Sem(h, self.nsem)
        self.nsem += 1
        return s

    def alloc(self, cols, dtype=BF16):
        n = cols * (2 if dtype == F32 else 1)
        n = (n + 15) // 16 * 16
        assert self.aoff + n <= ARENA_COLS, (self.aoff, n)
        v = self.arena[:, self.aoff:self.aoff + n]
        self.aoff += n
        if dtype == F32:
            v = v.bitcast(F32)
        if v.shape[1] != cols:
            v = v[:, 0:cols]
        return v, Buf()

    def dsem(self):
        s = self.dsems[self.dsem_i]
        self.dsem_i += 1
        assert self.dsem_i <= NDSEM
        return s

    def _wait(self, E, reads, writes):
        need = {}

        def add(tag):
            s, v = tag
            if need.get(s.idx, (None, 0))[1] < v:
                need[s.idx] = (s, v)

        for b in reads:
            if b.w is not None:
                add(b.w)
        for b in writes:
            if b.w is not None:
                add(b.w)
            for t in b.r.values():
                add(t)
        for idx, (s, v) in need.items():
            if E.is_pe and s in E.sems:
                continue
            if E.known.get(idx, 0) >= v:
                continue
            E.eng.wait_ge(s.h, v)
            E.known[idx] = v

    def _record(self, tag, reads, writes):
        s, v = tag
        for b in reads:
            old = b.r.get(s.idx)
            if old is None or old[1] < v:
                b.r[s.idx] = tag
        for b in writes:
            b.w = tag
            b.r = {}

    def op(self, E, fn, reads=(), writes=(), signal=True):
        self._wait(E, reads, writes)
        ins = fn(E.eng)
        tag = E.tag_next()
        if signal:
            ins.then_inc(tag[0].h, 1)
            E.count += 1
            E.pending = False
        else:
            E.pending = True
        self._record(tag, reads, writes)
        return ins

    def dma(self, Q, out, in_, sem, reads=(), writes=(), **kw):
        self._wait(Q, reads, writes)
        ins = Q.eng.dma_start(out=out, in_=in_, **kw)
        sem.count += 16
        ins.then_inc(sem.h, 16)
        self._record((sem, sem.count), reads, writes)
        return ins

    def barrier(self, skip_x=False):
        tags = []
        for E in self.engs:
            assert not E.pending, E.name
            t = E.tag_last()
            if t is not None:
                tags.append(t)
        for s in self.dsems + ([] if skip_x else self.xsems):
            if s.count > 0:
                tags.append((s, s.count))
        for E in self.engs:
            for (s, v) in tags:
                if s in E.sems:
                    continue
                if E.known.get(s.idx, 0) >= v:
                    continue
                E.eng.wait_ge(s.h, v)
                E.known[s.idx] = v
        self.aoff = 0
        self.dsem_i = 0
        for b in self.psb:
            b.w = None
            b.r = {}

    def finish(self):
        self.barrier()


def mm(S, out, lhsT, rhs, start, stop, reads, writes, sig=True):
    S.op(S.pe, lambda e: e.matmul(out, lhsT=lhsT, rhs=rhs, start=start, stop=stop),
         reads=reads, writes=writes, signal=(stop or sig))


class Consts:
    pass


def setup_consts(S, nc, io):
    C = Consts()
    C.ones, b0 = S.alloc(128)
    C.ident, b1 = S.alloc(128)
    C.ustrict, b2 = S.alloc(128)
    C.flag, b3 = S.alloc(1, F32)
    C.onesf, b4 = S.alloc(128)
    C.gains, b5 = S.alloc(io["gains"].shape[1], F32)
    sems = [S.dsem() for _ in range(5)]
    S.dma(S.sp, C.ones, io["c_ones"], sems[0], writes=[b0])
    S.dma(S.sp, C.ident, io["c_ident"], sems[1], writes=[b1])
    S.dma(S.sp, C.ustrict, io["c_ustrict"], sems[2], writes=[b2])
    S.dma(S.sp, C.flag, io["flag"], sems[3], writes=[b3])
    S.dma(S.sp, C.gains, io["gains"], sems[4], writes=[b5])
    S.op(S.dve, lambda e: e.tensor_scalar(out=C.onesf, in0=C.ones, scalar1=C.flag[:, 0:1], scalar2=None,
                                          op0=ALU.mult), reads=[b0, b3], writes=[b4])
    S.barrier()
    S.abase = S.aoff = (sum([128, 128, 128, 16, 128]) + io["gains"].shape[1] * 2 + 15) // 16 * 16
    return C


def phase_begin(S):
    S.barrier()
    S.aoff = S.abase


def phase_norm(S, C, xT, gcol, hT, hB):
    xs = []
    for i in range(3):
        v, b = S.alloc(T, F32)
        xs.append((v, b, S.dsem()))
    sq = [S.alloc(T) for _ in range(2)]
    rstd, rb = S.alloc(T, F32)
    ss = [S.ps[0], S.ps[1]]
    ssb = [S.psb[0], S.psb[1]]
    for c in range(KC):
        v, b, sem = xs[c % 3]
        S.dma(S.sp, v, xT[c * 128:(c + 1) * 128, :], sem, writes=[b])
        q, qb = sq[c % 2]
        S.op(S.act, lambda e: e.activation(out=q, in_=v, func=AF.Square), reads=[b], writes=[qb])
        for t in range(2):
            mm(S, ss[t][:, :], C.ones, q[:, t * 512:(t + 1) * 512], c == 0, c == KC - 1, [qb], [ssb[t]])
    for t in range(2):
        sl = slice(t * 512, (t + 1) * 512)
        S.op(S.dve, lambda e: e.tensor_scalar(out=rstd[:, sl], in0=ss[t][:, :], scalar1=1.0 / D, scalar2=EPS,
                                              op0=ALU.mult, op1=ALU.add), reads=[ssb[t]], writes=[rb])
    S.op(S.act, lambda e: e.activation(out=rstd, in_=rstd, func=AF.Sqrt), reads=[rb], writes=[rb])
    S.op(S.dve, lambda e: e.reciprocal(out=rstd, in_=rstd), reads=[rb], writes=[rb])
    for c in range(KC):
        v, b, sem = xs[c % 3]
        S.dma(S.sp, v, xT[c * 128:(c + 1) * 128, :], sem, writes=[b])
        S.op(S.dve, lambda e: e.scalar_tensor_tensor(out=hT[:, c * T:(c + 1) * T], in0=v,
                                                     scalar=C.gains[:, gcol + c:gcol + c + 1], in1=rstd,
                                                     op0=ALU.mult, op1=ALU.mult), reads=[b, rb], writes=[hB])


class Slabs:
    def __init__(self, S, nk, wmax, nbuf=2):
        self.S = S
        self.nk = nk
        self.wmax = wmax
        self.bufs = []
        for i in range(nbuf):
            v, b = S.alloc(nk * wmax)
            self.bufs.append((v, b, S.dsem()))
        self.i = 0

    def load(self, W, k0, c0, w, kstep=8):
        S = self.S
        v, b, sem = self.bufs[self.i % len(self.bufs)]
        self.i += 1
        view = v[:, 0:self.nk * w].rearrange("p (k c) -> p k c", k=self.nk)
        for ks in range(0, self.nk, kstep):
            ke = min(self.nk, ks + kstep)
            src = W[(k0 + ks) * 128:(k0 + ke) * 128, c0:c0 + w].rearrange("(k p) c -> p k c", p=128)
            S.dma(S.pool, view[:, ks:ke, :], src, sem, writes=[b])
        return view, b


def phase_proj_fm(S, C, hT, hB, W, segs, nk=KC):
    slabs = Slabs(S, nk, 512)
    stg = []
    for i in range(3):
        v, b = S.alloc(T)
        stg.append((v, b, S.dsem()))
    stgf = []
    for i in range(2):
        v, b = S.alloc(T, F32)
        stgf.append((v, b, S.dsem()))
    relu_tmp = [S.alloc(512, F32) for _ in range(2)]
    work = []
    for (c0, width, dst, epi) in segs:
        for s0 in range(0, width, 512):
            work.append((c0 + s0, min(512, width - s0), dst, s0, epi))
    pi = 0
    si = 0
    nxt = slabs.load(W, 0, work[0][0], work[0][1])
    for wi, (c0, w, dst, r0, epi) in enumerate(work):
        view, wb = nxt
        if wi + 1 < len(work):
            nxt = slabs.load(W, 0, work[wi + 1][0], work[wi + 1][1])
        for n0 in range(0, w, 128):
            m = min(128, w - n0)
            banks = [(pi * 2) % 8, (pi * 2 + 1) % 8]
            pi += 1
            for k in range(nk):
                for t in range(2):
                    mm(S, S.ps[banks[t]][0:m, :], view[:, k, n0:n0 + m], hT[:, k * T + t * 512:k * T + (t + 1) * 512],
                       k == 0, k == nk - 1, [wb, hB], [S.psb[banks[t]]], sig=False)
            if epi == "f32":
                v, b, sem = stgf[si % 2]
            else:
                v, b, sem = stg[si % 3]
            si += 1
            for t in range(2):
                o = v[0:m, t * 512:(t + 1) * 512]
                p = S.ps[banks[t]][0:m, :]
                pb = S.psb[banks[t]]
                if epi == "copy" or epi == "f32":
                    if t == 0:
                        S.op(S.act, lambda e: e.copy(out=o, in_=p), reads=[pb], writes=[b])
                    else:
                        S.op(S.dve, lambda e: e.tensor_copy(out=o, in_=p), reads=[pb], writes=[b])
                elif epi == "sigmoid":
                    S.op(S.act, lambda e: e.activation(out=o, in_=p, func=AF.Sigmoid), reads=[pb], writes=[b])
                elif epi == "relu2":
                    rt, rtb = relu_tmp[(si + t) % 2]
                    S.op(S.act, lambda e: e.activation(out=rt[0:m, :], in_=p, func=AF.Relu), reads=[pb], writes=[rtb])
                    S.op(S.dve, lambda e: e.tensor_tensor(out=o, in0=rt[0:m, :], in1=rt[0:m, :], op=ALU.mult),
                         reads=[rtb], writes=[b])
                else:
                    raise ValueError(epi)
            S.dma(S.sp, dst[r0 + n0:r0 + n0 + m, :], v[0:m, :], sem, reads=[b])


def make_gates_co(S, C, hT, hB, W, c0, width, dst, banks=(6, 7), sw=256):
    slabs = Slabs(S, KC, sw)
    stg = []
    for i in range(3):
        v, b = S.alloc(T)
        stg.append((v, b, S.dsem()))

    def gen():
        work = [(c0 + s0, min(sw, width - s0), s0) for s0 in range(0, width, sw)]
        nxt = slabs.load(W, 0, work[0][0], work[0][1])
        si = 0
        for wi, (cc, w, r0) in enumerate(work):
            view, wb = nxt
            if wi + 1 < len(work):
                nxt = slabs.load(W, 0, work[wi + 1][0], work[wi + 1][1])
            for n0 in range(0, w, 128):
                for k in range(KC):
                    for t in range(2):
                        mm(S, S.ps[banks[t]][:, :], view[:, k, n0:n0 + 128], hT[:, k * T + t * 512:k * T + (t + 1) * 512],
                           k == 0, k == KC - 1, [wb, hB], [S.psb[banks[t]]], sig=False)
                    yield
                v, b, sem = stg[si % 3]
                si += 1
                for t in range(2):
                    S.op(S.act, lambda e: e.activation(out=v[:, t * 512:(t + 1) * 512], in_=S.ps[banks[t]][:, :],
                                                       func=AF.Sigmoid), reads=[S.psb[banks[t]]], writes=[b])
                S.dma(S.sp, dst[r0 + n0:r0 + n0 + 128, :], v, sem, reads=[b])
                yield
    return gen()


def phase_proj_tm(S, C, hT, hB, W, segs):
    slabs = Slabs(S, KC, 512)
    stg = []
    for i in range(3):
        v, b = S.alloc(512)
        stg.append((v, b, S.dsem()))
    work = []
    for (c0, width, dst) in segs:
        for s0 in range(0, width, 512):
            work.append((c0 + s0, min(512, width - s0), dst, s0))
    pi = 0
    si = 0
    nxt = slabs.load(W, 0, work[0][0], work[0][1])
    for wi, (c0, w, dst, r0) in enumerate(work):
        view, wb = nxt
        if wi + 1 < len(work):
            nxt = slabs.load(W, 0, work[wi + 1][0], work[wi + 1][1])
        for tb in range(T // 128):
            bank = pi % 8
            pi += 1
            for k in range(KC):
                mm(S, S.ps[bank][:, 0:w], hT[:, k * T + tb * 128:k * T + (tb + 1) * 128], view[:, k, :],
                   k == 0, k == KC - 1, [wb, hB], [S.psb[bank]], sig=False)
            v, b, sem = stg[si % 3]
            si += 1
            if tb % 2 == 0:
                S.op(S.act, lambda e: e.copy(out=v[:, 0:w], in_=S.ps[bank][:, 0:w]), reads=[S.psb[bank]], writes=[b])
            else:
                S.op(S.dve, lambda e: e.tensor_copy(out=v[:, 0:w], in_=S.ps[bank][:, 0:w]), reads=[S.psb[bank]],
                     writes=[b])
            S.dma(S.sp, dst[tb * 128:(tb + 1) * 128, r0:r0 + w], v[:, 0:w], sem, reads=[b])


def phase_linear_resid(S, C, inT, W, K, yT, x_src, x_dst, gcol):
    N = D
    kch = K // 128
    FG = 16
    nfg = kch // FG
    NG = 4
    wsl = []
    for i in range(2):
        v, b = S.alloc(FG * NG * 128)
        wsl.append((v, b, S.dsem()))
    usl = []
    for i in range(2):
        v, b = S.alloc(FG * T)
        usl.append((v, b, S.dsem()))
    ystg = []
    for i in range(2):
        v, b = S.alloc(T, F32)
        ystg.append((v, b, S.dsem()))
    sq = [S.alloc(T, F32) for _ in range(2)]
    SQ, SQb = S.alloc(T, F32)
    SQh, SQhb = S.alloc(T)
    S.op(S.dve, lambda e: e.memset(SQ, 0.0), writes=[SQb])
    ss = [S.ps[0], S.ps[1]]
    ssb = [S.psb[0], S.psb[1]]
    groups = []
    n = 0
    nch = N // 128
    while n < nch:
        g = min(NG, nch - n)
        groups.append((n, g))
        n += g
    li = 0
    yi = 0
    for (n0, g) in groups:
        for fg in range(nfg):
            wv, wb, wsem = wsl[li % 2]
            uv, ub, usem = usl[li % 2]
            li += 1
            wview = wv[:, 0:FG * g * 128].rearrange("p (k c) -> p k c", k=FG)
            for ks in range(0, FG, 8):
                src = W[(fg * FG + ks) * 128:(fg * FG + ks + 8) * 128, n0 * 128:(n0 + g) * 128].rearrange(
                    "(k p) c -> p k c", p=128)
                S.dma(S.pool, wview[:, ks:ks + 8, :], src, wsem, writes=[wb])
            uview = uv.rearrange("p (k t) -> p k t", k=FG)
            for ks in range(0, FG, 8):
                src = inT[(fg * FG + ks) * 128:(fg * FG + ks + 8) * 128, :].rearrange("(k p) t -> p k t", p=128)
                S.dma(S.sp, uview[:, ks:ks + 8, :], src, usem, writes=[ub])
            for j in range(g):
                for k in range(FG):
                    for t in range(2):
                        bank = j * 2 + t
                        mm(S, S.ps[bank][:, :], wview[:, k, j * 128:(j + 1) * 128], uview[:, k, t * 512:(t + 1) * 512],
                           fg == 0 and k == 0, fg == nfg - 1 and k == FG - 1, [wb, ub], [S.psb[bank]],
                           sig=(k == FG - 1 and j == g - 1 and t == 1))
        for j in range(g):
            v, b, sem = ystg[yi % 2]
            q, qb = sq[yi % 2]
            yi += 1
            for t in range(2):
                bank = j * 2 + t
                sl = slice(t * 512, (t + 1) * 512)
                S.op(S.act, lambda e: e.copy(out=v[:, sl], in_=S.ps[bank][:, :]), reads=[S.psb[bank]], writes=[b])
                S.op(S.dve, lambda e: e.tensor_tensor(out=q[:, sl], in0=S.ps[bank][:, :], in1=v[:, sl], op=ALU.mult),
                     reads=[S.psb[bank], b], writes=[qb])
            S.op(S.dve, lambda e: e.tensor_tensor(out=SQ, in0=SQ, in1=q, op=ALU.add), reads=[SQb, qb], writes=[SQb])
            S.dma(S.sp, yT[(n0 + j) * 128:(n0 + j + 1) * 128, :], v, sem, reads=[b])
    S.op(S.act, lambda e: e.copy(out=SQh, in_=SQ), reads=[SQb], writes=[SQhb])
    for t in range(2):
        mm(S, ss[t][:, :], C.ones, SQh[:, t * 512:(t + 1) * 512], True, True, [SQhb], [ssb[t]])
    rstd, rb = S.alloc(T, F32)
    for t in range(2):
        sl = slice(t * 512, (t + 1) * 512)
        S.op(S.dve, lambda e: e.tensor_scalar(out=rstd[:, sl], in0=ss[t][:, :], scalar1=1.0 / D, scalar2=EPS,
                                              op0=ALU.mult, op1=ALU.add), reads=[ssb[t]], writes=[rb])
    S.op(S.act, lambda e: e.activation(out=rstd, in_=rstd, func=AF.Sqrt), reads=[rb], writes=[rb])
    S.op(S.dve, lambda e: e.reciprocal(out=rstd, in_=rstd), reads=[rb], writes=[rb])
    ydone = Buf()
    for (v, b, sem) in ystg:
        ydone.w = (sem, sem.count) if ydone.w is None else ydone.w
    ysrc = []
    for i in range(2):
        v, b = S.alloc(T, F32)
        ysrc.append((v, b, S.dsem()))
    xsrc = []
    for i in range(2):
        v, b = S.alloc(T, F32)
        xsrc.append((v, b, S.dsem()))
    xo = []
    for i in range(2):
        v, b = S.alloc(T, F32)
        xo.append((v, b, S.dsem()))
    ystore_bufs = [b for (_, b, _) in ystg]
    for c in range(KC):
        yv, yb, ysem = ysrc[c % 2]
        xv, xb, xsem = xsrc[c % 2]
        ov, ob, osem = xo[c % 2]
        S.dma(S.sp, yv, yT[c * 128:(c + 1) * 128, :], ysem, reads=[], writes=[yb] + (ystore_bufs if c == 0 else []))
        S.dma(S.sp, xv, x_src[c * 128:(c + 1) * 128, :], xsem, writes=[xb])
        S.op(S.dve, lambda e: e.scalar_tensor_tensor(out=yv, in0=yv, scalar=C.gains[:, gcol + c:gcol + c + 1], in1=rstd,
                                                     op0=ALU.mult, op1=ALU.mult), reads=[yb, rb], writes=[yb])
        S.op(S.pool, lambda e: e.tensor_tensor(out=ov, in0=yv, in1=xv, op=ALU.add), reads=[yb, xb], writes=[ob])
        S.dma(S.sp, x_dst[c * 128:(c + 1) * 128, :], ov, osem, reads=[ob])


def phase_merge(S, C, oT, gatesT, Wb, mT):
    o_sb, ob = S.alloc(24 * T)
    osem = S.dsem()
    oview = o_sb.rearrange("p (k t) -> p k t", k=24)
    for i in range(3):
        S.dma(S.sp, oview[:, i * 8:(i + 1) * 8, :], oT[i * 1024:(i + 1) * 1024, :].rearrange("(k p) t -> p k t", p=128),
              osem, writes=[ob])
    slabs = Slabs(S, 8, 512, nbuf=6)
    gt = []
    for i in range(2):
        v, b = S.alloc(3 * T)
        gt.append((v, b, S.dsem()))
    acc = [S.alloc(512, F32) for _ in range(2)]
    tmp = [S.alloc(512, F32) for _ in range(4)]
    mst = []
    for i in range(2):
        v, b = S.alloc(T)
        mst.append((v, b, S.dsem()))
    pi = 0
    ci = 0
    for c0 in range(0, D, 512):
        wv = [slabs.load(Wb[i], 0, c0, 512) for i in range(3)]
        for n in range(4):
            cc = c0 // 128 + n
            gv, gb, gsem = gt[cc % 2]
            gview = gv.rearrange("p (i t) -> p i t", i=3)
            src = gatesT.rearrange("(i c) t -> c i t", i=3)[cc * 128:(cc + 1) * 128, :, :]
            S.dma(S.sp, gview, src, gsem, writes=[gb])
            mv, mb, msem = mst[cc % 2]
            for t in range(2):
                banks = [(pi * 3 + i) % 6 for i in range(3)]
                pi += 1
                for i in range(3):
                    for k in range(8):
                        mm(S, S.ps[banks[i]][:, :], wv[i][0][:, k, n * 128:(n + 1) * 128],
                           oview[:, i * 8 + k, t * 512:(t + 1) * 512], k == 0, k == 7, [wv[i][1], ob], [S.psb[banks[i]]],
                           sig=False)
                a, ab = acc[ci % 2]
                t1, t1b = tmp[(2 * ci) % 4]
                t2, t2b = tmp[(2 * ci + 1) % 4]
                ci += 1
                sl = slice(t * 512, (t + 1) * 512)
                S.op(S.dve, lambda e: e.tensor_tensor(out=a, in0=S.ps[banks[0]][:, :], in1=gview[:, 0, sl], op=ALU.mult),
                     reads=[S.psb[banks[0]], gb], writes=[ab])
                S.op(S.dve, lambda e: e.tensor_tensor(out=t1, in0=S.ps[banks[1]][:, :], in1=gview[:, 1, sl], op=ALU.mult),
                     reads=[S.psb[banks[1]], gb], writes=[t1b])
                S.op(S.dve, lambda e: e.tensor_tensor(out=t2, in0=S.ps[banks[2]][:, :], in1=gview[:, 2, sl], op=ALU.mult),
                     reads=[S.psb[banks[2]], gb], writes=[t2b])
                S.op(S.pool, lambda e: e.tensor_tensor(out=a, in0=a, in1=t1, op=ALU.add), reads=[ab, t1b], writes=[ab])
                S.op(S.pool, lambda e: e.tensor_tensor(out=mv[:, sl], in0=a, in1=t2, op=ALU.add), reads=[ab, t2b],
                     writes=[mb])
            S.dma(S.sp, mT[cc * 128:(cc + 1) * 128, :], mv, msem, reads=[mb])


GC_PER_LAYER = 134


def gcols(l):
    b = l * GC_PER_LAYER
    return dict(mix_pre=b, mix_post=b + 32, mlp_pre=b + 64, mlp_post=b + 96, gla_gain=b + 128, gla_bias=b + 130)


def phase_gla_local(S, C, io, qkT, gaT, gvTM, gate_up, gc, qgT, oaloc, sfin):
    c_dk = 128 ** -0.5
    rmask, rmb = S.alloc(T, F32)
    onesr, orb = S.alloc(T, F32)
    cz, czb = S.alloc(64, F32)
    gu, gub = S.alloc(512, F32)
    ga, gab = S.alloc(T, F32)
    nb, nbb = S.alloc(4, F32)
    sems = [S.dsem() for _ in range(5)]
    S.dma(S.sp, rmask, io["c_resetmask"], sems[0], writes=[rmb])
    S.dma(S.sp, onesr, io["c_onesrow"], sems[1], writes=[orb])
    S.dma(S.sp, cz[0:64, :], io["c_causal64"], sems[2], writes=[czb])
    S.dma(S.sp, gu[0:16, :], gate_up, sems[3], writes=[gub])
    S.dma(S.sp, ga[0:16, :], gaT, sems[4], writes=[gab])
    S.op(S.dve, lambda e: e.tensor_scalar(out=nb, in0=C.gains[:, gc["gla_bias"]:gc["gla_bias"] + 4], scalar1=-1.0,
                                          scalar2=None, op0=ALU.mult), writes=[nbb])
    ld = []
    for i in range(2):
        q, qb = S.alloc(T)
        k, kb = S.alloc(T)
        v, vb = S.alloc(16 * 256)
        ld.append((q, qb, k, kb, v, vb, S.dsem(), S.dsem(), S.dsem()))
    e1, e1b = S.alloc(T, F32)
    sp, spb = S.alloc(T, F32)
    bs, bsb = S.alloc(T, F32)
    bg, bgb = S.alloc(T, F32)
    ex, exb = S.alloc(T, F32)
    dl, dlb = S.alloc(T, F32)
    edec, edb = S.alloc(16, F32)
    qe, qeb = S.alloc(T)
    ke, keb = S.alloc(T)
    kd, kdb = S.alloc(T)
    qg, qgb = S.alloc(T)
    qgsem = S.dsem()
    atm = [S.alloc(64) for _ in range(2)]
    kdt = [S.alloc(128) for _ in range(2)]
    Sst, Sb = S.alloc(256, F32)
    Sbf, Sbfb = S.alloc(256)
    ost = []
    for i in range(2):
        v, b = S.alloc(512, F32)
        ost.append((v, b, S.dsem()))
    sfsem = S.dsem()
    oi = 0
    for h in range(4):
        q, qb, k, kb, v, vb, s0, s1, s2 = ld[h % 2]
        S.dma(S.sp, q, qkT[h * 128:(h + 1) * 128, :], s0, writes=[qb])
        S.dma(S.sp, k, qkT[512 + h * 128:512 + (h + 1) * 128, :], s1, writes=[kb])
        vview = v[0:64, :].rearrange("p (n e) -> p n e", n=16)
        S.dma(S.sp, vview, gvTM[:, h * 256:(h + 1) * 256].rearrange("(n j) e -> j n e", j=64), s2, writes=[vb])
        for t in range(2):
            mm(S, S.ps[t][:, :], gu[0:16, h * 128:(h + 1) * 128], ga[0:16, t * 512:(t + 1) * 512], True, True,
               [gub, gab], [S.psb[t]])
            S.op(S.act, lambda e: e.activation(out=e1[:, t * 512:(t + 1) * 512], in_=S.ps[t][:, :], func=AF.Exp,
                                               scale=-1.0, bias=nb[:, h:h + 1]), reads=[S.psb[t], nbb], writes=[e1b])
        S.op(S.act, lambda e: e.activation(out=sp, in_=e1, func=AF.Ln, bias=1.0), reads=[e1b], writes=[spb])
        S.op(S.dve, lambda e: e.tensor_tensor_scan(out=bs, data0=rmask, data1=sp, initial=0.0, op0=ALU.mult,
                                                   op1=ALU.add), reads=[rmb, spb], writes=[bsb])
        S.op(S.dve, lambda e: e.tensor_tensor_scan(out=bg, data0=onesr, data1=sp, initial=0.0, op0=ALU.mult,
                                                   op1=ALU.add), reads=[orb, spb], writes=[bgb])
        bs3 = bs.rearrange("p (n j) -> p n j", j=64)
        S.op(S.act, lambda e: e.activation(out=ex, in_=bs, func=AF.Exp, scale=-1.0 / 16), reads=[bsb], writes=[exb])
        S.op(S.dve, lambda e: e.scalar_tensor_tensor(out=qe, in0=q, scalar=c_dk, in1=ex, op0=ALU.mult, op1=ALU.mult),
             reads=[qb, exb], writes=[qeb])
        S.op(S.act, lambda e: e.activation(out=ex, in_=bs, func=AF.Exp, scale=1.0 / 16), reads=[bsb], writes=[exb])
        S.op(S.dve, lambda e: e.tensor_tensor(out=ke, in0=k, in1=ex, op=ALU.mult), reads=[kb, exb], writes=[keb])
        S.op(S.dve, lambda e: e.tensor_tensor(out=dl.rearrange("p (n j) -> p n j", j=64), in0=bs3,
                                              in1=bs3[:, :, 63:64].to_broadcast([128, 16, 64]), op=ALU.subtract),
             reads=[bsb], writes=[dlb])
        S.op(S.act, lambda e: e.activation(out=ex, in_=dl, func=AF.Exp, scale=1.0 / 16), reads=[dlb], writes=[exb])
        S.op(S.dve, lambda e: e.tensor_tensor(out=kd, in0=k, in1=ex, op=ALU.mult), reads=[kb, exb], writes=[kdb])
        S.op(S.act, lambda e: e.activation(out=edec, in_=bs3[:, :, 63], func=AF.Exp, scale=-1.0 / 16), reads=[bsb],
             writes=[edb])
        S.op(S.act, lambda e: e.activation(out=ex, in_=bg, func=AF.Exp, scale=-1.0 / 16), reads=[bgb], writes=[exb])
        S.op(S.dve, lambda e: e.scalar_tensor_tensor(out=qg, in0=q, scalar=c_dk, in1=ex, op0=ALU.mult, op1=ALU.mult),
             reads=[qb, exb], writes=[qgb])
        S.dma(S.sp, qgT[h * 128:(h + 1) * 128, :], qg, qgsem, reads=[qgb])
        for n in range(16):
            cs = slice(n * 64, (n + 1) * 64)
            mm(S, S.ps[2][0:64, 0:64], ke[:, cs], qe[:, cs], True, True, [keb, qeb], [S.psb[2]])
            am, amb = atm[n % 2]
            S.op(S.dve, lambda e: e.tensor_tensor(out=am[0:64, :], in0=S.ps[2][0:64, 0:64], in1=cz[0:64, :], op=ALU.mult),
                 reads=[S.psb[2], czb], writes=[amb])
            pt = S.ps[3][:, :].bitcast(BF16)
            S.op(S.pe, lambda e: e.transpose(out=pt[0:64, 0:128], in_=kd[:, cs], identity=C.ident), reads=[kdb],
                 writes=[S.psb[3]])
            kt, ktb = kdt[n % 2]
            S.op(S.act, lambda e: e.copy(out=kt[0:64, :], in_=pt[0:64, 0:128]), reads=[S.psb[3]], writes=[ktb])
            for eh in range(2):
                ob_ = S.psb[4 + eh]
                oc = S.ps[4 + eh][:, (n % 8) * 64:(n % 8 + 1) * 64]
                mm(S, oc, vview[:, n, eh * 128:(eh + 1) * 128], am[0:64, :], True, n == 0, [vb, amb], [ob_])
                if n > 0:
                    mm(S, oc, Sbf[:, eh * 128:(eh + 1) * 128], qe[:, cs], False, True, [Sbfb, qeb], [ob_])
            mm(S, S.ps[6][:, 0:256], kt[0:64, :], vview[:, n, :], True, True, [ktb, vb], [S.psb[6]])
            if n == 0:
                S.op(S.dve, lambda e: e.tensor_copy(out=Sst, in_=S.ps[6][:, 0:256]), reads=[S.psb[6]], writes=[Sb])
            else:
                S.op(S.dve, lambda e: e.scalar_tensor_tensor(out=Sst, in0=Sst, scalar=edec[:, n:n + 1],
                                                             in1=S.ps[6][:, 0:256], op0=ALU.mult, op1=ALU.add),
                     reads=[Sb, edb, S.psb[6]], writes=[Sb])
            if n < 15:
                S.op(S.act, lambda e: e.copy(out=Sbf, in_=Sst), reads=[Sb], writes=[Sbfb])
            if n % 8 == 7:
                tt = n // 8
                for eh in range(2):
                    ov, ob2, osem = ost[oi % 2]
                    oi += 1
                    S.op(S.act, lambda e: e.copy(out=ov, in_=S.ps[4 + eh][:, :]), reads=[S.psb[4 + eh]], writes=[ob2])
                    S.dma(S.sp, oaloc[h * 256 + eh * 128:h * 256 + (eh + 1) * 128, tt * 512:(tt + 1) * 512], ov, osem,
                          reads=[ob2])
        S.dma(S.sp, sfin[h * 128:(h + 1) * 128, :], Sst, sfsem, reads=[Sb])


def phase_gla_fin(S, C, sfin_o, qgT, oaloc, ggT, gc, oT):
    c256 = 1.0 / 256
    ld = []
    for i in range(2):
        si, sib = S.alloc(256, F32)
        qg, qgb = S.alloc(T)
        ol, olb = S.alloc(2 * T, F32)
        gg, ggb = S.alloc(2 * T)
        ld.append((si, sib, qg, qgb, ol, olb, gg, ggb, S.dsem(), S.dsem(), S.dsem(), S.dsem()))
    sbf, sbfb = S.alloc(256)
    sq, sqb = S.alloc(2 * T)
    rstd, rb = S.alloc(T, F32)
    sg, sgb = S.alloc(2 * T, F32)
    outs = []
    for i in range(2):
        v, b = S.alloc(2 * T)
        outs.append((v, b, S.dsem()))
    for h in range(4):
        si, sib, qg, qgb, ol, olb, gg, ggb, s0, s1, s2, s3 = ld[h % 2]
        S.dma(S.sp, si, sfin_o[h * 128:(h + 1) * 128, :], s0, writes=[sib])
        S.dma(S.sp, qg, qgT[h * 128:(h + 1) * 128, :], s1, writes=[qgb])
        olv = ol.rearrange("p (e t) -> p e t", e=2)
        S.dma(S.sp, olv, oaloc[h * 256:(h + 1) * 256, :].rearrange("(e p) t -> p e t", p=128), s2, writes=[olb])
        ggv = gg.rearrange("p (e t) -> p e t", e=2)
        S.dma(S.sp, ggv, ggT[h * 256:(h + 1) * 256, :].rearrange("(e p) t -> p e t", p=128), s3, writes=[ggb])
        S.op(S.dve, lambda e: e.tensor_scalar(out=sbf, in0=si, scalar1=C.flag[:, 0:1], scalar2=None, op0=ALU.mult),
             reads=[sib], writes=[sbfb])
        for eh in range(2):
            for t in range(2):
                bank = eh * 2 + t
                mm(S, S.ps[bank][:, :], sbf[:, eh * 128:(eh + 1) * 128], qg[:, t * 512:(t + 1) * 512], True, True,
                   [sbfb, qgb], [S.psb[bank]])
                sl = slice(t * 512, (t + 1) * 512)
                S.op(S.dve, lambda e: e.tensor_tensor(out=olv[:, eh, sl], in0=S.ps[bank][:, :], in1=olv[:, eh, sl],
                                                      op=ALU.add), reads=[S.psb[bank], olb], writes=[olb])
        S.op(S.act, lambda e: e.activation(out=sq, in_=ol, func=AF.Square), reads=[olb], writes=[sqb])
        sqv = sq.rearrange("p (e t) -> p e t", e=2)
        for t in range(2):
            for eh in range(2):
                mm(S, S.ps[4 + t][:, :], C.ones, sqv[:, eh, t * 512:(t + 1) * 512], eh == 0, eh == 1, [sqb], [S.psb[4 + t]])
            S.op(S.dve, lambda e: e.tensor_scalar(out=rstd[:, t * 512:(t + 1) * 512], in0=S.ps[4 + t][:, :], scalar1=c256,
                                                  scalar2=EPS, op0=ALU.mult, op1=ALU.add), reads=[S.psb[4 + t]],
                 writes=[rb])
        S.op(S.act, lambda e: e.activation(out=rstd, in_=rstd, func=AF.Sqrt), reads=[rb], writes=[rb])
        S.op(S.dve, lambda e: e.reciprocal(out=rstd, in_=rstd), reads=[rb], writes=[rb])
        S.op(S.act, lambda e: e.activation(out=sg, in_=gg, func=AF.Silu), reads=[ggb], writes=[sgb])
        sgv = sg.rearrange("p (e t) -> p e t", e=2)
        ov, ob2, osem = outs[h % 2]
        ovv = ov.rearrange("p (e t) -> p e t", e=2)
        for eh in range(2):
            S.op(S.dve, lambda e: e.scalar_tensor_tensor(out=olv[:, eh, :], in0=olv[:, eh, :],
                                                         scalar=C.gains[:, gc["gla_gain"] + eh:gc["gla_gain"] + eh + 1],
                                                         in1=rstd, op0=ALU.mult, op1=ALU.mult), reads=[olb, rb],
                 writes=[olb])
            S.op(S.pool, lambda e: e.tensor_tensor(out=ovv[:, eh, :], in0=olv[:, eh, :], in1=sgv[:, eh, :], op=ALU.mult),
                 reads=[olb, sgb], writes=[ob2])
        S.dma(S.sp, oT[h * 256:(h + 1) * 256, :].rearrange("(e p) t -> p e t", p=128), ovv, osem, reads=[ob2])


def phase_sb(S, C, io, sqT, skT, svTM, skT_o, svTM_o, oT):
    c = 128 ** -0.5
    msk, mskb = S.alloc(4 * 512)
    msem = S.dsem()
    S.dma(S.sp, msk.rearrange("p (r t) -> p r t", r=4), io["c_sbmask"], msem, writes=[mskb])
    mview = msk.rearrange("p (r t) -> p r t", r=4)
    ld = []
    for i in range(2):
        q, qb = S.alloc(T)
        k, kb = S.alloc(T)
        ko, kob = S.alloc(T)
        v, vb = S.alloc(T)
        vo, vob = S.alloc(T)
        ld.append((q, qb, k, kb, ko, kob, v, vb, vo, vob, [S.dsem() for _ in range(5)]))
    NB = 10
    ZB = [0, 1, 4]
    AB = [2, 3, 5]
    e1 = [S.alloc(512, F32) for _ in range(NB)]
    sp = [S.alloc(512, F32) for _ in range(NB)]
    mt = [S.alloc(512) for _ in range(NB)]
    ms = [S.alloc(512) for _ in range(5)]
    tt_ = [S.alloc(512, F32) for _ in range(NB)]
    wt = [S.alloc(512) for _ in range(NB)]
    ost = []
    for i in range(2):
        v_, b_ = S.alloc(512)
        ost.append((v_, b_, S.dsem()))
    descs = []
    units = []
    for h in range(8):
        for tt in range(2):
            blocks = [("own", g) for g in range(4 * tt + 3, -1, -1)] + [("oth", g) for g in range(7, -1, -1)]
            u = len(units)
            units.append((h, tt, len(blocks)))
            for bidx, (kind, g) in enumerate(blocks):
                descs.append((u, h, tt, bidx, len(blocks), kind, g))
    N = len(descs)

    def views(h):
        q, qb, k, kb, ko, kob, v, vb, vo, vob, sems = ld[h % 2]
        return (q, qb, k, kb, ko, kob, v.rearrange("p (b d) -> p b d", b=8), vb,
                vo.rearrange("p (b d) -> p b d", b=8), vob, vo, sems)

    def load_head(h):
        q, qb, k, kb, ko, kob, vv, vb, vov, vob, vo, sems = views(h)
        S.dma(S.sp, q, sqT[h * 128:(h + 1) * 128, :], sems[0], writes=[qb])
        S.dma(S.sp, k, skT[h * 128:(h + 1) * 128, :], sems[1], writes=[kb])
        S.dma(S.sp, ko, skT_o[h * 128:(h + 1) * 128, :], sems[2], writes=[kob])
        S.dma(S.sp, vv, svTM[:, h * 128:(h + 1) * 128].rearrange("(b p) d -> p b d", p=128), sems[3], writes=[vb])
        S.dma(S.sp, vov, svTM_o[:, h * 128:(h + 1) * 128].rearrange("(b p) d -> p b d", p=128), sems[4], writes=[vob])

    def info(gi):
        u, h, tt, bidx, nblk, kind, g = descs[gi]
        masked = kind == "own" and g >= 4 * tt
        return u, h, tt, bidx, nblk, kind, g, masked, g - 4 * tt, gi % NB, gi % 3

    def stage1(gi):
        u, h, tt, bidx, nblk, kind, g, masked, r, si, zi = info(gi)
        q, qb, k, kb, ko, kob, vv, vb, vov, vob, vo, sems = views(h)
        if tt == 0 and bidx == 0:
            S.op(S.act, lambda e: e.mul(out=vo, in_=vo, mul=C.flag[:, 0:1]), reads=[vob], writes=[vob])
            if h + 1 < 8:
                load_head(h + 1)
        tsl = slice(tt * 512, (tt + 1) * 512)
        kk, kkb = (k, kb) if kind == "own" else (ko, kob)
        e_, eb_ = e1[si]
        s_, sb_ = sp[si]
        m_, mb_ = mt[si]
        zb = ZB[zi]
        mm(S, S.ps[zb][:, :], kk[:, g * 128:(g + 1) * 128], q[:, tsl], True, True, [kkb, qb], [S.psb[zb]])
        S.op(S.act, lambda e: e.activation(out=e_, in_=S.ps[zb][:, :], func=AF.Exp, scale=-c), reads=[S.psb[zb]],
             writes=[eb_])
        S.op(S.act, lambda e: e.activation(out=s_, in_=e_, func=AF.Ln, bias=1.0), reads=[eb_], writes=[sb_])
        S.op(S.dve, lambda e: e.scalar_tensor_tensor(out=m_, in0=S.ps[zb][:, :], scalar=c, in1=s_, op0=ALU.mult,
                                                     op1=ALU.add), reads=[S.psb[zb], sb_], writes=[mb_])
        if masked:
            S.op(S.pool, lambda e: e.tensor_tensor(out=m_, in0=m_, in1=mview[:, r, :], op=ALU.mult),
                 reads=[mb_, mskb], writes=[mb_])
        if bidx < nblk - 1:
            nm = ms[gi % 5]
            if bidx == 0:
                S.op(S.pool, lambda e: e.tensor_copy(out=nm[0], in_=m_), reads=[mb_], writes=[nm[1]])
            else:
                pm = ms[(gi - 1) % 5]
                S.op(S.pool, lambda e: e.tensor_tensor(out=nm[0], in0=pm[0], in1=m_, op=ALU.add),
                     reads=[pm[1], mb_], writes=[nm[1]])

    def stage2a(gi):
        u, h, tt, bidx, nblk, kind, g, masked, r, si, zi = info(gi)
        ab = AB[zi]
        s_, sb_ = sp[si]
        m_, mb_ = mt[si]
        t_, tb_ = tt_[si]
        mm(S, S.ps[ab][:, :], C.ustrict, m_, True, bidx == 0, [mb_], [S.psb[ab]])
        if bidx > 0:
            pm = ms[(gi - 1) % 5]
            mm(S, S.ps[ab][:, :], C.ones, pm[0], False, True, [pm[1]], [S.psb[ab]])
        S.op(S.dve, lambda e: e.tensor_tensor(out=t_, in0=S.ps[ab][:, :], in1=s_, op=ALU.add),
             reads=[S.psb[ab], sb_], writes=[tb_])

    def stage2b(gi):
        u, h, tt, bidx, nblk, kind, g, masked, r, si, zi = info(gi)
        t_, tb_ = tt_[si]
        w_, wb_ = wt[si]
        S.op(S.act, lambda e: e.activation(out=w_, in_=t_, func=AF.Exp, scale=-1.0), reads=[tb_], writes=[wb_])
        if masked:
            S.op(S.pool, lambda e: e.tensor_tensor(out=w_, in0=w_, in1=mview[:, r, :], op=ALU.mult),
                 reads=[wb_, mskb], writes=[wb_])

    def stage3(gi):
        u, h, tt, bidx, nblk, kind, g, masked, r, si, zi = info(gi)
        q, qb, k, kb, ko, kob, vv, vb, vov, vob, vo, sems = views(h)
        vsrc, vsb = (vv, vb) if kind == "own" else (vov, vob)
        w_, wb_ = wt[si]
        obank = 6 + (u % 2)
        mm(S, S.ps[obank][:, :], vsrc[:, g, :], w_, bidx == 0, bidx == nblk - 1, [vsb, wb_], [S.psb[obank]])
        if bidx == nblk - 1:
            ov, ob2, osem = ost[u % 2]
            S.op(S.act, lambda e: e.copy(out=ov, in_=S.ps[obank][:, :]), reads=[S.psb[obank]], writes=[ob2])
            S.dma(S.sp, oT[2048 + h * 128:2048 + (h + 1) * 128, tt * 512:(tt + 1) * 512], ov, osem, reads=[ob2])

    load_head(0)
    for i in range(N + 7):
        if i < N:
            stage1(i)
        if 0 <= i - 1 < N:
            stage2a(i - 1)
        if 0 <= i - 5 < N:
            stage2b(i - 5)
        if 0 <= i - 7 < N:
            stage3(i - 7)


BIG = 1.0e30
OHW = 1152
NBIS = 26
TOPK_MODE = "bisect"


def phase_dsa(S, C, io, dqT, dkT, dvTM, iqT, ikT, iwTM, dkT_o, dvTM_o, ikT_o, rel_bias, gvec, oT, nc, co=None,
              low_mark=None):
    c = 128 ** -0.5
    wconst = (64 ** -0.5) * (32 ** -0.5)

    def tick(n):
        if co is not None:
            for _ in range(n):
                next(co, None)

    dsa_lo = S.aoff
    rb31, rb31b = S.alloc(8, F32)
    rbsem = S.dsem()
    S.dma(S.sp, rb31, rel_bias[31:32, :].partition_broadcast(128), rbsem, writes=[rb31b])
    iq, iqb = S.alloc(16 * T)
    iqv = iq.rearrange("p (a t) -> p a t", a=16)
    ik, ikb = S.alloc(2 * T)
    ik2, ik2b = S.alloc(2 * T)
    iw, iwb = S.alloc(8 * 32)
    wsc, wscb = S.alloc(8 * 32, F32)
    wscv = wsc.rearrange("p (b h) -> p b h", b=8)
    nbig, nbigb = S.alloc(1, F32)
    selT, selTb = S.alloc(16 * T)
    selTv = selT.rearrange("p (b t) -> p b t", b=16)
    s2 = [S.dsem() for _ in range(3)]
    S.op(S.dve, lambda e: e.memset(ik, 0.0), writes=[ikb])
    S.op(S.dve, lambda e: e.memset(ik2, 0.0), writes=[ik2b])
    for half in range(2):
        S.dma(S.sp, iqv[half * 64:(half + 1) * 64, :, :],
              iqT[half * 1024:(half + 1) * 1024, :].rearrange("(a d) t -> d a t", d=64), s2[0], writes=[iqb])
    S.dma(S.sp, ik[0:64, 0:T], ikT_o, s2[1], writes=[ikb])
    S.dma(S.sp, ik[0:64, T:2 * T], ikT, s2[1], writes=[ikb])
    ik2sem = S.dsem()
    S.dma(S.sp, ik2[64:128, 0:T], ikT_o, ik2sem, writes=[ik2b])
    S.dma(S.sp, ik2[64:128, T:2 * T], ikT, ik2sem, writes=[ik2b])
    S.dma(S.sp, iw.rearrange("p (b h) -> p b h", b=8), iwTM.rearrange("(b p) h -> p b h", p=128), s2[2], writes=[iwb])
    S.op(S.dve, lambda e: e.tensor_scalar(out=wsc, in0=iw, scalar1=wconst, scalar2=None, op0=ALU.mult), reads=[iwb],
         writes=[wscb])
    S.op(S.dve, lambda e: e.tensor_scalar(out=nbig, in0=C.flag, scalar1=-1.0, scalar2=BIG, op0=ALU.add, op1=ALU.mult),
         writes=[nbigb])
    S.op(S.pool, lambda e: e.memset(selT, 0.0), writes=[selTb])
    Is = [S.alloc(2 * T, F32) for _ in range(1)]
    dgm, dgmb = S.alloc(128, F32)
    dgn, dgnb = S.alloc(128, F32)
    dgsem = S.dsem()
    S.dma(S.sp, dgm, io["c_diagm"], dgsem, writes=[dgmb])
    S.dma(S.sp, dgn, io["c_diagn"], dgsem, writes=[dgnb])
    I2, I2b = S.alloc(2 * T, F32)
    Dt, Dtb = S.alloc(NBIS, F32)
    p2, p2b = S.alloc(NBIS, F32)
    p2sem = S.dsem()
    S.dma(S.sp, p2, io["c_pow2"], p2sem, writes=[p2b])
    rts = [S.alloc(512, F32) for _ in range(2)]
    m8, m8b = S.alloc(8, F32)
    thr, thrb = S.alloc(1, F32)
    sel, selb = S.alloc(2 * T)
    pi = 0
    ri_ = 0
    for j in range(8):
        L = T + (j + 1) * 128
        I, Ib = Is[0]
        S.op(S.dve, lambda e: e.memset(I[:, 0:L], 0.0), writes=[Ib])
        S.op(S.dve, lambda e: e.tensor_scalar(out=I[:, 0:T], in0=I[:, 0:T], scalar1=nbig[:, 0:1], scalar2=None,
                                              op0=ALU.add), reads=[nbigb, Ib], writes=[Ib])
        for hh in range(32):
            half, a = hh // 16, hh % 16
            chunks = []
            for c0 in range(0, L, 512):
                w = min(512, L - c0)
                bank = pi % 4
                pi += 1
                kt_, ktb_ = (ik, ikb) if half == 0 else (ik2, ik2b)
                mm(S, S.ps[bank][:, 0:w], iqv[:, a, j * 128:(j + 1) * 128], kt_[:, c0:c0 + w], True, True,
                   [iqb, ktb_], [S.psb[bank]])
                chunks.append((c0, w, bank))
            tick(len(chunks))
            for (c0, w, bank) in chunks:
                rt, rtb = rts[ri_ % 2]
                ri_ += 1
                S.op(S.act, lambda e: e.activation(out=rt[:, 0:w], in_=S.ps[bank][:, 0:w], func=AF.Relu),
                     reads=[S.psb[bank]], writes=[rtb])
                S.op(S.dve, lambda e: e.scalar_tensor_tensor(out=I[:, c0:c0 + w], in0=rt[:, 0:w],
                                                             scalar=wscv[:, j, hh:hh + 1], in1=I[:, c0:c0 + w],
                                                             op0=ALU.mult, op1=ALU.add), reads=[rtb, wscb, Ib],
                     writes=[Ib])
            tick(len(chunks))
        dg = I[:, T + j * 128:T + (j + 1) * 128]
        S.op(S.dve, lambda e: e.tensor_tensor(out=dg, in0=dg, in1=dgm, op=ALU.mult), reads=[Ib, dgmb], writes=[Ib])
        S.op(S.dve, lambda e: e.tensor_tensor(out=dg, in0=dg, in1=dgn, op=ALU.add), reads=[Ib, dgnb], writes=[Ib])
        if True:
            W1 = I2[:, 0:L]
            S.op(S.dve, lambda e: e.scalar_tensor_tensor(out=W1, in0=I[:, 0:L], scalar=-BIG / 2, in1=I[:, 0:L],
                                                         op0=ALU.is_ge, op1=ALU.mult), reads=[Ib], writes=[I2b])
            S.op(S.dve, lambda e: e.tensor_reduce(out=thr, in_=W1, axis=AX.X, op=ALU.min), reads=[I2b], writes=[thrb])
            S.op(S.dve, lambda e: e.reduce_max(out=m8[:, 1:2], in_=I[:, 0:L], axis=AX.X), reads=[Ib], writes=[m8b])
            S.op(S.dve, lambda e: e.tensor_tensor(out=m8[:, 2:3], in0=m8[:, 1:2], in1=thr, op=ALU.subtract),
                 reads=[m8b, thrb], writes=[m8b])
            S.op(S.dve, lambda e: e.tensor_scalar(out=Dt, in0=p2, scalar1=m8[:, 2:3], scalar2=None, op0=ALU.mult),
                 reads=[m8b, p2b], writes=[Dtb])
            for it in range(NBIS):
                S.op(S.dve, lambda e: e.tensor_tensor(out=m8[:, 3:4], in0=Dt[:, it:it + 1], in1=thr, op=ALU.add),
                     reads=[Dtb, thrb], writes=[m8b])
                S.op(S.dve, lambda e: e.tensor_scalar(out=sel[:, 0:L], in0=I[:, 0:L], scalar1=m8[:, 3:4], scalar2=None,
                                                      op0=ALU.is_ge, op1=ALU.add, accum_out=m8[:, 4:5]),
                     reads=[Ib, m8b], writes=[selb, m8b])
                S.op(S.dve, lambda e: e.tensor_scalar(out=m8[:, 5:6], in0=m8[:, 4:5], scalar1=255.5,
                                                      scalar2=Dt[:, it:it + 1], op0=ALU.is_ge, op1=ALU.mult),
                     reads=[m8b, Dtb], writes=[m8b])
                S.op(S.dve, lambda e: e.tensor_tensor(out=thr, in0=thr, in1=m8[:, 5:6], op=ALU.add),
                     reads=[thrb, m8b], writes=[thrb])
                tick(6)
        S.op(S.dve, lambda e: e.tensor_scalar(out=sel[:, 0:L], in0=I[:, 0:L], scalar1=thr[:, 0:1], scalar2=None,
                                              op0=ALU.is_ge), reads=[Ib, thrb], writes=[selb])
        nblk = 8 + j + 1
        for b0 in range(0, nblk, 4):
            nb_ = min(4, nblk - b0)
            bank = 4 + (b0 // 4) % 2
            pt = S.ps[bank][:, :].bitcast(BF16)
            for bb in range(nb_):
                S.op(S.pe, lambda e: e.transpose(out=pt[:, bb * 128:(bb + 1) * 128],
                                                 in_=sel[:, (b0 + bb) * 128:(b0 + bb + 1) * 128], identity=C.ident),
                     reads=[selb], writes=[S.psb[bank]])
            S.op(S.act, lambda e: e.copy(out=selTv[:, b0:b0 + nb_, j * 128:(j + 1) * 128],
                                         in_=pt[:, 0:nb_ * 128].rearrange("p (b t) -> p b t", b=nb_)),
                 reads=[S.psb[bank]], writes=[selTb])
    if co is not None:
        for _ in co:
            pass
    hi_mark = S.aoff
    if low_mark is not None:
        S.barrier()
        S.aoff = low_mark
    rbs, rbsb = S.alloc(8, F32)
    oh, ohb = S.alloc(OHW, F32)
    gvs, gvsb = S.alloc(OHW)
    eb, ebb = S.alloc(8 * 5 * 512)
    ebv = eb.rearrange("p (h r t) -> p h r t", h=8, r=5)
    sems = [S.dsem() for _ in range(5)]
    S.dma(S.sp, rbs[0:32, :], rel_bias, sems[0], writes=[rbsb])
    S.dma(S.sp, oh[0:32, :], io["c_oh"], sems[1], writes=[ohb])
    S.op(S.act, lambda e: e.activation(out=rbs[0:32, :], in_=rbs[0:32, :], func=AF.Exp), reads=[rbsb], writes=[rbsb])
    for i in range(3):
        mm(S, S.ps[i][0:8, 0:384], rbs[0:32, 0:8], oh[0:32, i * 384:(i + 1) * 384], True, True, [rbsb, ohb], [S.psb[i]])
        S.op(S.act, lambda e: e.copy(out=gvs[0:8, i * 384:(i + 1) * 384], in_=S.ps[i][0:8, 0:384]), reads=[S.psb[i]],
             writes=[gvsb])
    gvb = Buf()
    S.dma(S.sp, gvec, gvs[0:8, :], sems[3], reads=[gvsb], writes=[gvb])
    aid, aidb = S.alloc(128)
    S.dma(S.sp, aid, io["c_antiident"], sems[4], writes=[aidb])
    hk = []
    for i in range(3):
        v_, b_ = S.alloc(512)
        hk.append((v_, b_, S.dsem()))
    ti = 0
    for h in range(8):
        for ri in range(5):
            r = ri - 1
            src = bass.AP(gvec.tensor, h * OHW + 385 - 128 * r, [[1, 128], [1, 512]])
            hv, hb, hsem = hk[ti % 3]
            bank = 4 + (ti % 4)
            ti += 1
            S.dma(S.sp, hv, src, hsem, reads=[gvb], writes=[hb])
            mm(S, S.ps[bank][:, :], aid, hv, True, True, [aidb, hb], [S.psb[bank]])
            S.op(S.act, lambda e: e.copy(out=ebv[:, h, ri, :], in_=S.ps[bank][:, :]), reads=[S.psb[bank]], writes=[ebb])
    dk, dkb = S.alloc(2 * T)
    dv, dvb = S.alloc(2 * T)
    dvv = dv.rearrange("p (b d) -> p b d", b=16)
    s3 = [S.dsem() for _ in range(2)]
    S.dma(S.sp, dk[:, 0:T], dkT_o, s3[0], writes=[dkb])
    S.dma(S.sp, dk[:, T:2 * T], dkT, s3[0], writes=[dkb])
    S.dma(S.sp, dvv[:, 0:8, :], dvTM_o.rearrange("(b p) d -> p b d", p=128), s3[1], writes=[dvb])
    S.dma(S.sp, dvv[:, 8:16, :], dvTM.rearrange("(b p) d -> p b d", p=128), s3[1], writes=[dvb])
    S.op(S.act, lambda e: e.mul(out=dv[:, 0:T], in_=dv[:, 0:T], mul=C.flag[:, 0:1]), reads=[dvb], writes=[dvb])
    qs = []
    for i in range(2):
        v, b = S.alloc(T)
        qs.append((v, b, S.dsem()))
    pts = [S.alloc(512) for _ in range(8)]
    rec, recb = S.alloc(512, F32)
    ost = []
    for i in range(2):
        v_, b_ = S.alloc(512)
        ost.append((v_, b_, S.dsem()))
    if low_mark is not None:
        assert S.aoff <= dsa_lo, (S.aoff, dsa_lo)
        S.aoff = hi_mark
    bi = 0
    oi = 0
    for h in range(8):
        q, qb, qsem = qs[h % 2]
        S.dma(S.sp, q, dqT[h * 128:(h + 1) * 128, :], qsem, writes=[qb])
        for tt in range(2):
            tsl = slice(tt * 512, (tt + 1) * 512)
            blocks = [("oth", g) for g in range(8)] + [("own", g) for g in range(4 * tt + 4)]
            nblk = len(blocks)
            ob_ = 2 + (oi % 2)
            db_ = 4 + (oi % 2)
            base = bi
            bi += nblk

            def stage1(bidx):
                kind, g = blocks[bidx]
                bix = g if kind == "oth" else 8 + g
                r = g - 4 * tt if kind == "own" else g - 8 - 4 * tt
                near = r >= -1
                lb = (base + bidx) % 2
                p_, pb_ = pts[(base + bidx) % 8]
                mm(S, S.ps[lb][:, :], dk[:, bix * 128:(bix + 1) * 128], q[:, tsl], True, True, [dkb, qb], [S.psb[lb]])
                if near:
                    S.op(S.act, lambda e: e.activation(out=p_, in_=S.ps[lb][:, :], func=AF.Exp, scale=c),
                         reads=[S.psb[lb]], writes=[pb_])
                    S.op(S.pool, lambda e: e.tensor_tensor(out=p_, in0=p_, in1=ebv[:, h, r + 1, :], op=ALU.mult),
                         reads=[pb_, ebb], writes=[pb_])
                else:
                    S.op(S.act, lambda e: e.activation(out=p_, in_=S.ps[lb][:, :], func=AF.Exp, scale=c,
                                                       bias=rb31[:, h:h + 1]), reads=[S.psb[lb], rb31b], writes=[pb_])
                S.op(S.dve, lambda e: e.tensor_tensor(out=p_, in0=p_, in1=selTv[:, bix, tsl], op=ALU.mult),
                     reads=[pb_, selTb], writes=[pb_])

            def stage2(bidx):
                kind, g = blocks[bidx]
                bix = g if kind == "oth" else 8 + g
                p_, pb_ = pts[(base + bidx) % 8]
                mm(S, S.ps[ob_][:, :], dvv[:, bix, :], p_, bidx == 0, bidx == nblk - 1, [dvb, pb_], [S.psb[ob_]])
                mm(S, S.ps[db_][:, :], C.onesf if kind == "oth" else C.ones, p_, bidx == 0, bidx == nblk - 1, [pb_],
                   [S.psb[db_]])

            for i in range(nblk + 4):
                if i < nblk:
                    stage1(i)
                if 0 <= i - 4 < nblk:
                    stage2(i - 4)
            ov, ob2, osem = ost[oi % 2]
            oi += 1
            S.op(S.dve, lambda e: e.reciprocal(out=rec, in_=S.ps[db_][:, :]), reads=[S.psb[db_]], writes=[recb])
            S.op(S.dve, lambda e: e.tensor_tensor(out=ov, in0=S.ps[ob_][:, :], in1=rec, op=ALU.mult),
                 reads=[S.psb[ob_], recb], writes=[ob2])
            S.dma(S.sp, oT[1024 + h * 128:1024 + (h + 1) * 128, tsl], ov, osem, reads=[ob2])


R_FM = 6144
FM_ROWS = dict(gq=0, gk=512, gg=1024, dq=2048, iq=3072, sq=5120)
TM_COLS = dict(gv=0, iw=1024)
TM_W = 1056
PAIRS = [[0, 1], [2, 3], [4, 5], [6, 7]]

CONST_SPECS = dict(
    c_ones=([128, 128], BF16), c_ident=([128, 128], BF16), c_ustrict=([128, 128], BF16), flag=([128, 1], F32),
    gains=([128, 2 * GC_PER_LAYER], F32), c_resetmask=([128, T], F32), c_onesrow=([128, T], F32),
    c_causal64=([64, 64], F32), c_sbmask=([128, 4, 512], BF16), c_oh=([32, OHW], F32),
    c_antiident=([128, 128], BF16), c_pow2=([128, NBIS], F32),
    c_diagm=([128, 128], F32), c_diagn=([128, 128], F32),
)


def host_consts():
    bf = ml_dtypes.bfloat16
    c = {}
    c["c_ones"] = np.ones((128, 128), bf)
    c["c_ident"] = np.eye(128, dtype=np.float32).astype(bf)
    j = np.arange(128)[:, None]
    s = np.arange(128)[None, :]
    c["c_ustrict"] = (j > s).astype(np.float32).astype(bf)
    t = np.arange(T)
    c["c_resetmask"] = np.broadcast_to((t % 64 != 0).astype(np.float32)[None, :], (128, T)).copy()
    c["c_onesrow"] = np.ones((128, T), np.float32)
    jj = np.arange(64)[:, None]
    ii = np.arange(64)[None, :]
    c["c_causal64"] = (jj <= ii).astype(np.float32)
    r = np.arange(4)[None, :, None]
    sp = np.arange(128)[:, None, None]
    tp = np.arange(512)[None, None, :]
    c["c_sbmask"] = ((r * 128 + sp) < tp).astype(np.float32).astype(bf)
    dist = np.arange(OHW) - 512
    d = np.maximum(dist, 1).astype(np.float32)
    large = 16 + (np.log(d / 16) / np.log(128 / 16) * 16).astype(np.int32)
    large = np.minimum(large, 31)
    bucket = np.where(dist < 16, np.maximum(dist, 0), large)
    oh = np.zeros((32, OHW), np.float32)
    for dd in range(OHW):
        if dist[dd] >= 0:
            oh[bucket[dd], dd] = 1.0
    c["c_oh"] = oh
    c["c_antiident"] = np.eye(128, dtype=np.float32)[::-1].copy().astype(bf)
    tq = np.arange(128)[:, None]
    sq_ = np.arange(128)[None, :]
    c["c_diagm"] = (sq_ <= tq).astype(np.float32)
    c["c_diagn"] = np.where(sq_ <= tq, 0.0, -BIG).astype(np.float32)
    c["c_pow2"] = np.broadcast_to((0.5 ** np.arange(1, NBIS + 1)).astype(np.float32)[None, :], (128, NBIS)).copy()
    return c


def host_gains(inp):
    g = np.zeros((128, 2 * GC_PER_LAYER), np.float32)
    for l in range(2):
        gc = gcols(l)
        for nm, key in [("mix_pre", "norm_mix_pre"), ("mix_post", "norm_mix_post"), ("mlp_pre", "norm_mlp_pre"),
                        ("mlp_post", "norm_mlp_post")]:
            g[:, gc[nm]:gc[nm] + 32] = np.asarray(inp[key][l], np.float32).reshape(32, 128).T
        g[:, gc["gla_gain"]:gc["gla_gain"] + 2] = np.asarray(inp["gla_head_gain"][l], np.float32).reshape(2, 128).T
        g[:, gc["gla_bias"]:gc["gla_bias"] + 4] = np.asarray(inp["gla_gate_bias"][l], np.float32).reshape(4, 128).T
    return g


class Prog:
    def __init__(self):
        self.nc = bass.Bass("TRN2", target_bir_lowering=False)
        self.t = {}

    def dram(self, name, shape, dtype, kind):
        self.t[name] = self.nc.dram_tensor(name, list(shape), dtype, kind=kind).ap()
        return self.t[name]


def declare_consts(P):
    io = {}
    for k, (shp, dt_) in CONST_SPECS.items():
        io[k] = P.dram(k, shp, dt_, "ExternalInput")
    return io


def issue_collectives(S, nc, items):
    sem = S.xsems[0]
    for (src, dst) in items:
        ins = nc.gpsimd.collective_compute("AllGather", ALU.bypass, replica_groups=PAIRS, ins=[src.opt()],
                                           outs=[dst.opt()])
        sem.count += 1
        ins.then_inc(sem.h, 1)


def emit_layer(S, C, io, nc, l, xT, xoutT, w_in, gate_up, rel_bias, w_branch, w_out, w_up, w_down, sc):
    gc = gcols(l)
    qkT, skT, dkik, svt, dvt, gaT, tm = sc["qkT"], sc["skT"], sc["dkik"], sc["svt"], sc["dvt"], sc["gaT"], sc["tm"]
    gatesT, qgT, oaloc, sfin = sc["gatesT"], sc["qgT"], sc["oaloc"], sc["sfin"]
    phase_begin(S)
    hT, hB = S.alloc(KC * T)
    mark = S.aoff
    phase_norm(S, C, xT, gc["mix_pre"], hT, hB)
    S.barrier()
    S.aoff = mark
    segs = []
    for nm in ["gq", "gk", "gg", "dq", "iq", "sq"]:
        c0, w = SEG[nm]
        segs.append((c0, w, qkT[FM_ROWS[nm]:FM_ROWS[nm] + w, :], "copy"))
    segs.append((SEG["sk"][0], 1024, skT, "copy"))
    segs.append((SEG["dk"][0], 128, dkik[0:128, :], "copy"))
    segs.append((SEG["ik"][0], 64, dkik[128:192, :], "copy"))
    segs.append((SEG["ga"][0], 16, gaT, "f32"))
    phase_proj_fm(S, C, hT, hB, w_in, segs)
    S.barrier()
    S.aoff = mark
    tsegs = []
    for nm in ["gv", "iw"]:
        c0, w = SEG[nm]
        tsegs.append((c0, w, tm[:, TM_COLS[nm]:TM_COLS[nm] + w]))
    tsegs.append((SEG["sv"][0], 1024, svt))
    tsegs.append((SEG["dv"][0], 128, dvt))
    phase_proj_tm(S, C, hT, hB, w_in, tsegs)
    S.barrier()
    issue_collectives(S, nc, [(sc[k], sc[k + "_g"]) for k in ["skT", "dkik", "svt", "dvt"]])
    S.aoff = mark
    phase_gla_local(S, C, io, qkT, gaT, tm[:, 0:1024], gate_up, gc, qgT, oaloc, sfin)
    S.barrier()
    issue_collectives(S, nc, [(sc["sfin"], sc["sfin_g"])])
    S.barrier()
    S.aoff = mark
    skT_o, dkik_o, svt_o, dvt_o = sc["skT_g"][0:1024, :], sc["dkik_g"][0:192, :], sc["svt_g"][0:T, :], sc["dvt_g"][0:T, :]
    sfin_o = sc["sfin_g"][0:512, :]
    oT, mT, yT, x1T, uT = sc["oT"], sc["mT"], sc["yT"], sc["x1T"], sc["uT"]
    co = make_gates_co(S, C, hT, hB, w_in, SEG["gates"][0], 12288, gatesT)
    phase_dsa(S, C, io, qkT[FM_ROWS["dq"]:FM_ROWS["dq"] + 1024, :], dkik[0:128, :], dvt,
              qkT[FM_ROWS["iq"]:FM_ROWS["iq"] + 2048, :], dkik[128:192, :], tm[:, TM_COLS["iw"]:TM_COLS["iw"] + 32],
              dkik_o[0:128, :], dvt_o, dkik_o[128:192, :], rel_bias, sc["gvec"], oT, nc, co=co, low_mark=S.abase)
    phase_begin(S)
    phase_sb(S, C, io, qkT[FM_ROWS["sq"]:FM_ROWS["sq"] + 1024, :], skT, svt, skT_o, svt_o, oT)
    phase_begin(S)
    phase_gla_fin(S, C, sfin_o, qgT, oaloc, qkT[FM_ROWS["gg"]:FM_ROWS["gg"] + 1024, :], gc, oT)
    phase_begin(S)
    phase_merge(S, C, oT, gatesT, w_branch, mT)
    phase_begin(S)
    phase_linear_resid(S, C, mT, w_out, D, yT, xT, x1T, gc["mix_post"])
    phase_begin(S)
    hT, hB = S.alloc(KC * T)
    mark = S.aoff
    phase_norm(S, C, x1T, gc["mlp_pre"], hT, hB)
    S.barrier()
    S.aoff = mark
    phase_proj_fm(S, C, hT, hB, w_up, [(0, DFF, uT, "relu2")])
    phase_begin(S)
    phase_linear_resid(S, C, uT, w_down, DFF, yT, x1T, xoutT, gc["mlp_post"])


SCRATCH = dict(qkT=([R_FM, T], BF16), skT=([1024, T], BF16), dkik=([192, T], BF16), svt=([T, 1024], BF16),
               dvt=([T, 128], BF16), gaT=([16, T], F32), tm=([T, TM_W], BF16),
               gatesT=([12288, T], BF16), qgT=([512, T], BF16), oaloc=([1024, T], F32), sfin=([512, 256], F32),
               skT_g=([2048, T], BF16), dkik_g=([384, T], BF16), svt_g=([2 * T, 1024], BF16), dvt_g=([2 * T, 128], BF16),
               sfin_g=([1024, 256], F32),
               gvec=([8, OHW], BF16), oT=([3072, T], BF16), mT=([D, T], BF16), yT=([D, T], F32), x1T=([D, T], F32),
               uT=([DFF, T], BF16), x2T=([D, T], F32))


def build_full():
    P = Prog()
    nc = P.nc
    io = declare_consts(P)
    xT = P.dram("xT", [D, T], F32, "ExternalInput")
    w_in = P.dram("w_in", [2, D, IN_COLS], F32, "ExternalInput")
    gate_up = P.dram("gate_up", [2, 16, 512], F32, "ExternalInput")
    rel_bias = P.dram("rel_bias", [32, 8], F32, "ExternalInput")
    w_branch = P.dram("w_branch", [2, 3, 1024, D], F32, "ExternalInput")
    w_out = P.dram("w_out", [2, D, D], F32, "ExternalInput")
    w_up = P.dram("w_up", [2, D, DFF], F32, "ExternalInput")
    w_down = P.dram("w_down", [2, DFF, D], F32, "ExternalInput")
    sc = {k: P.dram(k, shp, dt_, "Internal") for k, (shp, dt_) in SCRATCH.items()}
    xoutT = P.dram("xoutT", [D, T], F32, "ExternalOutput")
    with ExitStack() as st:
        S = Sched(nc, st)
        C = setup_consts(S, nc, io)
        xin = xT
        for l in range(2):
            xo = sc["x2T"] if l == 0 else xoutT
            emit_layer(S, C, io, nc, l, xin, xo, w_in[l], gate_up[l], rel_bias, [w_branch[l, i] for i in range(3)],
                       w_out[l], w_up[l], w_down[l], sc)
            xin = sc["x2T"]
        S.finish()
    return nc


def kernel(**inputs):
    x = np.asarray(inputs["x"], np.float32)
    consts = host_consts()
    consts["gains"] = host_gains(inputs)
    n = 8
    shared = dict(
        w_in=np.asarray(inputs["w_in"], np.float32), gate_up=np.asarray(inputs["gla_gate_up"], np.float32),
        rel_bias=np.asarray(inputs["rel_bias"], np.float32), w_branch=np.asarray(inputs["w_branch"], np.float32),
        w_out=np.asarray(inputs["w_out"], np.float32), w_up=np.asarray(inputs["w_mlp_up"], np.float32),
        w_down=np.asarray(inputs["w_mlp_down"], np.float32))
    in_maps = []
    for c in range(n):
        b, half = c // 2, c % 2
        m = dict(consts)
        m.update(shared)
        m["flag"] = np.full((128, 1), float(half), np.float32)
        m["xT"] = np.ascontiguousarray(x[b, half * T:(half + 1) * T, :].T)
        in_maps.append(m)
    nc = build_full()
    res = run_bass_kernel_spmd(nc, in_maps, core_ids=list(range(n))).results
    out = np.empty((4, 2048, D), np.float32)
    for c in range(n):
        b, half = c // 2, c % 2
        out[b, half * T:(half + 1) * T, :] = np.asarray(res[c]["xoutT"]).T
    return out
```
